# Optimizing a Trainium2 kernel written in Bass

```python
import jax, jax.numpy as jnp
from jax import lax
import numpy as np

D_MODEL = 1024
BATCH = 1
SEQ = 16384
DEPTH = 2

HEAD_DIM = 64
N_ATTN_HEADS = 8
N_KV_GROUPS = 2
GQA_RATIO = N_ATTN_HEADS // N_KV_GROUPS
ATTN_WIDTH = N_ATTN_HEADS * HEAD_DIM
N_MLP_GROUPS = 8
MLP_WIDTH = N_MLP_GROUPS * HEAD_DIM
MIX_WIDTH = ATTN_WIDTH + MLP_WIDTH
KV_WIDTH = N_KV_GROUPS * HEAD_DIM
N_BRANCH = 3
GATE_WIDTH = N_ATTN_HEADS * N_BRANCH
IN_SIZES = [ATTN_WIDTH] + [KV_WIDTH] * 6 + [GATE_WIDTH, MLP_WIDTH, MLP_WIDTH]
IN_WIDTH = ATTN_WIDTH + 6 * KV_WIDTH + GATE_WIDTH + 2 * MLP_WIDTH
ROT_DIM = HEAD_DIM // 4
ROPE_THETA = 500000.0
CMP_LEN = 32
CMP_STRIDE = 16
CMP_HIDDEN = 256
SEL_LEN = 64
N_SELECT = 16
WINDOW = 512
Q_BLOCK = 128
CHUNK = 128
D_FF = 2816
CONV_WIDTH = 3
PLE_DIM = 256
NORM_EPS = 1e-6
NEG = -1e30
FORCED = 1e9

kernel_name = "hybrid_nsa_gmlp_convffn_trunk"


def rms_norm(x, gain):
    xf = x.astype(jnp.float32)
    y = xf * lax.rsqrt(jnp.mean(xf * xf, axis=-1, keepdims=True) + NORM_EPS)
    return (y * gain.astype(jnp.float32)).astype(x.dtype)


def layer_norm(x, gain, bias):
    xf = x.astype(jnp.float32)
    mu = jnp.mean(xf, axis=-1, keepdims=True)
    var = jnp.mean(jnp.square(xf - mu), axis=-1, keepdims=True)
    y = (xf - mu) * lax.rsqrt(var + NORM_EPS) * gain.astype(jnp.float32) + bias.astype(jnp.float32)
    return y.astype(x.dtype)


def rope_partial(x, pos):
    half = ROT_DIM // 2
    inv = ROPE_THETA ** (-jnp.arange(half, dtype=jnp.float32) / half)
    ang = pos.astype(jnp.float32)[:, None] * inv[None, :]
    cos = jnp.cos(ang)[:, None, :]
    sin = jnp.sin(ang)[:, None, :]
    xr = x[..., :ROT_DIM].astype(jnp.float32)
    x1, x2 = xr[..., :half], xr[..., half:]
    rot = jnp.concatenate([x1 * cos - x2 * sin, x2 * cos + x1 * sin], axis=-1).astype(x.dtype)
    return jnp.concatenate([rot, x[..., ROT_DIM:]], axis=-1)


def masked_softmax(s, mask):
    s = jnp.where(mask, s, NEG)
    m = jnp.max(s, axis=-1, keepdims=True)
    e = jnp.exp(s - m) * mask
    return e / jnp.maximum(jnp.sum(e, axis=-1, keepdims=True), 1e-30)


def compress(t, pe, w1, b1, w2, b2):
    B, S, G, D = t.shape
    r = CMP_LEN // CMP_STRIDE
    nch = S // CMP_STRIDE
    nc = nch - r + 1
    ch = t.reshape(B, nch, CMP_STRIDE, G, D)
    blocks = jnp.concatenate([ch[:, j:j + nc] for j in range(r)], axis=2)
    blocks = blocks + pe[None, None, :, None, :]
    flat = blocks.transpose(0, 1, 3, 2, 4).reshape(B, nc, G, CMP_LEN * D)
    h = jax.nn.gelu(flat @ w1 + b1)
    return h @ w2 + b2


def nsa_attention(q, kc, vc, ks, vs, kw, vw, gates):
    B, S, H, D = q.shape
    G = kc.shape[2]
    R = H // G
    nc = kc.shape[1]
    ns = S // SEL_LEN
    n_sel = min(N_SELECT, ns)
    scale = D ** -0.5
    cmp_start = jnp.arange(nc) * CMP_STRIDE
    cmp_end = cmp_start + CMP_LEN - 1
    sel_start = jnp.arange(ns) * SEL_LEN
    overlap = ((cmp_end[:, None] >= sel_start[None, :]) &
               (cmp_start[:, None] <= sel_start[None, :] + SEL_LEN - 1)).astype(jnp.float32)
    ks_blk = ks.reshape(B, ns, SEL_LEN, G, D).transpose(0, 3, 1, 2, 4)
    vs_blk = vs.reshape(B, ns, SEL_LEN, G, D).transpose(0, 3, 1, 2, 4)
    kw_pad = jnp.pad(kw, ((0, 0), (WINDOW, 0), (0, 0), (0, 0)))
    vw_pad = jnp.pad(vw, ((0, 0), (WINDOW, 0), (0, 0), (0, 0)))
    qg = q.reshape(B, S, G, R, D)
    gg = gates.reshape(B, S, G, R, N_BRANCH)
    bi = jnp.arange(B)[:, None, None, None]
    gi = jnp.arange(G)[None, :, None, None]
    blk = jnp.arange(ns)
    f32 = jnp.float32

    def block(qi):
        t0 = qi * Q_BLOCK
        tq = t0 + jnp.arange(Q_BLOCK)
        qb = lax.dynamic_slice_in_dim(qg, t0, Q_BLOCK, axis=1)
        gb = lax.dynamic_slice_in_dim(gg, t0, Q_BLOCK, axis=1)
        s_c = jnp.einsum('bqgrd,bngd->bgrqn', qb, kc, preferred_element_type=f32) * scale
        p_c = masked_softmax(s_c, cmp_end[None, :] <= tq[:, None])
        o_c = jnp.einsum('bgrqn,bngd->bqgrd', p_c.astype(vc.dtype), vc)
        imp = jnp.einsum('bgrqn,nj->bgqj', p_c, overlap)
        cur = tq // SEL_LEN
        valid = blk[None, :] <= cur[:, None]
        forced = valid & ((blk[None, :] == 0) | (blk[None, :] == cur[:, None]) | (blk[None, :] == cur[:, None] - 1))
        score = jnp.where(forced, FORCED, jnp.where(valid, imp, NEG))
        vals, idx = lax.top_k(score, n_sel)
        sel_ok = vals > NEG * 0.5
        k_g = ks_blk[bi, gi, idx].reshape(B, G, Q_BLOCK, n_sel * SEL_LEN, D)
        v_g = vs_blk[bi, gi, idx].reshape(B, G, Q_BLOCK, n_sel * SEL_LEN, D)
        kpos = idx[..., None] * SEL_LEN + jnp.arange(SEL_LEN)
        mask_s = ((kpos <= tq[:, None, None]) & sel_ok[..., None]).reshape(B, G, Q_BLOCK, n_sel * SEL_LEN)
        s_s = jnp.einsum('bqgrd,bgqkd->bgrqk', qb, k_g, preferred_element_type=f32) * scale
        p_s = masked_softmax(s_s, mask_s[:, :, None])
        o_s = jnp.einsum('bgrqk,bgqkd->bqgrd', p_s.astype(v_g.dtype), v_g)
        kwb = lax.dynamic_slice_in_dim(kw_pad, t0, Q_BLOCK + WINDOW, axis=1)
        vwb = lax.dynamic_slice_in_dim(vw_pad, t0, Q_BLOCK + WINDOW, axis=1)
        kpos_w = t0 - WINDOW + jnp.arange(Q_BLOCK + WINDOW)
        mask_w = ((kpos_w[None, :] <= tq[:, None]) & (kpos_w[None, :] > tq[:, None] - WINDOW)
                  & (kpos_w[None, :] >= 0))
        s_w = jnp.einsum('bqgrd,bkgd->bgrqk', qb, kwb, preferred_element_type=f32) * scale
        p_w = masked_softmax(s_w, mask_w)
        o_w = jnp.einsum('bgrqk,bkgd->bqgrd', p_w.astype(vwb.dtype), vwb)
        o = gb[..., 0:1] * o_c + gb[..., 1:2] * o_s + gb[..., 2:3] * o_w
        return o.reshape(B, Q_BLOCK, H * D)

    out = lax.map(block, jnp.arange(S // Q_BLOCK))
    return out.transpose(1, 0, 2, 3).reshape(B, S, H * D)


def spatial_gating(u, v, ln_g, ln_b, w_s, b_s):
    B, S, _ = u.shape
    u = jax.nn.gelu(u)
    v = layer_norm(jax.nn.gelu(v), ln_g, ln_b)
    vc = v.reshape(B, S // CHUNK, CHUNK, N_MLP_GROUPS, HEAD_DIM)
    causal = jnp.tril(jnp.ones((CHUNK, CHUNK), dtype=bool))
    ws = jnp.where(causal[None], w_s, 0)
    mixed = jnp.einsum('gts,bcsgd->bctgd', ws, vc) + b_s.T[None, None, :, :, None]
    return u * mixed.reshape(B, S, MLP_WIDTH)


def conv_ffn(x, w_up, conv_w, conv_b, w_down):
    S = x.shape[1]
    h = x @ w_up
    hp = jnp.pad(h, ((0, 0), (CONV_WIDTH - 1, 0), (0, 0)))
    hc = conv_b
    for k in range(CONV_WIDTH):
        hc = hc + hp[:, k:k + S] * conv_w[k]
    g, up = jnp.split(hc, 2, axis=-1)
    return (jax.nn.silu(g) * up) @ w_down


def setup_inputs(seed: int = 0) -> dict:
    key = jax.random.key(seed)
    ks = jax.random.split(key, 32)
    L = DEPTH

    def nrm(k, shape, scale):
        return jax.random.normal(k, shape, dtype=jnp.float32) * scale

    def gain(k, shape):
        return 1.0 + nrm(k, shape, 0.05)

    return {
        "x": nrm(ks[0], (BATCH, SEQ, D_MODEL), 1.0),
        "p": nrm(ks[1], (DEPTH, BATCH, SEQ, PLE_DIM), 1.0),
        "pre_mix_g": gain(ks[2], (L, D_MODEL)),
        "w_in": nrm(ks[3], (L, D_MODEL, IN_WIDTH), D_MODEL ** -0.5),
        "cmp_pe": nrm(ks[4], (L, 2, CMP_LEN, HEAD_DIM), 0.1),
        "cmp_w1": nrm(ks[5], (L, 2, CMP_LEN * HEAD_DIM, CMP_HIDDEN), (CMP_LEN * HEAD_DIM) ** -0.5),
        "cmp_b1": nrm(ks[6], (L, 2, CMP_HIDDEN), 0.02),
        "cmp_w2": nrm(ks[7], (L, 2, CMP_HIDDEN, HEAD_DIM), CMP_HIDDEN ** -0.5),
        "cmp_b2": nrm(ks[8], (L, 2, HEAD_DIM), 0.02),
        "gmlp_ln_g": gain(ks[9], (L, MLP_WIDTH)),
        "gmlp_ln_b": nrm(ks[10], (L, MLP_WIDTH), 0.02),
        "gmlp_ws": nrm(ks[11], (L, N_MLP_GROUPS, CHUNK, CHUNK), 0.5 * CHUNK ** -0.5),
        "gmlp_bs": 1.0 + nrm(ks[12], (L, N_MLP_GROUPS, CHUNK), 0.1),
        "attn_out_g": gain(ks[13], (L, ATTN_WIDTH)),
        "mlp_out_g": gain(ks[14], (L, MLP_WIDTH)),
        "w_o": nrm(ks[15], (L, MIX_WIDTH, D_MODEL), MIX_WIDTH ** -0.5),
        "post_mix_g": gain(ks[16], (L, D_MODEL)),
        "pre_ffn_g": gain(ks[17], (L, D_MODEL)),
        "w_up": nrm(ks[18], (L, D_MODEL, 2 * D_FF), D_MODEL ** -0.5),
        "conv_w": nrm(ks[19], (L, CONV_WIDTH, 2 * D_FF), CONV_WIDTH ** -0.5),
        "conv_b": nrm(ks[20], (L, 2 * D_FF), 0.02),
        "w_down": nrm(ks[21], (L, D_FF, D_MODEL), D_FF ** -0.5),
        "post_ffn_g": gain(ks[22], (L, D_MODEL)),
        "ple_norm_g": gain(ks[23], (L, D_MODEL)),
        "w_ple_gate": nrm(ks[24], (L, D_MODEL, D_MODEL), D_MODEL ** -0.5),
        "w_ple_proj": nrm(ks[25], (L, PLE_DIM, D_MODEL), PLE_DIM ** -0.5),
    }


def reference(x, p, pre_mix_g, w_in, cmp_pe, cmp_w1, cmp_b1, cmp_w2, cmp_b2,
              gmlp_ln_g, gmlp_ln_b, gmlp_ws, gmlp_bs, attn_out_g, mlp_out_g, w_o,
              post_mix_g, pre_ffn_g, w_up, conv_w, conv_b, w_down, post_ffn_g,
              ple_norm_g, w_ple_gate, w_ple_proj):
    B, S, _ = x.shape
    pos = jnp.arange(S)
    split_at = [int(o) for o in np.cumsum(IN_SIZES)[:-1]]
    nc = S // CMP_STRIDE - CMP_LEN // CMP_STRIDE + 1
    cmp_pos = jnp.arange(nc) * CMP_STRIDE + CMP_LEN - 1
    h = x
    for i in range(DEPTH):
        a = rms_norm(h, pre_mix_g[i])
        z = a @ w_in[i]
        zq, zkc, zvc, zks, zvs, zkw, zvw, zg, zu, zv = jnp.split(z, split_at, axis=-1)
        kv = lambda t: t.reshape(B, S, N_KV_GROUPS, HEAD_DIM)
        q = rope_partial(zq.reshape(B, S, N_ATTN_HEADS, HEAD_DIM), pos)
        kc = compress(kv(zkc), cmp_pe[i, 0], cmp_w1[i, 0], cmp_b1[i, 0], cmp_w2[i, 0], cmp_b2[i, 0])
        vc = compress(kv(zvc), cmp_pe[i, 1], cmp_w1[i, 1], cmp_b1[i, 1], cmp_w2[i, 1], cmp_b2[i, 1])
        kc = rope_partial(kc, cmp_pos)
        k_sel = rope_partial(kv(zks), pos)
        k_win = rope_partial(kv(zkw), pos)
        gates = jax.nn.sigmoid(zg.reshape(B, S, N_ATTN_HEADS, N_BRANCH))
        attn = nsa_attention(q, kc, vc, k_sel, kv(zvs), k_win, kv(zvw), gates)
        mlp = spatial_gating(zu, zv, gmlp_ln_g[i], gmlp_ln_b[i], gmlp_ws[i], gmlp_bs[i])
        mix = jnp.concatenate([rms_norm(attn, attn_out_g[i]), rms_norm(mlp, mlp_out_g[i])], axis=-1) @ w_o[i]
        h = h + rms_norm(mix, post_mix_g[i])
        f = conv_ffn(rms_norm(h, pre_ffn_g[i]), w_up[i], conv_w[i], conv_b[i], w_down[i])
        h = h + rms_norm(f, post_ffn_g[i])
        gate = jax.nn.sigmoid(rms_norm(h, ple_norm_g[i]) @ w_ple_gate[i])
        h = h + gate * (p[i] @ w_ple_proj[i])
    return h
```

```python
import numpy as np
import ml_dtypes
from contextlib import ExitStack
import concourse.bass as bass
import concourse.mybir as mybir
from concourse.bass_utils import run_bass_kernel_spmd

F32 = mybir.dt.float32
BF16 = mybir.dt.bfloat16
AF = mybir.ActivationFunctionType
ALU = mybir.AluOpType
AX = mybir.AxisListType
NPBF16 = ml_dtypes.bfloat16


class Buf:
    __slots__ = ("name", "last_w", "readers")

    def __init__(self, name=""):
        self.name = name
        self.last_w = None
        self.readers = []


class Op:
    __slots__ = ("eng", "fn", "deps", "is_dma", "stream", "needs_inc", "tok", "idx", "dma_upto")

    def __init__(self, eng, fn, is_dma=False, stream=None):
        self.eng = eng
        self.fn = fn
        self.deps = set()
        self.is_dma = is_dma
        self.stream = stream
        self.needs_inc = False
        self.tok = None
        self.idx = -1
        self.dma_upto = {}


class Prog:
    ENGS = ("sync", "scalar", "vector", "gpsimd", "tensor")
    SEM_ROLL = 6000

    def __init__(self, nc):
        self.nc = nc
        self.ops = []
        self.stack = ExitStack()
        self.nbuf = 0

    def buf(self, name=""):
        self.nbuf += 1
        return Buf(name or f"b{self.nbuf}")

    def sb(self, name, shape, dtype):
        return self.stack.enter_context(self.nc.sbuf_tensor("sb_" + name, list(shape), dtype))

    def ps(self, name, shape, dtype=F32):
        return self.stack.enter_context(self.nc.psum_tensor("ps_" + name, list(shape), dtype))

    def op(self, eng, fn, reads=(), writes=(), accum=False):
        o = Op(eng, fn)
        self._deps(o, reads, writes, accum)
        return o

    def dma(self, eng, fn, stream, reads=(), writes=()):
        o = Op(eng, fn, is_dma=True, stream=stream)
        self._deps(o, reads, writes, False)
        return o

    def _deps(self, o, reads, writes, accum):
        o.idx = len(self.ops)
        for b in reads:
            if b.last_w is not None:
                o.deps.add(b.last_w)
        for b in writes:
            if b.last_w is not None:
                lw = self.ops[b.last_w]
                if not (accum and lw.eng == o.eng and not lw.is_dma):
                    o.deps.add(b.last_w)
            for r in b.readers:
                o.deps.add(r)
        for b in reads:
            b.readers.append(o.idx)
        for b in writes:
            b.last_w = o.idx
            b.readers = []
        o.deps.discard(o.idx)
        if o.eng == "tensor" and not o.is_dma:
            o.deps = {d for d in o.deps if not (self.ops[d].eng == "tensor" and not self.ops[d].is_dma)}
        self.ops.append(o)

    def emit(self):
        nc = self.nc
        ops = self.ops
        for o in ops:
            for d in o.deps:
                ops[d].needs_inc = True
        sems = {}

        def new_sem(tag):
            return self.stack.enter_context(nc.semaphore(f"s_{tag}_{len(sems)}"))

        eng_sem = {}
        eng_cnt = {}
        stream_sem = {}
        stream_cnt = {}
        stream_hist = {}
        for o in ops:
            if o.is_dma:
                if o.stream not in stream_sem:
                    stream_sem[o.stream] = new_sem("d")
                    sems[len(sems)] = 1
                    stream_cnt[o.stream] = 0
                    stream_hist[o.stream] = []
                stream_cnt[o.stream] += 16
                o.tok = (stream_sem[o.stream], stream_cnt[o.stream])
                stream_hist[o.stream].append(o.idx)
            elif o.needs_inc:
                if o.eng not in eng_sem or eng_cnt[o.eng] >= self.SEM_ROLL:
                    eng_sem[o.eng] = new_sem(o.eng[0])
                    sems[len(sems)] = 1
                    eng_cnt[o.eng] = 0
                eng_cnt[o.eng] += 1
                o.tok = (eng_sem[o.eng], eng_cnt[o.eng])
        self.n_sems = len(sems)
        import bisect
        seen = {e: {} for e in self.ENGS}
        waits = [None] * len(ops)
        for o in ops:
            need = {}
            for d in o.deps:
                do = ops[d]
                sem, val = do.tok
                if do.is_dma:
                    h = stream_hist[do.stream]
                    k = bisect.bisect_left(h, o.idx)
                    val = 16 * k
                key = id(sem)
                if key not in need or need[key][1] < val:
                    need[key] = (sem, val)
            w = []
            s = seen[o.eng]
            for key, (sem, val) in need.items():
                if s.get(key, 0) < val:
                    s[key] = val
                    w.append((sem, val))
            waits[o.idx] = w
        with nc.Block() as block:
            def make(engname):
                def body(e):
                    for o in ops:
                        if o.eng != engname:
                            continue
                        for sem, val in waits[o.idx]:
                            e.wait_ge(sem, val)
                        ins = o.fn(e)
                        if o.is_dma:
                            ins.then_inc(o.tok[0], 16)
                        elif o.needs_inc:
                            ins.then_inc(o.tok[0], 1)
                    if engname == "sync":
                        for st, sem in stream_sem.items():
                            e.wait_ge(sem, stream_cnt[st])
                return body
            block.sync(make("sync"))
            block.scalar(make("scalar"))
            block.vector(make("vector"))
            block.gpsimd(make("gpsimd"))
            block.tensor(make("tensor"))
        self.stack.close()

BF = NPBF16

S = 16384; D = 1024; NCORE = 8; TPC = 2048; NTILE = 16
INW = 2328
EPS = 1e-6

def build_W(ncols, chunk=4096):
    nc = bass.Bass("TRN2", target_bir_lowering=False)
    win = nc.dram_tensor("win", [128, ncols], F32, kind="ExternalInput").ap()
    wout = nc.dram_tensor("wout", [128, ncols], BF16, kind="ExternalOutput").ap()
    P = Prog(nc)
    nb = 3
    st = [P.sb(f"st{i}", [128, chunk], F32) for i in range(nb)]
    sb = [P.sb(f"sb{i}", [128, chunk], BF16) for i in range(nb)]
    b_st = [P.buf() for _ in range(nb)]
    b_sb = [P.buf() for _ in range(nb)]
    engs = ["vector", "gpsimd", "vector"]
    i = 0
    for c0 in range(0, ncols, chunk):
        c1 = min(ncols, c0 + chunk); w = c1 - c0; k = i % nb
        P.dma("sync", lambda e, k=k, c0=c0, c1=c1, w=w: e.dma_start(out=st[k][:, 0:w], in_=win[:, c0:c1]), f"wl{k}", writes=[b_st[k]])
        en = engs[i % 3]
        if en == "scalar":
            P.op(en, lambda e, k=k, w=w: e.copy(out=sb[k][:, 0:w], in_=st[k][:, 0:w]), reads=[b_st[k]], writes=[b_sb[k]])
        else:
            P.op(en, lambda e, k=k, w=w: e.tensor_copy(out=sb[k][:, 0:w], in_=st[k][:, 0:w]), reads=[b_st[k]], writes=[b_sb[k]])
        P.dma("sync", lambda e, k=k, c0=c0, c1=c1, w=w: e.dma_start(out=wout[:, c0:c1], in_=sb[k][:, 0:w]), f"ws{k}", reads=[b_sb[k]])
        i += 1
    P.emit()
    return nc


def build_A():
    nc = bass.Bass("TRN2", target_bir_lowering=False)
    din = lambda n, sh, dt=F32: nc.dram_tensor(n, sh, dt, kind="ExternalInput").ap()
    dout = lambda n, sh, dt=F32: nc.dram_tensor(n, sh, dt, kind="ExternalOutput").ap()
    h_d = din("h", [TPC, D])
    gpre_d = din("gpre", [128, D])
    w_d = din("w", [128, 8 * INW], BF16)
    cos_d = din("cos", [128, NTILE * 8]); sin_d = din("sin", [128, NTILE * 8])
    lng_d = din("lng", [128, 512]); lnb_d = din("lnb", [128, 512]); gmlp_d = din("gmlp", [128, 512])
    bsT_d = din("bsT", [128, 8])
    wsT_d = din("wsT", [128, 8 * 128], BF16)
    tri_d = din("tri", [128, 128], BF16)
    id_d = din("ident", [128, 128], BF16)
    qkv_o = dout("qkv", [TPC, 1280], BF16)
    gates_o = dout("gates", [TPC, 24])
    mlpn_o = dout("mlpn", [TPC, 512], BF16)
    P = Prog(nc)
    W = P.sb("W", [128, 8, INW], BF16); b_W = P.buf()
    gpre = P.sb("gpre", [128, D], F32); b_c = P.buf()
    cos = P.sb("cos", [128, NTILE, 8], F32); sin = P.sb("sin", [128, NTILE, 8], F32)
    lng = P.sb("lng", [128, 512], F32); lnb = P.sb("lnb", [128, 512], F32); gmlp = P.sb("gmlpg", [128, 512], F32)
    bsT = P.sb("bsT", [128, 8], F32)
    wsT = P.sb("wsT", [128, 8, 128], BF16); b_ws = P.buf()
    tri = P.sb("tri", [128, 128], BF16)
    ident = P.sb("ident", [128, 128], BF16)
    for kt in range(8):
        P.dma("sync" if kt % 2 == 0 else "gpsimd", lambda e, kt=kt: e.dma_start(out=W[:, kt, :], in_=w_d[:, kt * INW:(kt + 1) * INW]), f"cw{kt%2}", writes=[b_W])
    for (t, d_) in [(gpre, gpre_d), (lng, lng_d), (lnb, lnb_d), (gmlp, gmlp_d), (bsT, bsT_d), (tri, tri_d), (ident, id_d)]:
        P.dma("sync", lambda e, t=t, d_=d_: e.dma_start(out=t[:], in_=d_), "cc", writes=[b_c])
    P.dma("sync", lambda e: e.dma_start(out=cos[:].rearrange("p a b -> p (a b)"), in_=cos_d), "cc", writes=[b_c])
    P.dma("sync", lambda e: e.dma_start(out=sin[:].rearrange("p a b -> p (a b)"), in_=sin_d), "cc", writes=[b_c])
    P.dma("sync", lambda e: e.dma_start(out=wsT[:].rearrange("p a b -> p (a b)"), in_=wsT_d), "cc", writes=[b_ws])
    P.op("vector", lambda e: e.tensor_tensor(out=wsT[:], in0=wsT[:], in1=tri[:].unsqueeze(1).to_broadcast([128, 8, 128]), op=ALU.mult), reads=[b_ws, b_c], writes=[b_ws])

    NB = 2
    hs = [P.sb(f"hs{i}", [128, D], F32) for i in range(NB)]; b_hs = [P.buf() for _ in range(NB)]
    junk = P.sb("junk", [128, D], BF16); b_junk = P.buf()
    st = [P.sb(f"stat{i}", [128, 8], F32) for i in range(NB)]; b_st = [P.buf() for _ in range(NB)]
    a_bf = P.sb("a_bf", [128, D], BF16); b_a = P.buf()
    aT = P.sb("aT", [128, 8, 128], BF16); b_aT = P.buf()
    pT = P.ps("pT", [128, 8, 128], BF16); b_pT = P.buf()
    zps = [P.ps(f"zps{i}", [128, 512], F32) for i in range(3)]; b_zps = [P.buf() for _ in range(3)]
    mixps = P.ps("mixps", [128, 512], F32); b_mix = P.buf()
    z = P.sb("z", [128, INW], F32); b_z = [P.buf() for _ in range(5)]
    qkv = [P.sb(f"qkv{i}", [128, 1280], BF16) for i in range(NB)]; b_qkv = [P.buf() for _ in range(NB)]
    rt = P.sb("rt", [128, 4, 12, 8], F32); b_rt = P.buf()
    gt = [P.sb(f"gt{i}", [128, 24], F32) for i in range(NB)]; b_gt = [P.buf() for _ in range(NB)]
    u = P.sb("u", [128, 512], F32); b_u = P.buf()
    v = P.sb("v", [128, 512], F32); b_v = P.buf()
    bnst = P.sb("bnst", [128, 6], F32); b_bn = P.buf()
    mv = P.sb("mv", [128, 4], F32); b_mv = P.buf()
    vn = P.sb("vn", [128, 512], BF16); b_vn = P.buf()
    m1 = P.sb("m1", [128, 512], F32); b_m1 = P.buf()
    mlpn = [P.sb(f"mlpn{i}", [128, 512], BF16) for i in range(NB)]; b_mlpn = [P.buf() for _ in range(NB)]
    chunks = [(0, 512), (512, 512), (1024, 512), (1536, 512), (2048, 280)]

    def load(ti):
        k = ti % NB
        P.dma("sync", lambda e: e.dma_start(out=hs[k][:], in_=h_d[ti * 128:(ti + 1) * 128, :]), f"hl{k}", writes=[b_hs[k]])

    def do_tile(ti):
        k = ti % NB
        if ti + 1 < NTILE:
            load(ti + 1)
        s_ = st[k]
        P.op("scalar", lambda e: e.activation(out=junk[:], in_=hs[k][:], func=AF.Square, accum_out=s_[:, 0:1]), reads=[b_hs[k]], writes=[b_junk, b_st[k]])
        P.op("scalar", lambda e: e.activation(out=s_[:, 1:2], in_=s_[:, 0:1], func=AF.Sqrt, scale=1.0 / D, bias=EPS), reads=[b_st[k]], writes=[b_st[k]])
        P.op("vector", lambda e: e.reciprocal(out=s_[:, 2:3], in_=s_[:, 1:2]), reads=[b_st[k]], writes=[b_st[k]])
        P.op("vector", lambda e: e.scalar_tensor_tensor(out=a_bf[:], in0=hs[k][:], scalar=s_[:, 2:3], in1=gpre[:], op0=ALU.mult, op1=ALU.mult), reads=[b_hs[k], b_st[k], b_c], writes=[b_a])
        for kt in range(8):
            P.op("tensor", lambda e, kt=kt: e.transpose(out=pT[:, kt, :], in_=a_bf[:, kt * 128:(kt + 1) * 128], identity=ident[:]), reads=[b_a, b_c], writes=[b_pT], accum=True)
        P.op("vector", lambda e: e.tensor_copy(out=aT[:], in_=pT[:]), reads=[b_pT], writes=[b_aT])
        for ci, (c0, cw) in enumerate(chunks):
            zp = zps[ci % 3]; bz = b_zps[ci % 3]
            for kt in range(8):
                P.op("tensor", lambda e, kt=kt, zp=zp, c0=c0, cw=cw: e.matmul(zp[:, 0:cw], lhsT=aT[:, kt, :], rhs=W[:, kt, c0:c0 + cw], start=(kt == 0), stop=(kt == 7)), reads=[b_aT, b_W], writes=[bz], accum=True)
            if ci % 2 == 0:
                P.op("scalar", lambda e, zp=zp, c0=c0, cw=cw: e.copy(out=z[:, c0:c0 + cw], in_=zp[:, 0:cw]), reads=[bz], writes=[b_z[ci]])
            else:
                P.op("vector", lambda e, zp=zp, c0=c0, cw=cw: e.tensor_copy(out=z[:, c0:c0 + cw], in_=zp[:, 0:cw]), reads=[bz], writes=[b_z[ci]])
        P.op("gpsimd", lambda e: e.tensor_copy(out=qkv[k][:], in_=z[:, 0:1280]), reads=[b_z[0], b_z[1], b_z[2]], writes=[b_qkv[k]])
        zr = z[:, 0:768].rearrange("p (h d) -> p h d", d=64)
        qr = qkv[k][:, 0:768].rearrange("p (h d) -> p h d", d=64)
        cb = cos[:, ti, :].unsqueeze(1).to_broadcast([128, 12, 8]); sb_ = sin[:, ti, :].unsqueeze(1).to_broadcast([128, 12, 8])
        x1 = zr[:, :, 0:8]; x2 = zr[:, :, 8:16]
        P.op("vector", lambda e: e.tensor_tensor(out=rt[:, 0], in0=x1, in1=cb, op=ALU.mult), reads=[b_z[0], b_z[1], b_c], writes=[b_rt])
        P.op("vector", lambda e: e.tensor_tensor(out=rt[:, 1], in0=x2, in1=sb_, op=ALU.mult), reads=[b_z[0], b_z[1], b_c], writes=[b_rt])
        P.op("vector", lambda e: e.tensor_tensor(out=rt[:, 2], in0=x2, in1=cb, op=ALU.mult), reads=[b_z[0], b_z[1], b_c], writes=[b_rt])
        P.op("vector", lambda e: e.tensor_tensor(out=rt[:, 3], in0=x1, in1=sb_, op=ALU.mult), reads=[b_z[0], b_z[1], b_c], writes=[b_rt])
        P.op("vector", lambda e: e.tensor_tensor(out=qr[:, :, 0:8], in0=rt[:, 0], in1=rt[:, 1], op=ALU.subtract), reads=[b_rt], writes=[b_qkv[k]])
        P.op("vector", lambda e: e.tensor_tensor(out=qr[:, :, 8:16], in0=rt[:, 2], in1=rt[:, 3], op=ALU.add), reads=[b_rt], writes=[b_qkv[k]])
        P.dma("sync", lambda e: e.dma_start(out=qkv_o[ti * 128:(ti + 1) * 128, :], in_=qkv[k][:]), f"so{k}", reads=[b_qkv[k]])
        P.op("scalar", lambda e: e.activation(out=gt[k][:], in_=z[:, 1280:1304], func=AF.Sigmoid), reads=[b_z[2]], writes=[b_gt[k]])
        P.dma("sync", lambda e: e.dma_start(out=gates_o[ti * 128:(ti + 1) * 128, :], in_=gt[k][:]), f"so{k}", reads=[b_gt[k]])
        P.op("scalar", lambda e: e.activation(out=u[:], in_=z[:, 1304:1816], func=AF.Gelu_apprx_tanh), reads=[b_z[2], b_z[3]], writes=[b_u])
        P.op("scalar", lambda e: e.activation(out=v[:], in_=z[:, 1816:2328], func=AF.Gelu_apprx_tanh), reads=[b_z[3], b_z[4]], writes=[b_v])
        P.op("vector", lambda e: e.bn_stats(out=bnst[:], in_=v[:]), reads=[b_v], writes=[b_bn])
        P.op("vector", lambda e: e.bn_aggr(out=mv[:, 0:2], in_=bnst[:]), reads=[b_bn], writes=[b_mv])
        P.op("scalar", lambda e: e.activation(out=mv[:, 2:3], in_=mv[:, 1:2], func=AF.Sqrt, scale=1.0, bias=EPS), reads=[b_mv], writes=[b_mv])
        P.op("vector", lambda e: e.reciprocal(out=mv[:, 3:4], in_=mv[:, 2:3]), reads=[b_mv], writes=[b_mv])
        P.op("vector", lambda e: e.tensor_scalar(out=v[:], in0=v[:], scalar1=mv[:, 0:1], scalar2=mv[:, 3:4], op0=ALU.subtract, op1=ALU.mult), reads=[b_v, b_mv], writes=[b_v])
        P.op("gpsimd", lambda e: e.tensor_tensor(out=v[:], in0=v[:], in1=lng[:], op=ALU.mult), reads=[b_v, b_c], writes=[b_v])
        P.op("gpsimd", lambda e: e.tensor_tensor(out=vn[:], in0=v[:], in1=lnb[:], op=ALU.add), reads=[b_v, b_c], writes=[b_vn])
        for g in range(8):
            P.op("tensor", lambda e, g=g: e.matmul(mixps[:, g * 64:(g + 1) * 64], lhsT=wsT[:, g, :], rhs=vn[:, g * 64:(g + 1) * 64], start=True, stop=True), reads=[b_ws, b_vn], writes=[b_mix], accum=True)
        P.op("vector", lambda e: e.tensor_tensor(out=m1[:].rearrange("p (g d) -> p g d", d=64), in0=mixps[:].rearrange("p (g d) -> p g d", d=64), in1=bsT[:].unsqueeze(2).to_broadcast([128, 8, 64]), op=ALU.add), reads=[b_mix, b_c], writes=[b_m1])
        P.op("vector", lambda e: e.tensor_tensor(out=m1[:], in0=m1[:], in1=u[:], op=ALU.mult), reads=[b_m1, b_u], writes=[b_m1])
        P.op("scalar", lambda e: e.activation(out=junk[:, 0:512], in_=m1[:], func=AF.Square, accum_out=s_[:, 4:5]), reads=[b_m1], writes=[b_junk, b_st[k]])
        P.op("scalar", lambda e: e.activation(out=s_[:, 5:6], in_=s_[:, 4:5], func=AF.Sqrt, scale=1.0 / 512, bias=EPS), reads=[b_st[k]], writes=[b_st[k]])
        P.op("vector", lambda e: e.reciprocal(out=s_[:, 6:7], in_=s_[:, 5:6]), reads=[b_st[k]], writes=[b_st[k]])
        P.op("vector", lambda e: e.scalar_tensor_tensor(out=mlpn[k][:], in0=m1[:], scalar=s_[:, 6:7], in1=gmlp[:], op0=ALU.mult, op1=ALU.mult), reads=[b_m1, b_st[k], b_c], writes=[b_mlpn[k]])
        P.dma("sync", lambda e: e.dma_start(out=mlpn_o[ti * 128:(ti + 1) * 128, :], in_=mlpn[k][:]), f"so{k}", reads=[b_mlpn[k]])
    load(0)
    for ti in range(NTILE):
        do_tile(ti)
    P.emit()
    return nc


S = 16384
NEGM = -30000.0
SCALE = 0.125

def slot_qi(c, s):
    j = s // 2
    return 16 * j + c if s % 2 == 0 else 16 * j + 15 - c

def build_B(nslots=16, debug=False):
    nc = bass.Bass("TRN2", target_bir_lowering=False)
    din = lambda n, sh, dt=F32: nc.dram_tensor(n, sh, dt, kind="ExternalInput").ap()
    dout = lambda n, sh, dt=F32: nc.dram_tensor(n, sh, dt, kind="ExternalOutput").ap()
    QT_d = din("QT", [nslots, 128, 512], BF16)
    KsT_d = din("KsT", [128, S], BF16)
    Vs_d = din("Vs", [128, 128 * 2 * 65], BF16)
    KwT_d = din("KwT", [nslots, 128, 640], BF16)
    Vw_d = din("Vw", [nslots, 128, 5 * 2 * 65], BF16)
    F_d = din("F", [2, 2, 4, 128, 16 * 256], BF16)
    gates_d = din("gates", [nslots, 128, 24])
    sbias_d = din("sbias", [nslots, 128, 256])
    msk_d = din("msk", [nslots, 128, 15 * 128], BF16)
    w1_d = din("w1", [2, 128, 16 * 256], BF16)
    w2_d = din("w2", [2, 128, 2 * 64], BF16)
    b1T_d = din("b1T", [2, 128, 2])
    peT_d = din("peT", [2, 128, 16], BF16)
    b2_d = din("b2", [2, 128, 64])
    cosc_d = din("cosc", [128, 64]); sinc_d = din("sinc", [128, 64])
    ov_d = din("ov", [128, 8 * 256], BF16)
    ind_d = din("ind", [128, 64 * 128], BF16)
    id_d = din("ident", [128, 128], BF16)
    attn_o = dout("attn", [nslots * 128, 512])
    dbg_o = dout("dbg", [nslots * 128, 3 * 512]) if debug else None
    P = Prog(nc)
    KsT = P.sb("KsT", [128, S], BF16); b_KsT = P.buf()
    Vs = P.sb("Vs", [128, 128, 2, 65], BF16); b_Vs = P.buf()
    IndAll = P.sb("IndAll", [128, 64, 128], BF16)
    ov = P.sb("ov", [128, 8, 256], BF16)
    ident = P.sb("ident", [128, 128], BF16)
    cosc = P.sb("cosc", [128, 8, 8], F32); sinc = P.sb("sinc", [128, 8, 8], F32)
    b_c = P.buf()
    w1 = P.sb("w1", [128, 2, 16, 256], BF16); w2 = P.sb("w2", [128, 2, 2, 64], BF16)
    b1T = P.sb("b1T", [128, 2, 2], F32); peT = P.sb("peT", [128, 2, 16], BF16); b2 = P.sb("b2", [128, 2, 64], F32)
    b_cw = P.buf()
    KcT = P.sb("KcT", [128, 1024], BF16); b_KcT = P.buf()
    Vc = P.sb("Vc", [128, 8, 2, 65], BF16); b_Vc = P.buf()
    for X in range(2):
        P.dma("sync", lambda e, X=X: e.dma_start(out=w1[:, X].rearrange("p a b -> p (a b)"), in_=w1_d[X]), "cc", writes=[b_cw])
        P.dma("sync", lambda e, X=X: e.dma_start(out=w2[:, X].rearrange("p a b -> p (a b)"), in_=w2_d[X]), "cc", writes=[b_cw])
        P.dma("sync", lambda e, X=X: e.dma_start(out=b1T[:, X], in_=b1T_d[X]), "cc", writes=[b_cw])
        P.dma("sync", lambda e, X=X: e.dma_start(out=peT[:, X], in_=peT_d[X]), "cc", writes=[b_cw])
        P.dma("sync", lambda e, X=X: e.dma_start(out=b2[:, X], in_=b2_d[X]), "cc", writes=[b_cw])
    P.dma("sync", lambda e: e.dma_start(out=ident[:], in_=id_d), "cc", writes=[b_c])
    P.dma("sync", lambda e: e.dma_start(out=cosc[:].rearrange("p a b -> p (a b)"), in_=cosc_d), "cc", writes=[b_c])
    P.dma("sync", lambda e: e.dma_start(out=sinc[:].rearrange("p a b -> p (a b)"), in_=sinc_d), "cc", writes=[b_c])
    P.dma("sync", lambda e: e.dma_start(out=ov[:].rearrange("p a b -> p (a b)"), in_=ov_d), "cc", writes=[b_c])
    P.dma("gpsimd", lambda e: e.dma_start(out=IndAll[:].rearrange("p a b -> p (a b)"), in_=ind_d), "cc2", writes=[b_c])
    for q4 in range(4):
        P.dma("gpsimd", lambda e, q4=q4: e.dma_start(out=KsT[:, q4 * 4096:(q4 + 1) * 4096], in_=KsT_d[:, q4 * 4096:(q4 + 1) * 4096]), "cc2", writes=[b_KsT])
        P.dma("gpsimd", lambda e, q4=q4: e.dma_start(out=Vs[:, q4 * 32:(q4 + 1) * 32].rearrange("p a b c -> p (a b c)"), in_=Vs_d[:, q4 * 32 * 130:(q4 + 1) * 32 * 130]), "cc2", writes=[b_Vs])
    sTs = [P.ps(f"sT{i}", [128, 512], F32) for i in range(3)]; b_sT = [P.buf() for _ in range(3)]
    oaccs = [P.ps(f"oacc{i}", [128, 4, 128], F32) for i in range(2)]; b_oacc = [P.buf() for _ in range(2)]
    imps = [P.ps(f"imp{i}", [128, 2, 256], F32) for i in range(2)]; b_imp = P.buf()
    tp = P.ps("tp", [128, 2, 128], BF16); b_tp = P.buf()
    cps = sTs[2][:, 0:64]; b_cps = b_sT[2]
    b1ps = sTs[2][:, 64:66]; b_b1ps = b_sT[2]
    Fb = [P.sb(f"Fb{i}", [128, 16, 256], BF16) for i in range(2)]; b_Fb = [P.buf() for _ in range(2)]
    hT = P.sb("hT", [128, 2, 256], BF16); b_hT = [P.buf() for _ in range(2)]
    bias1 = P.sb("bias1", [128, 2, 2], F32); b_bias1 = P.buf()
    kcf = P.sb("kcf", [128, 8, 2, 64], F32); b_kcf = P.buf()
    kcb = P.sb("kcb", [128, 8, 2, 64], BF16); b_kcb = P.buf()
    rt = P.sb("rt", [128, 4, 8, 2, 8], F32); b_rt = P.buf()
    P.op("vector", lambda e: e.memset(Vc[:], 1.0), writes=[b_Vc])
    fi = 0
    for X in range(2):
        for hc in range(2):
            for jp in range(16):
                P.op("tensor", lambda e, X=X, hc=hc, jp=jp: e.matmul(sTs[2][:, 64 + hc:65 + hc], lhsT=w1[:, X, jp, hc * 128:(hc + 1) * 128], rhs=peT[:, X, jp:jp + 1], start=(jp == 0), stop=(jp == 15)), reads=[b_cw], writes=[b_b1ps], accum=True)
        P.op("vector", lambda e, X=X: e.tensor_tensor(out=bias1[:, X], in0=b1ps, in1=b1T[:, X], op=ALU.add), reads=[b_b1ps, b_cw], writes=[b_bias1])
        for g in range(2):
            for nh in range(4):
                fb = Fb[fi % 2]; bfb = b_Fb[fi % 2]
                P.dma("sync", lambda e, fb=fb, X=X, g=g, nh=nh: e.dma_start(out=fb[:].rearrange("p a b -> p (a b)"), in_=F_d[X, g, nh]), f"F{fi%2}", writes=[bfb])
                fi += 1
                for hc in range(2):
                    sT = sTs[hc]
                    for jp in range(16):
                        P.op("tensor", lambda e, X=X, hc=hc, jp=jp, fb=fb, sT=sT: e.matmul(sT[:, 0:256], lhsT=w1[:, X, jp, hc * 128:(hc + 1) * 128], rhs=fb[:, jp, :], start=(jp == 0), stop=(jp == 15)), reads=[b_cw, bfb], writes=[b_sT[hc]], accum=True)
                    P.op("scalar", lambda e, X=X, hc=hc, sT=sT: e.activation(out=hT[:, hc, :], in_=sT[:, 0:256], func=AF.Gelu_apprx_tanh, bias=bias1[:, X, hc:hc + 1], scale=1.0), reads=[b_sT[hc], b_bias1], writes=[b_hT[hc]])
                for ntl in range(2):
                    nt = nh * 2 + ntl
                    for hc in range(2):
                        P.op("tensor", lambda e, X=X, hc=hc, ntl=ntl: e.matmul(cps, lhsT=hT[:, hc, ntl * 128:(ntl + 1) * 128], rhs=w2[:, X, hc, :], start=(hc == 0), stop=(hc == 1)), reads=[b_hT[hc], b_cw], writes=[b_cps], accum=True)
                    if X == 1:
                        P.op("vector", lambda e, nt=nt, g=g: e.tensor_tensor(out=Vc[:, nt, g, 0:64], in0=cps, in1=b2[:, 1], op=ALU.add), reads=[b_cps, b_cw], writes=[b_Vc])
                    else:
                        P.op("vector", lambda e, nt=nt, g=g: e.tensor_tensor(out=kcf[:, nt, g, :], in0=cps, in1=b2[:, 0], op=ALU.add), reads=[b_cps, b_cw], writes=[b_kcf])
        if X == 0:
            P.op("gpsimd", lambda e: e.tensor_copy(out=kcb[:], in_=kcf[:]), reads=[b_kcf], writes=[b_kcb])
            cb = cosc[:].unsqueeze(2).to_broadcast([128, 8, 2, 8]); sb_ = sinc[:].unsqueeze(2).to_broadcast([128, 8, 2, 8])
            x1 = kcf[:, :, :, 0:8]; x2 = kcf[:, :, :, 8:16]
            P.op("vector", lambda e: e.tensor_tensor(out=rt[:, 0], in0=x1, in1=cb, op=ALU.mult), reads=[b_kcf, b_c], writes=[b_rt])
            P.op("vector", lambda e: e.tensor_tensor(out=rt[:, 1], in0=x2, in1=sb_, op=ALU.mult), reads=[b_kcf, b_c], writes=[b_rt])
            P.op("vector", lambda e: e.tensor_tensor(out=rt[:, 2], in0=x2, in1=cb, op=ALU.mult), reads=[b_kcf, b_c], writes=[b_rt])
            P.op("vector", lambda e: e.tensor_tensor(out=rt[:, 3], in0=x1, in1=sb_, op=ALU.mult), reads=[b_kcf, b_c], writes=[b_rt])
            P.op("vector", lambda e: e.tensor_tensor(out=kcb[:, :, :, 0:8], in0=rt[:, 0], in1=rt[:, 1], op=ALU.subtract), reads=[b_rt], writes=[b_kcb])
            P.op("vector", lambda e: e.tensor_tensor(out=kcb[:, :, :, 8:16], in0=rt[:, 2], in1=rt[:, 3], op=ALU.add), reads=[b_rt], writes=[b_kcb])
            for nt in range(8):
                P.op("tensor", lambda e, nt=nt: e.transpose(out=tp[:, 0, :], in_=kcb[:, nt].rearrange("p a b -> p (a b)"), identity=ident[:]), reads=[b_kcb, b_c], writes=[b_tp])
                P.op("vector", lambda e, nt=nt: e.tensor_copy(out=KcT[:, nt * 128:(nt + 1) * 128], in_=tp[:, 0, :]), reads=[b_tp], writes=[b_KcT])
    QT = [P.sb(f"QT{i}", [128, 512], BF16) for i in range(2)]
    gts = [P.sb(f"gts{i}", [128, 8, 3], F32) for i in range(2)]
    sbias = [P.sb(f"sbias{i}", [128, 256], F32) for i in range(2)]
    msk = [P.sb(f"msk{i}", [128, 15, 128], BF16) for i in range(2)]
    KwT = [P.sb(f"KwT{i}", [128, 640], BF16) for i in range(2)]
    Vw = [P.sb(f"Vw{i}", [128, 5, 2, 65], BF16) for i in range(2)]
    b_sl = [P.buf() for _ in range(2)]
    eT = P.sb("eT", [128, 8, 512], BF16); b_eT = [P.buf() for _ in range(8)]
    pTs = [P.sb(f"pT{i}", [128, 512], BF16) for i in range(4)]; b_pT = [P.buf() for _ in range(4)]
    nsT4 = P.sb("nsT4", [128, 2, 4, 128], BF16); b_ns = P.buf()
    score = P.sb("score", [128, 256], F32); b_score = P.buf()
    sc2 = P.sb("sc2", [128, 256], F32); b_sc2 = P.buf()
    m8 = P.sb("m8", [128, 16], F32); b_m8 = P.buf()
    rd = P.sb("rd", [128, 4], F32); b_rd = P.buf()
    negsel = P.sb("negsel", [128, 256], BF16); b_negsel = P.buf()
    wcs = [P.sb(f"wc{i}", [128, 4], F32) for i in range(3)]; b_wc = [P.buf() for _ in range(3)]
    acc = [P.sb(f"acc{i}", [128, 8, 64], F32) for i in range(2)]; b_acc = [P.buf() for _ in range(2)]
    cnt = {"sT": 0, "pT": 0, "oa": 0}

    def load_slot(s):
        k2 = s % 2
        w = [b_sl[k2]]
        st = f"sl{k2}"
        P.dma("sync", lambda e: e.dma_start(out=QT[k2][:], in_=QT_d[s]), st, writes=w)
        P.dma("sync", lambda e: e.dma_start(out=gts[k2][:].rearrange("p a b -> p (a b)"), in_=gates_d[s]), st, writes=w)
        P.dma("sync", lambda e: e.dma_start(out=sbias[k2][:], in_=sbias_d[s]), st, writes=w)
        P.dma("sync", lambda e: e.dma_start(out=msk[k2][:].rearrange("p a b -> p (a b)"), in_=msk_d[s]), st, writes=w)
        P.dma("sync", lambda e: e.dma_start(out=KwT[k2][:], in_=KwT_d[s]), st, writes=w)
        P.dma("sync", lambda e: e.dma_start(out=Vw[k2][:].rearrange("p a b c -> p (a b c)"), in_=Vw_d[s]), st, writes=w)

    def branch(k2, g, tiles, lhs_fn, lhs_bufs, v_fn, v_bufs, mask_fn, pbufs, on_exp=None):
        gp = slice(g * 64, (g + 1) * 64)
        oi = cnt["oa"] % 2; cnt["oa"] += 1
        oacc = oaccs[oi]; boacc = b_oacc[oi]
        n = len(tiles)
        sbank = {}

        def S(i):
            t = tiles[i]
            bi = cnt["sT"] % 3; cnt["sT"] += 1
            sbank[i] = bi
            sT = sTs[bi]
            extra = mask_fn(t)
            l0 = lhs_fn(t); rq = QT[k2][gp, :]
            P.op("tensor", lambda e: e.matmul(sT[:], lhsT=l0, rhs=rq, start=True, stop=(len(extra) == 0)), reads=[b_sl[k2]] + lhs_bufs, writes=[b_sT[bi]])
            for xi, (kind, l_ap, r_ap, rb) in enumerate(extra):
                last = xi == len(extra) - 1
                if kind == "full":
                    P.op("tensor", lambda e, l_ap=l_ap, r_ap=r_ap, last=last: e.matmul(sT[:], lhsT=l_ap, rhs=r_ap, start=False, stop=last), reads=rb, writes=[b_sT[bi]], accum=True)
                else:
                    for h in range(4):
                        P.op("tensor", lambda e, l_ap=l_ap, r_ap=r_ap, last=last, h=h: e.matmul(sT[:, h * 128:(h + 1) * 128], lhsT=l_ap, rhs=r_ap, start=False, stop=(last and h == 3)), reads=rb, writes=[b_sT[bi]], accum=True)

        def E(i):
            t = tiles[i]
            bi = sbank[i]
            p_ap, p_b = pbufs(i)
            P.op("scalar", lambda e: e.activation(out=p_ap, in_=sTs[bi][:], func=AF.Exp, scale=SCALE), reads=[b_sT[bi]], writes=[p_b])

        def V(i):
            t = tiles[i]
            p_ap, p_b = pbufs(i)
            v0 = v_fn(t)
            for h in range(4):
                P.op("tensor", lambda e, h=h: e.matmul(oacc[:, h, 0:65], lhsT=p_ap[:, h * 128:(h + 1) * 128], rhs=v0, start=(i == 0 and h == 0), stop=(i == n - 1 and h == 3), skip_group_check=True), reads=[p_b] + v_bufs, writes=[boacc], accum=(i > 0 or h > 0))
            if on_exp is not None:
                on_exp(i, t, p_ap, p_b)

        for i in range(min(2, n)):
            S(i)
        for i in range(n):
            E(i)
            if i + 2 < n:
                S(i + 2)
            V(i)
        return oacc, boacc

    def finalize(k2, g, br, oacc, boacc, ak):
        wc = wcs[br]; bwc = b_wc[br]
        P.op("vector", lambda e: e.tensor_scalar(out=wc[:], in0=oacc[:, :, 64], scalar1=1e-30, scalar2=None, op0=ALU.max), reads=[boacc], writes=[bwc])
        P.op("vector", lambda e: e.reciprocal(out=wc[:], in_=wc[:]), reads=[bwc], writes=[bwc])
        if debug:
            P.op("vector", lambda e: e.tensor_tensor(out=dbg_t[:, br, 4 * g:4 * g + 4, :], in0=oacc[:, :, 0:64], in1=wc[:].unsqueeze(2).to_broadcast([128, 4, 64]), op=ALU.mult), reads=[boacc, bwc], writes=[b_dbg])
        P.op("vector", lambda e: e.tensor_tensor(out=wc[:], in0=wc[:], in1=gts[k2][:, 4 * g:4 * g + 4, br], op=ALU.mult), reads=[bwc, b_sl[k2]], writes=[bwc])
        dst = acc[ak][:, 4 * g:4 * g + 4, :]
        wb = wc[:].unsqueeze(2).to_broadcast([128, 4, 64])
        if br == 0:
            P.op("vector", lambda e: e.tensor_tensor(out=dst, in0=oacc[:, :, 0:64], in1=wb, op=ALU.mult), reads=[boacc, bwc], writes=[b_acc[ak]])
        else:
            tmp = acc_tmp
            P.op("vector", lambda e: e.tensor_tensor(out=tmp[:], in0=oacc[:, :, 0:64], in1=wb, op=ALU.mult), reads=[boacc, bwc], writes=[b_acctmp])
            P.op("gpsimd", lambda e: e.tensor_tensor(out=dst, in0=dst, in1=tmp[:], op=ALU.add), reads=[b_acctmp, b_acc[ak]], writes=[b_acc[ak]])

    acc_tmp = P.sb("acc_tmp", [128, 4, 64], F32); b_acctmp = P.buf()
    dbg_t = P.sb("dbg_t", [128, 3, 8, 64], F32); b_dbg = P.buf()

    def do_slot(s):
        k2 = s % 2; j = s // 2
        KT = 16 * j + 8 if s % 2 == 0 else 16 * j + 16
        rag0 = KT - 8
        if s + 1 < nslots:
            load_slot(s + 1)
        for g in range(2):
            gp = slice(g * 64, (g + 1) * 64)
            def cmask(nt):
                if nt >= j - 1:
                    mi = nt - (j - 1)
                    return [("head", ident[:], msk[k2][:, mi, :], [b_c, b_sl[k2]])]
                return []

            def imp_mm(i, nt, p_ap, p_b, nn=j + 1):
                for h in range(4):
                    P.op("tensor", lambda e, h=h: e.matmul(imps[h // 2][:, h % 2, :], lhsT=p_ap[:, h * 128:(h + 1) * 128], rhs=ov[:, nt, :], start=(i == 0 and h % 2 == 0), stop=(i == nn - 1 and h % 2 == 1), skip_group_check=True), reads=[p_b, b_c], writes=[b_imp], accum=(i > 0 or h > 0))

            oacc, boacc = branch(k2, g, list(range(j + 1)), lambda nt: KcT[gp, nt * 128:(nt + 1) * 128], [b_KcT], lambda nt: Vc[:, nt, g, :], [b_Vc], cmask,
                                 lambda i: (eT[:, i, :], b_eT[i]), on_exp=imp_mm)
            P.op("vector", lambda e: e.tensor_scalar(out=rd[:, 0:2], in0=imps[0][:, :, 255], scalar1=1e-30, scalar2=None, op0=ALU.max), reads=[b_imp], writes=[b_rd])
            P.op("vector", lambda e: e.tensor_scalar(out=rd[:, 2:4], in0=imps[1][:, :, 255], scalar1=1e-30, scalar2=None, op0=ALU.max), reads=[b_imp], writes=[b_rd])
            P.op("vector", lambda e: e.reciprocal(out=rd[:], in_=rd[:]), reads=[b_rd], writes=[b_rd])
            P.op("vector", lambda e: e.scalar_tensor_tensor(out=score[:], in0=imps[0][:, 0, :], scalar=rd[:, 0:1], in1=sbias[k2][:], op0=ALU.mult, op1=ALU.add), reads=[b_imp, b_rd, b_sl[k2]], writes=[b_score])
            for h in range(1, 4):
                P.op("vector", lambda e, h=h: e.scalar_tensor_tensor(out=score[:], in0=imps[h // 2][:, h % 2, :], scalar=rd[:, h:h + 1], in1=score[:], op0=ALU.mult, op1=ALU.add), reads=[b_imp, b_rd, b_score], writes=[b_score])
            P.op("vector", lambda e: e.max(out=m8[:, 0:8], in_=score[:]), reads=[b_score], writes=[b_m8])
            P.op("vector", lambda e: e.match_replace(out=sc2[:], in_to_replace=m8[:, 0:8], in_values=score[:], imm_value=-3e38), reads=[b_score, b_m8], writes=[b_sc2])
            P.op("vector", lambda e: e.max(out=m8[:, 8:16], in_=sc2[:]), reads=[b_sc2], writes=[b_m8])
            P.op("vector", lambda e: e.tensor_scalar(out=negsel[:], in0=score[:], scalar1=m8[:, 15:16], scalar2=NEGM, op0=ALU.is_lt, op1=ALU.mult), reads=[b_score, b_m8], writes=[b_negsel])
            for hf in range(2):
                P.op("tensor", lambda e, hf=hf: e.transpose(out=tp[:, hf, :], in_=negsel[:, hf * 128:(hf + 1) * 128], identity=ident[:]), reads=[b_negsel, b_c], writes=[b_tp], accum=(hf == 1))
            P.op("vector", lambda e: e.tensor_copy(out=nsT4[:], in_=tp[:].unsqueeze(2).to_broadcast([128, 2, 4, 128])), reads=[b_tp], writes=[b_ns])
            finalize(k2, g, 0, oacc, boacc, k2)
            oacc, boacc = branch(k2, g, list(range(5)), lambda w: KwT[k2][gp, w * 128:(w + 1) * 128], [], lambda w: Vw[k2][:, w, g, :], [b_sl[k2]],
                                 lambda w: [("head", ident[:], msk[k2][:, 10 + w, :], [b_c, b_sl[k2]])],
                                 lambda i: (pTs[cnt_p(i)][:], b_pT[cnt_p(i)]))
            finalize(k2, g, 2, oacc, boacc, k2)
            def smask(kt):
                ex = [("full", IndAll[:, kt % 64, :], nsT4[:, kt // 64].rearrange("p a b -> p (a b)"), [b_c, b_ns])]
                if kt >= rag0:
                    ex.append(("head", ident[:], msk[k2][:, 2 + kt - rag0, :], [b_c, b_sl[k2]]))
                return ex
            oacc, boacc = branch(k2, g, list(range(KT)), lambda kt: KsT[gp, kt * 128:(kt + 1) * 128], [b_KsT], lambda kt: Vs[:, kt, g, :], [b_Vs], smask,
                                 lambda i: (pTs[cnt_p(i)][:], b_pT[cnt_p(i)]))
            finalize(k2, g, 1, oacc, boacc, k2)
        P.dma("sync", lambda e: e.dma_start(out=attn_o[s * 128:(s + 1) * 128, :], in_=acc[k2][:].rearrange("p a b -> p (a b)")), f"ao{k2}", reads=[b_acc[k2]])
        if debug:
            P.dma("sync", lambda e: e.dma_start(out=dbg_o[s * 128:(s + 1) * 128, :], in_=dbg_t[:].rearrange("p a b c -> p (a b c)")), "dbg", reads=[b_dbg])

    def cnt_p(i):
        return i % 4

    load_slot(0)
    for s in range(nslots):
        do_slot(s)
    P.emit()
    return nc


D = 1024; TPC = 2048; DFF = 2816
EPS = 1e-6
CH = 512
NCH = TPC // CH
TPCH = CH // 128

def build_C(halo=True, nchunks=NCH, stages=(1, 2, 3), v=0):
    nc = bass.Bass("TRN2", target_bir_lowering=False)
    din = lambda n, sh, dt=F32: nc.dram_tensor(n, sh, dt, kind="ExternalInput").ap()
    dout = lambda n, sh, dt=F32: nc.dram_tensor(n, sh, dt, kind="ExternalOutput").ap()
    h_d = din("h", [TPC + 128, D])
    attn_d = din("attn", [TPC + 128, 512])
    mlpn_d = din("mlpn", [TPC + 128, 512], BF16)
    p_d = din("p", [TPC, 256])
    gattn_d = din("gattn", [128, 512]); gpost_d = din("gpost", [128, D]); gpre_d = din("gpre", [128, D])
    gpffn_d = din("gpffn", [128, D]); gple_d = din("gple", [128, D])
    conv_d = din("conv", [128, 44 * 4])
    wo_d = din("wo", [128, 8 * D], BF16)
    wup_d = din("wup", [22, 128, 2 * 8 * 128], BF16)
    wdn_d = din("wdn", [128, 22 * D], BF16)
    wg_d = din("wg", [128, 8 * D], BF16)
    wp_d = din("wp", [128, 2 * D], BF16)
    id_d = din("ident", [128, 128], BF16)
    out_d = dout("hout", [TPC, D])
    P = Prog(nc)
    wo = P.sb("wo", [128, 8, D], BF16); wdn = P.sb("wdn", [128, 22, D], BF16); wg = P.sb("wg", [128, 8, D], BF16); wp = P.sb("wp", [128, 2, D], BF16)
    b_w = P.buf()
    gattn = P.sb("gattn", [128, 512], F32); gpost = P.sb("gpost", [128, D], F32); gpre = P.sb("gpre", [128, D], F32)
    gpffn = P.sb("gpffn", [128, D], F32); gple = P.sb("gple", [128, D], F32)
    conv = P.sb("conv", [128, 44, 4], F32); ident = P.sb("ident", [128, 128], BF16)
    b_c = P.buf()
    for (t, d_, q) in [(wo, wo_d, "sync"), (wdn, wdn_d, "gpsimd"), (wg, wg_d, "sync"), (wp, wp_d, "gpsimd")]:
        P.dma(q, lambda e, t=t, d_=d_: e.dma_start(out=t[:].rearrange("p a b -> p (a b)"), in_=d_), "cw" + q, writes=[b_w])
    for (t, d_) in [(gattn, gattn_d), (gpost, gpost_d), (gpre, gpre_d), (gpffn, gpffn_d), (gple, gple_d), (ident, id_d)]:
        P.dma("sync", lambda e, t=t, d_=d_: e.dma_start(out=t[:], in_=d_), "cc", writes=[b_c])
    P.dma("sync", lambda e: e.dma_start(out=conv[:].rearrange("p a b -> p (a b)"), in_=conv_d), "cc", writes=[b_c])
    psT = P.ps("psT", [128, 8, 128], BF16); b_psT = P.buf()
    M = [P.ps(f"M{i}", [128, 512], F32) for i in range(2)]; b_M = P.buf()
    U = [P.ps(f"U{i}", [128, 512], F32) for i in range(4)]; b_U = [P.buf() for _ in range(4)]
    h1c = P.sb("h1c", [128, TPCH, D], F32); b_h1 = [P.buf() for _ in range(TPCH)]
    hh = P.sb("hh", [128, D], F32); b_hh = P.buf()
    hnT = P.sb("hnT", [128, 8, CH], BF16); b_hnT = [P.buf() for _ in range(TPCH)]
    hnTh = P.sb("hnTh", [128, 8, 2], BF16); b_hnTh = P.buf()
    actT = P.sb("actT", [128, 22, CH], BF16); b_actT = [P.buf() for _ in range(22)]
    carry = P.sb("carry", [128, 44, 2], F32); b_carry = [P.buf() for _ in range(22)]
    hup = [[P.sb(f"hup{s}{x}", [128, CH + 2], F32) for x in range(2)] for s in range(2)]; b_hup = [[P.buf() for x in range(2)] for s in range(2)]
    cgu = [[P.sb(f"cgu{s}{x}", [128, CH], F32) for x in range(2)] for s in range(2)]; b_cgu = [[P.buf() for x in range(2)] for s in range(2)]
    wub = [P.sb(f"wub{i}", [128, 2, 8, 128], BF16) for i in range(2)]; b_wub = [P.buf() for _ in range(2)]
    att = [P.sb(f"att{i}", [128, 512], F32) for i in range(2)]; mlb = [P.sb(f"mlb{i}", [128, 512], BF16) for i in range(2)]; b_in = [P.buf() for _ in range(2)]
    pin = [P.sb(f"pin{i}", [128, 256], F32) for i in range(2)]; b_pin = [P.buf() for _ in range(2)]
    xb = P.sb("xb", [128, D], BF16); b_xb = P.buf()
    xT = P.sb("xT", [128, 8, 128], BF16); b_xT = P.buf()
    pb = P.sb("pb", [128, 256], BF16); b_pb = P.buf()
    pT = P.sb("pT", [128, 2, 128], BF16); b_pT = P.buf()
    tmp = P.sb("tmp", [128, D], F32); b_tmp = P.buf()
    junk = P.sb("junk", [128, D], BF16); b_junk = P.buf()
    st = P.sb("st", [128, 16], F32); b_st = P.buf()
    cnt = {"w": 0, "in": 0, "p": 0}

    def rstd(src_ap, nparts, width, col, reads):
        P.op("scalar", lambda e: e.activation(out=junk[0:nparts, 0:width], in_=src_ap, func=AF.Square, accum_out=st[0:nparts, col:col + 1]), reads=reads, writes=[b_junk, b_st])
        P.op("scalar", lambda e: e.activation(out=st[0:nparts, col + 1:col + 2], in_=st[0:nparts, col:col + 1], func=AF.Sqrt, scale=1.0 / width, bias=EPS), reads=[b_st], writes=[b_st])
        P.op("vector", lambda e: e.reciprocal(out=st[0:nparts, col + 2:col + 3], in_=st[0:nparts, col + 1:col + 2]), reads=[b_st], writes=[b_st])
        return st[0:nparts, col + 2:col + 3]

    def rstd_psum(nparts, col, reads):
        P.op("scalar", lambda e: e.activation(out=junk[0:nparts, 0:512], in_=M[0][0:nparts, :], func=AF.Square, accum_out=st[0:nparts, col:col + 1]), reads=reads, writes=[b_junk, b_st])
        P.op("scalar", lambda e: e.activation(out=junk[0:nparts, 512:1024], in_=M[1][0:nparts, :], func=AF.Square, accum_out=st[0:nparts, col + 3:col + 4]), reads=reads, writes=[b_junk, b_st])
        P.op("vector", lambda e: e.tensor_tensor(out=st[0:nparts, col:col + 1], in0=st[0:nparts, col:col + 1], in1=st[0:nparts, col + 3:col + 4], op=ALU.add), reads=[b_st], writes=[b_st])
        P.op("scalar", lambda e: e.activation(out=st[0:nparts, col + 1:col + 2], in_=st[0:nparts, col:col + 1], func=AF.Sqrt, scale=1.0 / D, bias=EPS), reads=[b_st], writes=[b_st])
        P.op("vector", lambda e: e.reciprocal(out=st[0:nparts, col + 2:col + 3], in_=st[0:nparts, col + 1:col + 2]), reads=[b_st], writes=[b_st])
        return st[0:nparts, col + 2:col + 3]

    def transposes(src_bf, nparts, nk, dstT, b_dst, reads):
        for k in range(nk):
            P.op("tensor", lambda e, k=k: e.transpose(out=psT[:, k, 0:nparts], in_=src_bf[0:nparts, k * 128:(k + 1) * 128], identity=ident[0:nparts, 0:nparts]), reads=reads + [b_c], writes=[b_psT])
        P.op("vector", lambda e: e.tensor_copy(out=dstT, in_=psT[:, 0:nk, 0:nparts]), reads=[b_psT], writes=[b_dst])

    def mm1024(lhsT_fn, nk, w_t, nparts, reads):
        for nch in range(2):
            for k in range(nk):
                P.op("tensor", lambda e, k=k, nch=nch: e.matmul(M[nch][0:nparts, :], lhsT=lhsT_fn(k), rhs=w_t[:, k, nch * 512:(nch + 1) * 512], start=(k == 0), stop=(k == nk - 1)), reads=reads + [b_w], writes=[b_M])

    def load_in(row0, nparts):
        k = cnt["in"] % 2; cnt["in"] += 1
        P.dma("sync", lambda e: e.dma_start(out=att[k][0:nparts, :], in_=attn_d[row0:row0 + nparts, :]), f"in{k}", writes=[b_in[k]])
        P.dma("sync", lambda e: e.dma_start(out=mlb[k][0:nparts, :], in_=mlpn_d[row0:row0 + nparts, :]), f"in{k}", writes=[b_in[k]])
        return k

    def stage1(h_ap, b_h, row0, nparts, dstT, b_dst):
        k = load_in(row0, nparts)
        r = rstd(att[k][0:nparts, :], nparts, 512, 0, [b_in[k]])
        P.op("vector", lambda e: e.scalar_tensor_tensor(out=xb[0:nparts, 0:512], in0=att[k][0:nparts, :], scalar=r, in1=gattn[0:nparts, :], op0=ALU.mult, op1=ALU.mult), reads=[b_in[k], b_st, b_c], writes=[b_xb])
        P.op("gpsimd", lambda e: e.tensor_copy(out=xb[0:nparts, 512:1024], in_=mlb[k][0:nparts, :]), reads=[b_in[k]], writes=[b_xb])
        transposes(xb, nparts, 8, xT[:, :, 0:nparts], b_xT, [b_xb])
        mm1024(lambda kk: xT[:, kk, 0:nparts], 8, wo, nparts, [b_xT])
        r2 = rstd_psum(nparts, 4, [b_M])
        for nch in range(2):
            P.op("vector", lambda e, nch=nch: e.scalar_tensor_tensor(out=tmp[0:nparts, nch * 512:(nch + 1) * 512], in0=M[nch][0:nparts, :], scalar=r2, in1=gpost[0:nparts, nch * 512:(nch + 1) * 512], op0=ALU.mult, op1=ALU.mult), reads=[b_M, b_st, b_c], writes=[b_tmp])
        P.op("gpsimd", lambda e: e.tensor_tensor(out=h_ap, in0=h_ap, in1=tmp[0:nparts, :], op=ALU.add), reads=[b_tmp, b_h], writes=[b_h])
        r3 = rstd(h_ap, nparts, D, 8, [b_h])
        P.op("vector", lambda e: e.scalar_tensor_tensor(out=xb[0:nparts, :], in0=h_ap, scalar=r3, in1=gpre[0:nparts, :], op0=ALU.mult, op1=ALU.mult), reads=[b_h, b_st, b_c], writes=[b_xb])
        transposes(xb, nparts, 8, dstT, b_dst, [b_xb])

    def load_w(m):
        k = cnt["w"] % 2; cnt["w"] += 1
        P.dma("gpsimd" if k else "sync", lambda e: e.dma_start(out=wub[k][:].rearrange("p a b c -> p (a b c)"), in_=wup_d[m]), f"wu{k}", writes=[b_wub[k]])
        return k

    def stage2_halo():
        pend = load_w(0)
        for m in range(22):
            k = pend
            if m + 1 < 22:
                pend = load_w(m + 1)
            for x in range(2):
                for kt in range(8):
                    P.op("tensor", lambda e, x=x, kt=kt, m=m, k=k: e.matmul(U[0][:, (x * 22 + m) * 2:(x * 22 + m) * 2 + 2], lhsT=wub[k][:, x, kt, :], rhs=hnTh[:, kt, :], start=(kt == 0), stop=(kt == 7), skip_group_check=True), reads=[b_wub[k], b_hnTh], writes=[b_U[0]])
        P.op("vector", lambda e: e.tensor_copy(out=carry[:].rearrange("p a b -> p (a b)"), in_=U[0][:, 0:88]), reads=[b_U[0]], writes=b_carry)

    def stage2():
        pend = load_w(0)
        for m in range(22):
            k = pend
            if m + 1 < 22:
                pend = load_w(m + 1)
            s = m % 2
            for x in range(2):
                ub = U[s * 2 + x]; bub = b_U[s * 2 + x]
                for kt in range(8):
                    P.op("tensor", lambda e, x=x, kt=kt, ub=ub, k=k: e.matmul(ub[:], lhsT=wub[k][:, x, kt, :], rhs=hnT[:, kt, :], start=(kt == 0), stop=(kt == 7)), reads=[b_wub[k]] + b_hnT, writes=[bub])
            for x in range(2):
                ub = U[s * 2 + x]; bub = b_U[s * 2 + x]
                hu = hup[s][x]; bhu = b_hup[s][x]; cg = cgu[s][x]; bcg = b_cgu[s][x]
                ci = x * 22 + m
                P.op("gpsimd", lambda e, hu=hu, ci=ci: e.tensor_copy(out=hu[:, 0:2], in_=carry[:, ci, :]), reads=[b_carry[m]], writes=[bhu])
                P.op("vector", lambda e, hu=hu, ub=ub: e.tensor_copy(out=hu[:, 2:CH + 2], in_=ub[:]), reads=[bub], writes=[bhu])
                if True:
                    P.op("vector", lambda e, cg=cg, ub=ub, ci=ci: e.tensor_scalar(out=cg[:], in0=ub[:], scalar1=conv[:, ci, 2:3], scalar2=conv[:, ci, 3:4], op0=ALU.mult, op1=ALU.add), reads=[bub, b_c], writes=[bcg])
                else:
                    P.op("scalar", lambda e, cg=cg, ub=ub, ci=ci: e.activation(out=cg[:], in_=ub[:], func=AF.Identity, scale=conv[:, ci, 2:3], bias=conv[:, ci, 3:4]), reads=[bub, b_c], writes=[bcg])
                P.op("gpsimd", lambda e, hu=hu, ci=ci: e.tensor_copy(out=carry[:, ci, :], in_=hu[:, CH:CH + 2]), reads=[bhu], writes=[b_carry[m]])
                P.op("vector", lambda e, cg=cg, hu=hu, ci=ci: e.scalar_tensor_tensor(out=cg[:], in0=hu[:, 1:CH + 1], scalar=conv[:, ci, 1:2], in1=cg[:], op0=ALU.mult, op1=ALU.add), reads=[bhu, bcg, b_c], writes=[bcg])
                P.op("vector" if v & 2 else "gpsimd", lambda e, hu=hu, ci=ci: e.tensor_scalar(out=hu[:, 0:CH], in0=hu[:, 0:CH], scalar1=conv[:, ci, 0:1], scalar2=None, op0=ALU.mult), reads=[bhu, bcg, b_c], writes=[bhu])
                P.op("gpsimd", lambda e, cg=cg, hu=hu: e.tensor_tensor(out=cg[:], in0=cg[:], in1=hu[:, 0:CH], op=ALU.add), reads=[bhu, bcg], writes=[bcg])
            cg = cgu[s][0]; cu = cgu[s][1]
            P.op("scalar", lambda e, cg=cg: e.activation(out=cg[:], in_=cg[:], func=AF.Silu), reads=[b_cgu[s][0]], writes=[b_cgu[s][0]])
            P.op("gpsimd", lambda e, cg=cg, cu=cu, m=m: e.tensor_tensor(out=actT[:, m, :], in0=cg[:], in1=cu[:], op=ALU.mult), reads=[b_cgu[s][0], b_cgu[s][1]], writes=[b_actT[m]])

    def load_h(ti, row0):
        P.dma("sync", lambda e: e.dma_start(out=h1c[:, ti, :], in_=h_d[row0:row0 + 128, :]), f"hl{ti%2}", writes=[b_h1[ti]])

    def stage3(ti, row0):
        h_ap = h1c[:, ti, :]; b_h = b_h1[ti]
        kp = cnt["p"] % 2; cnt["p"] += 1
        P.dma("sync", lambda e: e.dma_start(out=pin[kp][:], in_=p_d[row0:row0 + 128, :]), f"pl{kp}", writes=[b_pin[kp]])
        for nch in range(2):
            for m in range(22):
                P.op("tensor", lambda e, m=m, nch=nch: e.matmul(M[nch][:], lhsT=actT[:, m, ti * 128:(ti + 1) * 128], rhs=wdn[:, m, nch * 512:(nch + 1) * 512], start=(m == 0), stop=(m == 21)), reads=[b_actT[m], b_w], writes=[b_M])
        r = rstd_psum(128, 4, [b_M])
        for nch in range(2):
            P.op("vector", lambda e, nch=nch: e.scalar_tensor_tensor(out=tmp[:, nch * 512:(nch + 1) * 512], in0=M[nch][:], scalar=r, in1=gpffn[:, nch * 512:(nch + 1) * 512], op0=ALU.mult, op1=ALU.mult), reads=[b_M, b_st, b_c], writes=[b_tmp])
        P.op("gpsimd", lambda e: e.tensor_tensor(out=h_ap, in0=h_ap, in1=tmp[:], op=ALU.add), reads=[b_tmp, b_h], writes=[b_h])
        r3 = rstd(h_ap, 128, D, 8, [b_h])
        P.op("vector", lambda e: e.scalar_tensor_tensor(out=xb[:], in0=h_ap, scalar=r3, in1=gple[:], op0=ALU.mult, op1=ALU.mult), reads=[b_h, b_st, b_c], writes=[b_xb])
        transposes(xb, 128, 8, xT[:], b_xT, [b_xb])
        mm1024(lambda kk: xT[:, kk, :], 8, wg, 128, [b_xT])
        for nch in range(2):
            P.op("scalar", lambda e, nch=nch: e.activation(out=tmp[:, nch * 512:(nch + 1) * 512], in_=M[nch][:], func=AF.Sigmoid), reads=[b_M], writes=[b_tmp])
        P.op("gpsimd", lambda e: e.tensor_copy(out=pb[:], in_=pin[kp][:]), reads=[b_pin[kp]], writes=[b_pb])
        transposes(pb, 128, 2, pT[:], b_pT, [b_pb])
        for nch in range(2):
            for k in range(2):
                P.op("tensor", lambda e, k=k, nch=nch: e.matmul(U[nch][:], lhsT=pT[:, k, :], rhs=wp[:, k, nch * 512:(nch + 1) * 512], start=(k == 0), stop=(k == 1)), reads=[b_pT, b_w], writes=[b_U[nch]])
            P.op("vector", lambda e, nch=nch: e.tensor_tensor(out=tmp[:, nch * 512:(nch + 1) * 512], in0=tmp[:, nch * 512:(nch + 1) * 512], in1=U[nch][:], op=ALU.mult), reads=[b_tmp, b_U[nch]], writes=[b_tmp])
        P.op("gpsimd", lambda e: e.tensor_tensor(out=h_ap, in0=h_ap, in1=tmp[:], op=ALU.add), reads=[b_tmp, b_h], writes=[b_h])
        P.dma("sync", lambda e: e.dma_start(out=out_d[row0:row0 + 128, :], in_=h_ap), f"so{ti%2}", reads=[b_h])

    if halo:
        P.dma("sync", lambda e: e.dma_start(out=hh[0:2, :], in_=h_d[TPC:TPC + 2, :]), "hl0", writes=[b_hh])
        stage1(hh[0:2, :], b_hh, TPC, 2, hnTh[:], b_hnTh)
        stage2_halo()
    else:
        P.op("vector", lambda e: e.memset(carry[:], 0.0), writes=b_carry)
    for ci in range(nchunks):
        for ti in range(TPCH):
            row0 = ci * CH + ti * 128
            load_h(ti, row0)
            if 1 in stages:
                stage1(h1c[:, ti, :], b_h1[ti], row0, 128, hnT[:, :, ti * 128:(ti + 1) * 128], b_hnT[ti])
        if 2 in stages:
            stage2()
        for ti in range(TPCH):
            if 3 in stages:
                stage3(ti, ci * CH + ti * 128)
            else:
                P.dma("sync", lambda e, ti=ti, ci=ci: e.dma_start(out=out_d[ci * CH + ti * 128:ci * CH + ti * 128 + 128, :], in_=h1c[:, ti, :]), f"so{ti%2}", reads=[b_h1[ti]])
    P.emit()
    return nc

S = 16384
PERM = np.concatenate([np.arange(0, 512), np.arange(768, 896), np.arange(1024, 1152), np.arange(512, 640), np.arange(640, 768), np.arange(896, 1024), np.arange(1152, 1280), np.arange(1280, 2328)])

def ktile(w):
    K, N = w.shape
    return np.ascontiguousarray(w.reshape(K // 128, 128, N).transpose(1, 0, 2)).reshape(128, -1)

def rope_tables(pos):
    half = 8
    inv = (np.float32(500000.0) ** (-np.arange(half, dtype=np.float32) / np.float32(half))).astype(np.float32)
    ang = pos.astype(np.float32)[:, None] * inv[None, :]
    return np.cos(ang).astype(np.float32), np.sin(ang).astype(np.float32)

def bc(v, n=128):
    return np.ascontiguousarray(np.broadcast_to(v[None, :], (n, v.shape[0]))).astype(np.float32)

def A_inputs(h, i, inp, wbf):
    cos, sin = rope_tables(np.arange(S))
    tri = (np.arange(128)[:, None] <= np.arange(128)[None, :]).astype(BF)
    maps = []
    for c in range(8):
        sl = slice(c * 2048, (c + 1) * 2048)
        t = lambda a: np.ascontiguousarray(a[sl].reshape(16, 128, 8).transpose(1, 0, 2)).reshape(128, 128)
        maps.append(dict(h=np.ascontiguousarray(h[sl]), gpre=bc(inp["pre_mix_g"][i]), w=wbf["w_in"], cos=t(cos), sin=t(sin),
                         lng=bc(inp["gmlp_ln_g"][i]), lnb=bc(inp["gmlp_ln_b"][i]), gmlp=bc(inp["mlp_out_g"][i]),
                         bsT=np.ascontiguousarray(inp["gmlp_bs"][i].T).astype(np.float32), wsT=wbf["wsT"], tri=tri, ident=np.eye(128, dtype=BF)))
    return maps

S = 16384
NEGM = -30000.0

def slot_qi(c, s):
    j = s // 2
    return 16 * j + c if s % 2 == 0 else 16 * j + 15 - c

_CONST = {}
def B_consts():
    if _CONST:
        return _CONST
    n = np.arange(1024)
    jb = np.arange(256)
    ov = ((16 * n[:, None] + 31 >= 64 * jb[None, :]) & (16 * n[:, None] <= 64 * jb[None, :] + 63)).astype(np.float32)
    ov[1023, :] = 0
    ov[:, 255] = 1.0
    _CONST["ov"] = np.ascontiguousarray(ov.reshape(8, 128, 256).transpose(1, 0, 2)).reshape(128, -1).astype(BF)
    ind = np.zeros((128, 64, 128), np.float32)
    for jj in range(64):
        ind[2 * jj, jj, :64] = 1; ind[2 * jj + 1, jj, 64:] = 1
    _CONST["ind"] = ind.reshape(128, -1).astype(BF)
    _CONST["ident"] = np.eye(128, dtype=BF)
    cosc, sinc = rope_tables(16 * np.arange(1024) + 31)
    t = lambda a: np.ascontiguousarray(a.reshape(8, 128, 8).transpose(1, 0, 2)).reshape(128, 64)
    _CONST["cosc"] = t(cosc); _CONST["sinc"] = t(sinc)
    kq = np.arange(128)
    msks = []; sbs = []
    for c in range(8):
        m = np.zeros((16, 128, 15, 128), np.float32)
        sb = np.zeros((16, 128, 256), np.float32)
        for s in range(16):
            qi = slot_qi(c, s); j = s // 2
            tq = 128 * qi + kq
            for mi, nt in enumerate((j - 1, j)):
                if nt < 0: continue
                nn = 128 * nt + kq
                ok = (16 * nn[:, None] + 31 <= tq[None, :]) & (nn[:, None] <= 1022)
                m[s, :, mi, :] = np.where(ok, 0.0, NEGM)
            KT = 16 * j + 8 if s % 2 == 0 else 16 * j + 16
            for r in range(8):
                kt = KT - 8 + r
                tk = 128 * kt + kq
                ok = tk[:, None] <= tq[None, :]
                m[s, :, 2 + r, :] = np.where(ok, 0.0, NEGM)
            for w in range(5):
                tk = 128 * qi - 512 + 128 * w + kq
                ok = (tk[:, None] >= 0) & (tk[:, None] <= tq[None, :]) & (tk[:, None] > tq[None, :] - 512)
                m[s, :, 10 + w, :] = np.where(ok, 0.0, NEGM)
            cur = tq // 64
            valid = jb[None, :] <= cur[:, None]
            forced = valid & ((jb[None, :] == 0) | (jb[None, :] == cur[:, None]) | (jb[None, :] == cur[:, None] - 1))
            sb[s] = np.where(forced, 1000.0, np.where(valid, 0.0, -1e29))
        msks.append(m.reshape(16, 128, -1).astype(BF)); sbs.append(sb)
    _CONST["msk"] = msks; _CONST["sbias"] = sbs
    return _CONST

def B_inputs(qkv, gates, i, inp, wbf):
    C = B_consts()
    q = qkv[:, 0:512].reshape(S, 2, 4, 64)
    ks = qkv[:, 512:640].reshape(S, 2, 64); kw = qkv[:, 640:768].reshape(S, 2, 64)
    zkc = qkv[:, 768:896].reshape(S, 2, 64); zvc = qkv[:, 896:1024].reshape(S, 2, 64)
    vs = qkv[:, 1024:1152].reshape(S, 2, 64); vw = qkv[:, 1152:1280].reshape(S, 2, 64)
    KsT = np.ascontiguousarray(ks.transpose(1, 2, 0)).reshape(128, S)
    one = np.ones((S, 2, 1), BF)
    Vs1 = np.concatenate([vs, one], -1)
    Vs = np.ascontiguousarray(Vs1.reshape(128, 128, 2, 65).transpose(1, 0, 2, 3)).reshape(128, -1)
    kwp = np.concatenate([np.zeros((512, 2, 64), BF), kw], 0)
    vwp = np.concatenate([np.zeros((512, 2, 65), BF), np.concatenate([vw, one], -1)], 0)
    F = np.zeros((2, 2, 128, 16, 1024), BF)
    for X, zz in enumerate((zkc, zvc)):
        zp = np.concatenate([zz, np.zeros((32, 2, 64), BF)], 0)
        n = np.arange(1024); jp = np.arange(16); jj = np.arange(2)
        tidx = 16 * n[None, None, :] + 2 * jp[None, :, None] + jj[:, None, None]
        g_ = zp[tidx]
        g_[:, :, 1023] = 0
        F[X] = g_.transpose(3, 0, 4, 1, 2).reshape(2, 128, 16, 1024)
    Fq = np.ascontiguousarray(F.reshape(2, 2, 128, 16, 4, 256).transpose(0, 1, 4, 2, 3, 5)).reshape(2, 2, 4, 128, 16 * 256)
    b1T = np.stack([np.ascontiguousarray(inp["cmp_b1"][i][X].reshape(2, 128).T) for X in range(2)]).astype(np.float32)
    b2 = np.stack([np.broadcast_to(inp["cmp_b2"][i][X][None, :], (128, 64)) for X in range(2)]).astype(np.float32)
    maps = []
    for c in range(8):
        qis = [slot_qi(c, s) for s in range(16)]
        QT = np.stack([np.ascontiguousarray(q[128 * qi:128 * qi + 128].transpose(1, 3, 2, 0)).reshape(128, 512) for qi in qis])
        KwT = np.stack([np.ascontiguousarray(kwp[128 * qi:128 * qi + 640].transpose(1, 2, 0)).reshape(128, 640) for qi in qis])
        Vw = np.stack([np.ascontiguousarray(vwp[128 * qi:128 * qi + 640].reshape(5, 128, 2, 65).transpose(1, 0, 2, 3)).reshape(128, -1) for qi in qis])
        gt = np.stack([gates[128 * qi:128 * qi + 128] for qi in qis]).astype(np.float32)
        maps.append(dict(QT=QT, KsT=KsT, Vs=Vs, KwT=KwT, Vw=Vw, F=Fq, gates=gt, sbias=C["sbias"][c], msk=C["msk"][c],
                         w1=wbf["cmp_w1"], w2=wbf["cmp_w2"], b1T=b1T, peT=wbf["cmp_peT"], b2=np.ascontiguousarray(b2),
                         cosc=C["cosc"], sinc=C["sinc"], ov=C["ov"], ind=C["ind"], ident=C["ident"]))
    return maps

def B_gather(results):
    attn = np.zeros((S, 512), np.float32)
    for c in range(8):
        a = results[c]["attn"]
        for s in range(16):
            qi = slot_qi(c, s)
            attn[128 * qi:128 * qi + 128] = a[128 * s:128 * s + 128]
    return attn

def B_weights_f32(inp, i):
    w1 = np.stack([ktile(inp["cmp_w1"][i][X]) for X in range(2)])
    w2 = np.stack([ktile(inp["cmp_w2"][i][X]) for X in range(2)])
    pe = inp["cmp_pe"][i]
    peT = np.stack([np.ascontiguousarray(pe[X].reshape(16, 2, 64).transpose(1, 2, 0)).reshape(128, 16) for X in range(2)])
    return w1, w2, peT

S = 16384

def C_weights_f32(inp, i):
    wup = inp["w_up"][i]
    wup_l = np.ascontiguousarray(wup.reshape(8, 128, 2, 22, 128).transpose(3, 1, 2, 0, 4)).reshape(22, 128, -1)
    return dict(wo=ktile(inp["w_o"][i]), wup=wup_l, wdn=ktile(inp["w_down"][i]), wg=ktile(inp["w_ple_gate"][i]), wp=ktile(inp["w_ple_proj"][i]))

def C_inputs(h, attn, mlpn, i, inp, wbf):
    cw = inp["conv_w"][i]; cb = inp["conv_b"][i]
    conv = np.concatenate([cw, cb[None, :]], 0)
    conv = np.ascontiguousarray(conv.reshape(4, 44, 128).transpose(2, 1, 0)).reshape(128, -1).astype(np.float32)
    maps = []
    for c in range(8):
        sl = slice(2048 * c, 2048 * (c + 1))
        def ext(a):
            o = np.zeros((2048 + 128,) + a.shape[1:], a.dtype)
            o[:2048] = a[sl]
            if c > 0:
                o[2048:2050] = a[2048 * c - 2:2048 * c]
            return o
        maps.append(dict(h=ext(h), attn=ext(attn), mlpn=ext(mlpn), p=np.ascontiguousarray(inp["p"][i, 0][sl]),
                         gattn=bc(inp["attn_out_g"][i]), gpost=bc(inp["post_mix_g"][i]), gpre=bc(inp["pre_ffn_g"][i]),
                         gpffn=bc(inp["post_ffn_g"][i]), gple=bc(inp["ple_norm_g"][i]), conv=conv,
                         wo=wbf["wo"], wup=wbf["wup"], wdn=wbf["wdn"], wg=wbf["wg"], wp=wbf["wp"], ident=np.eye(128, dtype=BF)))
    return maps


_PROGS = {}

def _prog(name, fn):
    if name not in _PROGS:
        _PROGS[name] = fn()
    return _PROGS[name]

def _run(nc, maps):
    res = run_bass_kernel_spmd(nc, maps, core_ids=list(range(8)))
    return res.results

WCOLS = [("w_in", 8 * 2328), ("wsT", 1024), ("cmp_w1", 2 * 4096), ("cmp_w2", 2 * 128), ("cmp_peT", 2 * 16),
         ("wo", 8192), ("wup", 22 * 2048), ("wdn", 22 * 1024), ("wg", 8192), ("wp", 2048)]
WTOT = sum(c for _, c in WCOLS)

def _pack_weights(inp, i):
    cw = C_weights_f32(inp, i)
    w1, w2, peT = B_weights_f32(inp, i)
    parts = {
        "w_in": ktile(inp["w_in"][i][:, PERM]),
        "wsT": np.ascontiguousarray(inp["gmlp_ws"][i].transpose(2, 0, 1)).reshape(128, 1024),
        "cmp_w1": np.ascontiguousarray(w1.transpose(1, 0, 2)).reshape(128, -1),
        "cmp_w2": np.ascontiguousarray(w2.transpose(1, 0, 2)).reshape(128, -1),
        "cmp_peT": np.ascontiguousarray(peT.transpose(1, 0, 2)).reshape(128, -1),
        "wo": cw["wo"], "wup": np.ascontiguousarray(cw["wup"].transpose(1, 0, 2)).reshape(128, -1),
        "wdn": cw["wdn"], "wg": cw["wg"], "wp": cw["wp"],
    }
    return np.concatenate([parts[n].astype(np.float32) for n, _ in WCOLS], axis=1)

def _unpack_weights(wb):
    out = {}
    o = 0
    for n, c in WCOLS:
        out[n] = np.ascontiguousarray(wb[:, o:o + c]); o += c
    out["cmp_w1"] = np.ascontiguousarray(out["cmp_w1"].reshape(128, 2, 4096).transpose(1, 0, 2))
    out["cmp_w2"] = np.ascontiguousarray(out["cmp_w2"].reshape(128, 2, 128).transpose(1, 0, 2))
    out["cmp_peT"] = np.ascontiguousarray(out["cmp_peT"].reshape(128, 2, 16).transpose(1, 0, 2))
    out["wup"] = np.ascontiguousarray(out["wup"].reshape(128, 22, 2048).transpose(1, 0, 2))
    return out

def kernel(**inputs):
    inp = {k: np.asarray(v) for k, v in inputs.items()}
    L = 2
    big = np.concatenate([_pack_weights(inp, i) for i in range(L)], axis=1)
    per = big.shape[1] // 8
    assert per * 8 == big.shape[1]
    ncW = _prog("W", lambda: build_W(per))
    resW = _run(ncW, [{"win": np.ascontiguousarray(big[:, c * per:(c + 1) * per])} for c in range(8)])
    wb = np.concatenate([r["wout"] for r in resW], axis=1)
    wbf = [_unpack_weights(wb[:, i * WTOT:(i + 1) * WTOT]) for i in range(L)]
    h = np.ascontiguousarray(inp["x"][0]).astype(np.float32)
    for i in range(L):
        ncA = _prog("A", build_A)
        resA = _run(ncA, A_inputs(h, i, inp, wbf[i]))
        qkv = np.concatenate([r["qkv"] for r in resA], 0)
        gates = np.concatenate([r["gates"] for r in resA], 0)
        mlpn = np.concatenate([r["mlpn"] for r in resA], 0)
        ncB = _prog("B", build_B)
        resB = _run(ncB, B_inputs(qkv, gates, i, inp, wbf[i]))
        attn = B_gather(resB)
        ncC = _prog("C", build_C)
        resC = _run(ncC, C_inputs(h, attn, mlpn, i, inp, wbf[i]))
        h = np.concatenate([r["hout"] for r in resC], 0)
    return h[None].astype(np.float32)
```

```python
import numpy as np
import ml_dtypes
from contextlib import ExitStack
import concourse.bass as bass
import concourse.mybir as mybir
from concourse.bass_utils import run_bass_kernel_spmd

F32 = mybir.dt.float32
BF16 = mybir.dt.bfloat16
AF = mybir.ActivationFunctionType
ALU = mybir.AluOpType
AX = mybir.AxisListType
NPBF16 = ml_dtypes.bfloat16


class Buf:
    __slots__ = ("name", "last_w", "readers")

    def __init__(self, name=""):
        self.name = name
        self.last_w = None
        self.readers = []


class Op:
    __slots__ = ("eng", "fn", "deps", "is_dma", "stream", "needs_inc", "tok", "idx", "dma_upto")

    def __init__(self, eng, fn, is_dma=False, stream=None):
        self.eng = eng
        self.fn = fn
        self.deps = set()
        self.is_dma = is_dma
        self.stream = stream
        self.needs_inc = False
        self.tok = None
        self.idx = -1
        self.dma_upto = {}


class Prog:
    ENGS = ("sync", "scalar", "vector", "gpsimd", "tensor")
    SEM_ROLL = 6000

    def __init__(self, nc):
        self.nc = nc
        self.ops = []
        self.stack = ExitStack()
        self.nbuf = 0

    def buf(self, name=""):
        self.nbuf += 1
        return Buf(name or f"b{self.nbuf}")

    def sb(self, name, shape, dtype):
        return self.stack.enter_context(self.nc.sbuf_tensor("sb_" + name, list(shape), dtype))

    def ps(self, name, shape, dtype=F32):
        return self.stack.enter_context(self.nc.psum_tensor("ps_" + name, list(shape), dtype))

    def op(self, eng, fn, reads=(), writes=(), accum=False):
        o = Op(eng, fn)
        self._deps(o, reads, writes, accum)
        return o

    def dma(self, eng, fn, stream, reads=(), writes=()):
        o = Op(eng, fn, is_dma=True, stream=stream)
        self._deps(o, reads, writes, False)
        return o

    def _deps(self, o, reads, writes, accum):
        o.idx = len(self.ops)
        for b in reads:
            if b.last_w is not None:
                o.deps.add(b.last_w)
        for b in writes:
            if b.last_w is not None:
                lw = self.ops[b.last_w]
                if not (accum and lw.eng == o.eng and not lw.is_dma):
                    o.deps.add(b.last_w)
            for r in b.readers:
                o.deps.add(r)
        for b in reads:
            b.readers.append(o.idx)
        for b in writes:
            b.last_w = o.idx
            b.readers = []
        o.deps.discard(o.idx)
        if o.eng == "tensor" and not o.is_dma:
            o.deps = {d for d in o.deps if not (self.ops[d].eng == "tensor" and not self.ops[d].is_dma)}
        self.ops.append(o)

    def emit(self):
        nc = self.nc
        ops = self.ops
        for o in ops:
            for d in o.deps:
                ops[d].needs_inc = True
        sems = {}

        def new_sem(tag):
            return self.stack.enter_context(nc.semaphore(f"s_{tag}_{len(sems)}"))

        eng_sem = {}
        eng_cnt = {}
        stream_sem = {}
        stream_cnt = {}
        stream_hist = {}
        for o in ops:
            if o.is_dma:
                if o.stream not in stream_sem:
                    stream_sem[o.stream] = new_sem("d")
                    sems[len(sems)] = 1
                    stream_cnt[o.stream] = 0
                    stream_hist[o.stream] = []
                stream_cnt[o.stream] += 16
                o.tok = (stream_sem[o.stream], stream_cnt[o.stream])
                stream_hist[o.stream].append(o.idx)
            elif o.needs_inc:
                if o.eng not in eng_sem or eng_cnt[o.eng] >= self.SEM_ROLL:
                    eng_sem[o.eng] = new_sem(o.eng[0])
                    sems[len(sems)] = 1
                    eng_cnt[o.eng] = 0
                eng_cnt[o.eng] += 1
                o.tok = (eng_sem[o.eng], eng_cnt[o.eng])
        self.n_sems = len(sems)
        import bisect
        seen = {e: {} for e in self.ENGS}
        waits = [None] * len(ops)
        for o in ops:
            need = {}
            for d in o.deps:
                do = ops[d]
                sem, val = do.tok
                if do.is_dma:
                    h = stream_hist[do.stream]
                    k = bisect.bisect_left(h, o.idx)
                    val = 16 * k
                key = id(sem)
                if key not in need or need[key][1] < val:
                    need[key] = (sem, val)
            w = []
            s = seen[o.eng]
            for key, (sem, val) in need.items():
                if s.get(key, 0) < val:
                    s[key] = val
                    w.append((sem, val))
            waits[o.idx] = w
        with nc.Block() as block:
            def make(engname):
                def body(e):
                    for o in ops:
                        if o.eng != engname:
                            continue
                        for sem, val in waits[o.idx]:
                            e.wait_ge(sem, val)
                        ins = o.fn(e)
                        if o.is_dma:
                            ins.then_inc(o.tok[0], 16)
                        elif o.needs_inc:
                            ins.then_inc(o.tok[0], 1)
                    if engname == "sync":
                        for st, sem in stream_sem.items():
                            e.wait_ge(sem, stream_cnt[st])
                return body
            block.sync(make("sync"))
            block.scalar(make("scalar"))
            block.vector(make("vector"))
            block.gpsimd(make("gpsimd"))
            block.tensor(make("tensor"))
        self.stack.close()

BF = NPBF16

S = 16384; D = 1024; NCORE = 8; TPC = 2048; NTILE = 16
INW = 2328
EPS = 1e-6

def build_W(ncols, chunk=4096):
    nc = bass.Bass("TRN2", target_bir_lowering=False)
    win = nc.dram_tensor("win", [128, ncols], F32, kind="ExternalInput").ap()
    wout = nc.dram_tensor("wout", [128, ncols], BF16, kind="ExternalOutput").ap()
    P = Prog(nc)
    nb = 3
    st = [P.sb(f"st{i}", [128, chunk], F32) for i in range(nb)]
    sb = [P.sb(f"sb{i}", [128, chunk], BF16) for i in range(nb)]
    b_st = [P.buf() for _ in range(nb)]
    b_sb = [P.buf() for _ in range(nb)]
    engs = ["vector", "gpsimd", "vector"]
    i = 0
    for c0 in range(0, ncols, chunk):
        c1 = min(ncols, c0 + chunk); w = c1 - c0; k = i % nb
        P.dma("sync", lambda e, k=k, c0=c0, c1=c1, w=w: e.dma_start(out=st[k][:, 0:w], in_=win[:, c0:c1]), f"wl{k}", writes=[b_st[k]])
        en = engs[i % 3]
        if en == "scalar":
            P.op(en, lambda e, k=k, w=w: e.copy(out=sb[k][:, 0:w], in_=st[k][:, 0:w]), reads=[b_st[k]], writes=[b_sb[k]])
        else:
            P.op(en, lambda e, k=k, w=w: e.tensor_copy(out=sb[k][:, 0:w], in_=st[k][:, 0:w]), reads=[b_st[k]], writes=[b_sb[k]])
        P.dma("sync", lambda e, k=k, c0=c0, c1=c1, w=w: e.dma_start(out=wout[:, c0:c1], in_=sb[k][:, 0:w]), f"ws{k}", reads=[b_sb[k]])
        i += 1
    P.emit()
    return nc


def build_A():
    nc = bass.Bass("TRN2", target_bir_lowering=False)
    din = lambda n, sh, dt=F32: nc.dram_tensor(n, sh, dt, kind="ExternalInput").ap()
    dout = lambda n, sh, dt=F32: nc.dram_tensor(n, sh, dt, kind="ExternalOutput").ap()
    h_d = din("h", [TPC, D])
    gpre_d = din("gpre", [128, D])
    w_d = din("w", [128, 8 * INW], BF16)
    cos_d = din("cos", [128, NTILE * 8]); sin_d = din("sin", [128, NTILE * 8])
    lng_d = din("lng", [128, 512]); lnb_d = din("lnb", [128, 512]); gmlp_d = din("gmlp", [128, 512])
    bsT_d = din("bsT", [128, 8])
    wsT_d = din("wsT", [128, 8 * 128], BF16)
    tri_d = din("tri", [128, 128], BF16)
    id_d = din("ident", [128, 128], BF16)
    qkv_o = dout("qkv", [TPC, 1280], BF16)
    gates_o = dout("gates", [TPC, 24])
    mlpn_o = dout("mlpn", [TPC, 512], BF16)
    P = Prog(nc)
    W = P.sb("W", [128, 8, INW], BF16); b_W = P.buf()
    gpre = P.sb("gpre", [128, D], F32); b_c = P.buf()
    cos = P.sb("cos", [128, NTILE, 8], F32); sin = P.sb("sin", [128, NTILE, 8], F32)
    lng = P.sb("lng", [128, 512], F32); lnb = P.sb("lnb", [128, 512], F32); gmlp = P.sb("gmlpg", [128, 512], F32)
    bsT = P.sb("bsT", [128, 8], F32)
    wsT = P.sb("wsT", [128, 8, 128], BF16); b_ws = P.buf()
    tri = P.sb("tri", [128, 128], BF16)
    ident = P.sb("ident", [128, 128], BF16)
    for kt in range(8):
        P.dma("sync" if kt % 2 == 0 else "gpsimd", lambda e, kt=kt: e.dma_start(out=W[:, kt, :], in_=w_d[:, kt * INW:(kt + 1) * INW]), f"cw{kt%2}", writes=[b_W])
    for (t, d_) in [(gpre, gpre_d), (lng, lng_d), (lnb, lnb_d), (gmlp, gmlp_d), (bsT, bsT_d), (tri, tri_d), (ident, id_d)]:
        P.dma("sync", lambda e, t=t, d_=d_: e.dma_start(out=t[:], in_=d_), "cc", writes=[b_c])
    P.dma("sync", lambda e: e.dma_start(out=cos[:].rearrange("p a b -> p (a b)"), in_=cos_d), "cc", writes=[b_c])
    P.dma("sync", lambda e: e.dma_start(out=sin[:].rearrange("p a b -> p (a b)"), in_=sin_d), "cc", writes=[b_c])
    P.dma("sync", lambda e: e.dma_start(out=wsT[:].rearrange("p a b -> p (a b)"), in_=wsT_d), "cc", writes=[b_ws])
    P.op("vector", lambda e: e.tensor_tensor(out=wsT[:], in0=wsT[:], in1=tri[:].unsqueeze(1).to_broadcast([128, 8, 128]), op=ALU.mult), reads=[b_ws, b_c], writes=[b_ws])

    NB = 2
    hs = [P.sb(f"hs{i}", [128, D], F32) for i in range(NB)]; b_hs = [P.buf() for _ in range(NB)]
    junk = P.sb("junk", [128, D], BF16); b_junk = P.buf()
    st = [P.sb(f"stat{i}", [128, 8], F32) for i in range(NB)]; b_st = [P.buf() for _ in range(NB)]
    a_bf = P.sb("a_bf", [128, D], BF16); b_a = P.buf()
    aT = P.sb("aT", [128, 8, 128], BF16); b_aT = P.buf()
    pT = P.ps("pT", [128, 8, 128], BF16); b_pT = P.buf()
    zps = [P.ps(f"zps{i}", [128, 512], F32) for i in range(3)]; b_zps = [P.buf() for _ in range(3)]
    mixps = P.ps("mixps", [128, 512], F32); b_mix = P.buf()
    z = P.sb("z", [128, INW], F32); b_z = [P.buf() for _ in range(5)]
    qkv = [P.sb(f"qkv{i}", [128, 1280], BF16) for i in range(NB)]; b_qkv = [P.buf() for _ in range(NB)]
    rt = P.sb("rt", [128, 4, 12, 8], F32); b_rt = P.buf()
    gt = [P.sb(f"gt{i}", [128, 24], F32) for i in range(NB)]; b_gt = [P.buf() for _ in range(NB)]
    u = P.sb("u", [128, 512], F32); b_u = P.buf()
    v = P.sb("v", [128, 512], F32); b_v = P.buf()
    bnst = P.sb("bnst", [128, 6], F32); b_bn = P.buf()
    mv = P.sb("mv", [128, 4], F32); b_mv = P.buf()
    vn = P.sb("vn", [128, 512], BF16); b_vn = P.buf()
    m1 = P.sb("m1", [128, 512], F32); b_m1 = P.buf()
    mlpn = [P.sb(f"mlpn{i}", [128, 512], BF16) for i in range(NB)]; b_mlpn = [P.buf() for _ in range(NB)]
    chunks = [(0, 512), (512, 512), (1024, 512), (1536, 512), (2048, 280)]

    def load(ti):
        k = ti % NB
        P.dma("sync", lambda e: e.dma_start(out=hs[k][:], in_=h_d[ti * 128:(ti + 1) * 128, :]), f"hl{k}", writes=[b_hs[k]])

    def do_tile(ti):
        k = ti % NB
        if ti + 1 < NTILE:
            load(ti + 1)
        s_ = st[k]
        P.op("scalar", lambda e: e.activation(out=junk[:], in_=hs[k][:], func=AF.Square, accum_out=s_[:, 0:1]), reads=[b_hs[k]], writes=[b_junk, b_st[k]])
        P.op("scalar", lambda e: e.activation(out=s_[:, 1:2], in_=s_[:, 0:1], func=AF.Sqrt, scale=1.0 / D, bias=EPS), reads=[b_st[k]], writes=[b_st[k]])
        P.op("vector", lambda e: e.reciprocal(out=s_[:, 2:3], in_=s_[:, 1:2]), reads=[b_st[k]], writes=[b_st[k]])
        P.op("vector", lambda e: e.scalar_tensor_tensor(out=a_bf[:], in0=hs[k][:], scalar=s_[:, 2:3], in1=gpre[:], op0=ALU.mult, op1=ALU.mult), reads=[b_hs[k], b_st[k], b_c], writes=[b_a])
        for kt in range(8):
            P.op("tensor", lambda e, kt=kt: e.transpose(out=pT[:, kt, :], in_=a_bf[:, kt * 128:(kt + 1) * 128], identity=ident[:]), reads=[b_a, b_c], writes=[b_pT], accum=True)
        P.op("vector", lambda e: e.tensor_copy(out=aT[:], in_=pT[:]), reads=[b_pT], writes=[b_aT])
        for ci, (c0, cw) in enumerate(chunks):
            zp = zps[ci % 3]; bz = b_zps[ci % 3]
            for kt in range(8):
                P.op("tensor", lambda e, kt=kt, zp=zp, c0=c0, cw=cw: e.matmul(zp[:, 0:cw], lhsT=aT[:, kt, :], rhs=W[:, kt, c0:c0 + cw], start=(kt == 0), stop=(kt == 7)), reads=[b_aT, b_W], writes=[bz], accum=True)
            if ci % 2 == 0:
                P.op("scalar", lambda e, zp=zp, c0=c0, cw=cw: e.copy(out=z[:, c0:c0 + cw], in_=zp[:, 0:cw]), reads=[bz], writes=[b_z[ci]])
            else:
                P.op("vector", lambda e, zp=zp, c0=c0, cw=cw: e.tensor_copy(out=z[:, c0:c0 + cw], in_=zp[:, 0:cw]), reads=[bz], writes=[b_z[ci]])
        P.op("gpsimd", lambda e: e.tensor_copy(out=qkv[k][:], in_=z[:, 0:1280]), reads=[b_z[0], b_z[1], b_z[2]], writes=[b_qkv[k]])
        zr = z[:, 0:768].rearrange("p (h d) -> p h d", d=64)
        qr = qkv[k][:, 0:768].rearrange("p (h d) -> p h d", d=64)
        cb = cos[:, ti, :].unsqueeze(1).to_broadcast([128, 12, 8]); sb_ = sin[:, ti, :].unsqueeze(1).to_broadcast([128, 12, 8])
        x1 = zr[:, :, 0:8]; x2 = zr[:, :, 8:16]
        P.op("vector", lambda e: e.tensor_tensor(out=rt[:, 0], in0=x1, in1=cb, op=ALU.mult), reads=[b_z[0], b_z[1], b_c], writes=[b_rt])
        P.op("vector", lambda e: e.tensor_tensor(out=rt[:, 1], in0=x2, in1=sb_, op=ALU.mult), reads=[b_z[0], b_z[1], b_c], writes=[b_rt])
        P.op("vector", lambda e: e.tensor_tensor(out=rt[:, 2], in0=x2, in1=cb, op=ALU.mult), reads=[b_z[0], b_z[1], b_c], writes=[b_rt])
        P.op("vector", lambda e: e.tensor_tensor(out=rt[:, 3], in0=x1, in1=sb_, op=ALU.mult), reads=[b_z[0], b_z[1], b_c], writes=[b_rt])
        P.op("vector", lambda e: e.tensor_tensor(out=qr[:, :, 0:8], in0=rt[:, 0], in1=rt[:, 1], op=ALU.subtract), reads=[b_rt], writes=[b_qkv[k]])
        P.op("vector", lambda e: e.tensor_tensor(out=qr[:, :, 8:16], in0=rt[:, 2], in1=rt[:, 3], op=ALU.add), reads=[b_rt], writes=[b_qkv[k]])
        P.dma("sync", lambda e: e.dma_start(out=qkv_o[ti * 128:(ti + 1) * 128, :], in_=qkv[k][:]), f"so{k}", reads=[b_qkv[k]])
        P.op("scalar", lambda e: e.activation(out=gt[k][:], in_=z[:, 1280:1304], func=AF.Sigmoid), reads=[b_z[2]], writes=[b_gt[k]])
        P.dma("sync", lambda e: e.dma_start(out=gates_o[ti * 128:(ti + 1) * 128, :], in_=gt[k][:]), f"so{k}", reads=[b_gt[k]])
        P.op("scalar", lambda e: e.activation(out=u[:], in_=z[:, 1304:1816], func=AF.Gelu_apprx_tanh), reads=[b_z[2], b_z[3]], writes=[b_u])
        P.op("scalar", lambda e: e.activation(out=v[:], in_=z[:, 1816:2328], func=AF.Gelu_apprx_tanh), reads=[b_z[3], b_z[4]], writes=[b_v])
        P.op("vector", lambda e: e.bn_stats(out=bnst[:], in_=v[:]), reads=[b_v], writes=[b_bn])
        P.op("vector", lambda e: e.bn_aggr(out=mv[:, 0:2], in_=bnst[:]), reads=[b_bn], writes=[b_mv])
        P.op("scalar", lambda e: e.activation(out=mv[:, 2:3], in_=mv[:, 1:2], func=AF.Sqrt, scale=1.0, bias=EPS), reads=[b_mv], writes=[b_mv])
        P.op("vector", lambda e: e.reciprocal(out=mv[:, 3:4], in_=mv[:, 2:3]), reads=[b_mv], writes=[b_mv])
        P.op("vector", lambda e: e.tensor_scalar(out=v[:], in0=v[:], scalar1=mv[:, 0:1], scalar2=mv[:, 3:4], op0=ALU.subtract, op1=ALU.mult), reads=[b_v, b_mv], writes=[b_v])
        P.op("gpsimd", lambda e: e.tensor_tensor(out=v[:], in0=v[:], in1=lng[:], op=ALU.mult), reads=[b_v, b_c], writes=[b_v])
        P.op("gpsimd", lambda e: e.tensor_tensor(out=vn[:], in0=v[:], in1=lnb[:], op=ALU.add), reads=[b_v, b_c], writes=[b_vn])
        for g in range(8):
            P.op("tensor", lambda e, g=g: e.matmul(mixps[:, g * 64:(g + 1) * 64], lhsT=wsT[:, g, :], rhs=vn[:, g * 64:(g + 1) * 64], start=True, stop=True), reads=[b_ws, b_vn], writes=[b_mix], accum=True)
        P.op("vector", lambda e: e.tensor_tensor(out=m1[:].rearrange("p (g d) -> p g d", d=64), in0=mixps[:].rearrange("p (g d) -> p g d", d=64), in1=bsT[:].unsqueeze(2).to_broadcast([128, 8, 64]), op=ALU.add), reads=[b_mix, b_c], writes=[b_m1])
        P.op("vector", lambda e: e.tensor_tensor(out=m1[:], in0=m1[:], in1=u[:], op=ALU.mult), reads=[b_m1, b_u], writes=[b_m1])
        P.op("scalar", lambda e: e.activation(out=junk[:, 0:512], in_=m1[:], func=AF.Square, accum_out=s_[:, 4:5]), reads=[b_m1], writes=[b_junk, b_st[k]])
        P.op("scalar", lambda e: e.activation(out=s_[:, 5:6], in_=s_[:, 4:5], func=AF.Sqrt, scale=1.0 / 512, bias=EPS), reads=[b_st[k]], writes=[b_st[k]])
        P.op("vector", lambda e: e.reciprocal(out=s_[:, 6:7], in_=s_[:, 5:6]), reads=[b_st[k]], writes=[b_st[k]])
        P.op("vector", lambda e: e.scalar_tensor_tensor(out=mlpn[k][:], in0=m1[:], scalar=s_[:, 6:7], in1=gmlp[:], op0=ALU.mult, op1=ALU.mult), reads=[b_m1, b_st[k], b_c], writes=[b_mlpn[k]])
        P.dma("sync", lambda e: e.dma_start(out=mlpn_o[ti * 128:(ti + 1) * 128, :], in_=mlpn[k][:]), f"so{k}", reads=[b_mlpn[k]])
    load(0)
    for ti in range(NTILE):
        do_tile(ti)
    P.emit()
    return nc


S = 16384
NEGM = -30000.0
SCALE = 0.125

def slot_qi(c, s):
    j = s // 2
    return 16 * j + c if s % 2 == 0 else 16 * j + 15 - c

def build_B(nslots=16, debug=False):
    nc = bass.Bass("TRN2", target_bir_lowering=False)
    din = lambda n, sh, dt=F32: nc.dram_tensor(n, sh, dt, kind="ExternalInput").ap()
    dout = lambda n, sh, dt=F32: nc.dram_tensor(n, sh, dt, kind="ExternalOutput").ap()
    QT_d = din("QT", [nslots, 128, 512], BF16)
    KsT_d = din("KsT", [128, S], BF16)
    Vs_d = din("Vs", [128, 128 * 2 * 65], BF16)
    KwT_d = din("KwT", [nslots, 128, 640], BF16)
    Vw_d = din("Vw", [nslots, 128, 5 * 2 * 65], BF16)
    F_d = din("F", [2, 2, 4, 128, 16 * 256], BF16)
    gates_d = din("gates", [nslots, 128, 24])
    sbias_d = din("sbias", [nslots, 128, 256])
    msk_d = din("msk", [nslots, 128, 15 * 128], BF16)
    w1_d = din("w1", [2, 128, 16 * 256], BF16)
    w2_d = din("w2", [2, 128, 2 * 64], BF16)
    b1T_d = din("b1T", [2, 128, 2])
    peT_d = din("peT", [2, 128, 16], BF16)
    b2_d = din("b2", [2, 128, 64])
    cosc_d = din("cosc", [128, 64]); sinc_d = din("sinc", [128, 64])
    ov_d = din("ov", [128, 8 * 256], BF16)
    ind_d = din("ind", [128, 64 * 128], BF16)
    id_d = din("ident", [128, 128], BF16)
    attn_o = dout("attn", [nslots * 128, 512])
    dbg_o = dout("dbg", [nslots * 128, 3 * 512]) if debug else None
    P = Prog(nc)
    KsT = P.sb("KsT", [128, S], BF16); b_KsT = P.buf()
    Vs = P.sb("Vs", [128, 128, 2, 65], BF16); b_Vs = P.buf()
    IndAll = P.sb("IndAll", [128, 64, 128], BF16)
    ov = P.sb("ov", [128, 8, 256], BF16)
    ident = P.sb("ident", [128, 128], BF16)
    cosc = P.sb("cosc", [128, 8, 8], F32); sinc = P.sb("sinc", [128, 8, 8], F32)
    b_c = P.buf()
    w1 = P.sb("w1", [128, 2, 16, 256], BF16); w2 = P.sb("w2", [128, 2, 2, 64], BF16)
    b1T = P.sb("b1T", [128, 2, 2], F32); peT = P.sb("peT", [128, 2, 16], BF16); b2 = P.sb("b2", [128, 2, 64], F32)
    b_cw = P.buf()
    KcT = P.sb("KcT", [128, 1024], BF16); b_KcT = P.buf()
    Vc = P.sb("Vc", [128, 8, 2, 65], BF16); b_Vc = P.buf()
    for X in range(2):
        P.dma("sync", lambda e, X=X: e.dma_start(out=w1[:, X].rearrange("p a b -> p (a b)"), in_=w1_d[X]), "cc", writes=[b_cw])
        P.dma("sync", lambda e, X=X: e.dma_start(out=w2[:, X].rearrange("p a b -> p (a b)"), in_=w2_d[X]), "cc", writes=[b_cw])
        P.dma("sync", lambda e, X=X: e.dma_start(out=b1T[:, X], in_=b1T_d[X]), "cc", writes=[b_cw])
        P.dma("sync", lambda e, X=X: e.dma_start(out=peT[:, X], in_=peT_d[X]), "cc", writes=[b_cw])
        P.dma("sync", lambda e, X=X: e.dma_start(out=b2[:, X], in_=b2_d[X]), "cc", writes=[b_cw])
    P.dma("sync", lambda e: e.dma_start(out=ident[:], in_=id_d), "cc", writes=[b_c])
    P.dma("sync", lambda e: e.dma_start(out=cosc[:].rearrange("p a b -> p (a b)"), in_=cosc_d), "cc", writes=[b_c])
    P.dma("sync", lambda e: e.dma_start(out=sinc[:].rearrange("p a b -> p (a b)"), in_=sinc_d), "cc", writes=[b_c])
    P.dma("sync", lambda e: e.dma_start(out=ov[:].rearrange("p a b -> p (a b)"), in_=ov_d), "cc", writes=[b_c])
    P.dma("gpsimd", lambda e: e.dma_start(out=IndAll[:].rearrange("p a b -> p (a b)"), in_=ind_d), "cc2", writes=[b_c])
    for q4 in range(4):
        P.dma("gpsimd", lambda e, q4=q4: e.dma_start(out=KsT[:, q4 * 4096:(q4 + 1) * 4096], in_=KsT_d[:, q4 * 4096:(q4 + 1) * 4096]), "cc2", writes=[b_KsT])
        P.dma("gpsimd", lambda e, q4=q4: e.dma_start(out=Vs[:, q4 * 32:(q4 + 1) * 32].rearrange("p a b c -> p (a b c)"), in_=Vs_d[:, q4 * 32 * 130:(q4 + 1) * 32 * 130]), "cc2", writes=[b_Vs])
    sTs = [P.ps(f"sT{i}", [128, 512], F32) for i in range(3)]; b_sT = [P.buf() for _ in range(3)]
    oaccs = [P.ps(f"oacc{i}", [128, 4, 128], F32) for i in range(2)]; b_oacc = [P.buf() for _ in range(2)]
    imps = [P.ps(f"imp{i}", [128, 2, 256], F32) for i in range(2)]; b_imp = P.buf()
    tp = P.ps("tp", [128, 2, 128], BF16); b_tp = P.buf()
    cps = sTs[2][:, 0:64]; b_cps = b_sT[2]
    b1ps = sTs[2][:, 64:66]; b_b1ps = b_sT[2]
    Fb = [P.sb(f"Fb{i}", [128, 16, 256], BF16) for i in range(2)]; b_Fb = [P.buf() for _ in range(2)]
    hT = P.sb("hT", [128, 2, 256], BF16); b_hT = [P.buf() for _ in range(2)]
    bias1 = P.sb("bias1", [128, 2, 2], F32); b_bias1 = P.buf()
    kcf = P.sb("kcf", [128, 8, 2, 64], F32); b_kcf = P.buf()
    kcb = P.sb("kcb", [128, 8, 2, 64], BF16); b_kcb = P.buf()
    rt = P.sb("rt", [128, 4, 8, 2, 8], F32); b_rt = P.buf()
    P.op("vector", lambda e: e.memset(Vc[:], 1.0), writes=[b_Vc])
    fi = 0
    for X in range(2):
        for hc in range(2):
            for jp in range(16):
                P.op("tensor", lambda e, X=X, hc=hc, jp=jp: e.matmul(sTs[2][:, 64 + hc:65 + hc], lhsT=w1[:, X, jp, hc * 128:(hc + 1) * 128], rhs=peT[:, X, jp:jp + 1], start=(jp == 0), stop=(jp == 15)), reads=[b_cw], writes=[b_b1ps], accum=True)
        P.op("vector", lambda e, X=X: e.tensor_tensor(out=bias1[:, X], in0=b1ps, in1=b1T[:, X], op=ALU.add), reads=[b_b1ps, b_cw], writes=[b_bias1])
        for g in range(2):
            for nh in range(4):
                fb = Fb[fi % 2]; bfb = b_Fb[fi % 2]
                P.dma("sync", lambda e, fb=fb, X=X, g=g, nh=nh: e.dma_start(out=fb[:].rearrange("p a b -> p (a b)"), in_=F_d[X, g, nh]), f"F{fi%2}", writes=[bfb])
                fi += 1
                for hc in range(2):
                    sT = sTs[hc]
                    for jp in range(16):
                        P.op("tensor", lambda e, X=X, hc=hc, jp=jp, fb=fb, sT=sT: e.matmul(sT[:, 0:256], lhsT=w1[:, X, jp, hc * 128:(hc + 1) * 128], rhs=fb[:, jp, :], start=(jp == 0), stop=(jp == 15)), reads=[b_cw, bfb], writes=[b_sT[hc]], accum=True)
                    P.op("scalar", lambda e, X=X, hc=hc, sT=sT: e.activation(out=hT[:, hc, :], in_=sT[:, 0:256], func=AF.Gelu_apprx_tanh, bias=bias1[:, X, hc:hc + 1], scale=1.0), reads=[b_sT[hc], b_bias1], writes=[b_hT[hc]])
                for ntl in range(2):
                    nt = nh * 2 + ntl
                    for hc in range(2):
                        P.op("tensor", lambda e, X=X, hc=hc, ntl=ntl: e.matmul(cps, lhsT=hT[:, hc, ntl * 128:(ntl + 1) * 128], rhs=w2[:, X, hc, :], start=(hc == 0), stop=(hc == 1)), reads=[b_hT[hc], b_cw], writes=[b_cps], accum=True)
                    if X == 1:
                        P.op("vector", lambda e, nt=nt, g=g: e.tensor_tensor(out=Vc[:, nt, g, 0:64], in0=cps, in1=b2[:, 1], op=ALU.add), reads=[b_cps, b_cw], writes=[b_Vc])
                    else:
                        P.op("vector", lambda e, nt=nt, g=g: e.tensor_tensor(out=kcf[:, nt, g, :], in0=cps, in1=b2[:, 0], op=ALU.add), reads=[b_cps, b_cw], writes=[b_kcf])
        if X == 0:
            P.op("gpsimd", lambda e: e.tensor_copy(out=kcb[:], in_=kcf[:]), reads=[b_kcf], writes=[b_kcb])
            cb = cosc[:].unsqueeze(2).to_broadcast([128, 8, 2, 8]); sb_ = sinc[:].unsqueeze(2).to_broadcast([128, 8, 2, 8])
            x1 = kcf[:, :, :, 0:8]; x2 = kcf[:, :, :, 8:16]
            P.op("vector", lambda e: e.tensor_tensor(out=rt[:, 0], in0=x1, in1=cb, op=ALU.mult), reads=[b_kcf, b_c], writes=[b_rt])
            P.op("vector", lambda e: e.tensor_tensor(out=rt[:, 1], in0=x2, in1=sb_, op=ALU.mult), reads=[b_kcf, b_c], writes=[b_rt])
            P.op("vector", lambda e: e.tensor_tensor(out=rt[:, 2], in0=x2, in1=cb, op=ALU.mult), reads=[b_kcf, b_c], writes=[b_rt])
            P.op("vector", lambda e: e.tensor_tensor(out=rt[:, 3], in0=x1, in1=sb_, op=ALU.mult), reads=[b_kcf, b_c], writes=[b_rt])
            P.op("vector", lambda e: e.tensor_tensor(out=kcb[:, :, :, 0:8], in0=rt[:, 0], in1=rt[:, 1], op=ALU.subtract), reads=[b_rt], writes=[b_kcb])
            P.op("vector", lambda e: e.tensor_tensor(out=kcb[:, :, :, 8:16], in0=rt[:, 2], in1=rt[:, 3], op=ALU.add), reads=[b_rt], writes=[b_kcb])
            for nt in range(8):
                P.op("tensor", lambda e, nt=nt: e.transpose(out=tp[:, 0, :], in_=kcb[:, nt].rearrange("p a b -> p (a b)"), identity=ident[:]), reads=[b_kcb, b_c], writes=[b_tp])
                P.op("vector", lambda e, nt=nt: e.tensor_copy(out=KcT[:, nt * 128:(nt + 1) * 128], in_=tp[:, 0, :]), reads=[b_tp], writes=[b_KcT])
    QT = [P.sb(f"QT{i}", [128, 512], BF16) for i in range(2)]
    gts = [P.sb(f"gts{i}", [128, 8, 3], F32) for i in range(2)]
    sbias = [P.sb(f"sbias{i}", [128, 256], F32) for i in range(2)]
    msk = [P.sb(f"msk{i}", [128, 15, 128], BF16) for i in range(2)]
    KwT = [P.sb(f"KwT{i}", [128, 640], BF16) for i in range(2)]
    Vw = [P.sb(f"Vw{i}", [128, 5, 2, 65], BF16) for i in range(2)]
    b_sl = [P.buf() for _ in range(2)]
    eT = P.sb("eT", [128, 8, 512], BF16); b_eT = [P.buf() for _ in range(8)]
    pTs = [P.sb(f"pT{i}", [128, 512], BF16) for i in range(4)]; b_pT = [P.buf() for _ in range(4)]
    nsT4 = P.sb("nsT4", [128, 2, 4, 128], BF16); b_ns = P.buf()
    score = P.sb("score", [128, 256], F32); b_score = P.buf()
    sc2 = P.sb("sc2", [128, 256], F32); b_sc2 = P.buf()
    m8 = P.sb("m8", [128, 16], F32); b_m8 = P.buf()
    rd = P.sb("rd", [128, 4], F32); b_rd = P.buf()
    negsel = P.sb("negsel", [128, 256], BF16); b_negsel = P.buf()
    wcs = [P.sb(f"wc{i}", [128, 4], F32) for i in range(3)]; b_wc = [P.buf() for _ in range(3)]
    acc = [P.sb(f"acc{i}", [128, 8, 64], F32) for i in range(2)]; b_acc = [P.buf() for _ in range(2)]
    cnt = {"sT": 0, "pT": 0, "oa": 0}

    def load_slot(s):
        k2 = s % 2
        w = [b_sl[k2]]
        st = f"sl{k2}"
        P.dma("sync", lambda e: e.dma_start(out=QT[k2][:], in_=QT_d[s]), st, writes=w)
        P.dma("sync", lambda e: e.dma_start(out=gts[k2][:].rearrange("p a b -> p (a b)"), in_=gates_d[s]), st, writes=w)
        P.dma("sync", lambda e: e.dma_start(out=sbias[k2][:], in_=sbias_d[s]), st, writes=w)
        P.dma("sync", lambda e: e.dma_start(out=msk[k2][:].rearrange("p a b -> p (a b)"), in_=msk_d[s]), st, writes=w)
        P.dma("sync", lambda e: e.dma_start(out=KwT[k2][:], in_=KwT_d[s]), st, writes=w)
        P.dma("sync", lambda e: e.dma_start(out=Vw[k2][:].rearrange("p a b c -> p (a b c)"), in_=Vw_d[s]), st, writes=w)

    def branch(k2, g, tiles, lhs_fn, lhs_bufs, v_fn, v_bufs, mask_fn, pbufs, on_exp=None):
        gp = slice(g * 64, (g + 1) * 64)
        oi = cnt["oa"] % 2; cnt["oa"] += 1
        oacc = oaccs[oi]; boacc = b_oacc[oi]
        n = len(tiles)
        sbank = {}

        def S(i):
            t = tiles[i]
            bi = cnt["sT"] % 3; cnt["sT"] += 1
            sbank[i] = bi
            sT = sTs[bi]
            extra = mask_fn(t)
            l0 = lhs_fn(t); rq = QT[k2][gp, :]
            P.op("tensor", lambda e: e.matmul(sT[:], lhsT=l0, rhs=rq, start=True, stop=(len(extra) == 0)), reads=[b_sl[k2]] + lhs_bufs, writes=[b_sT[bi]])
            for xi, (kind, l_ap, r_ap, rb) in enumerate(extra):
                last = xi == len(extra) - 1
                if kind == "full":
                    P.op("tensor", lambda e, l_ap=l_ap, r_ap=r_ap, last=last: e.matmul(sT[:], lhsT=l_ap, rhs=r_ap, start=False, stop=last), reads=rb, writes=[b_sT[bi]], accum=True)
                else:
                    for h in range(4):
                        P.op("tensor", lambda e, l_ap=l_ap, r_ap=r_ap, last=last, h=h: e.matmul(sT[:, h * 128:(h + 1) * 128], lhsT=l_ap, rhs=r_ap, start=False, stop=(last and h == 3)), reads=rb, writes=[b_sT[bi]], accum=True)

        def E(i):
            t = tiles[i]
            bi = sbank[i]
            p_ap, p_b = pbufs(i)
            P.op("scalar", lambda e: e.activation(out=p_ap, in_=sTs[bi][:], func=AF.Exp, scale=SCALE), reads=[b_sT[bi]], writes=[p_b])

        def V(i):
            t = tiles[i]
            p_ap, p_b = pbufs(i)
            v0 = v_fn(t)
            for h in range(4):
                P.op("tensor", lambda e, h=h: e.matmul(oacc[:, h, 0:65], lhsT=p_ap[:, h * 128:(h + 1) * 128], rhs=v0, start=(i == 0 and h == 0), stop=(i == n - 1 and h == 3), skip_group_check=True), reads=[p_b] + v_bufs, writes=[boacc], accum=(i > 0 or h > 0))
            if on_exp is not None:
                on_exp(i, t, p_ap, p_b)

        for i in range(min(2, n)):
            S(i)
        for i in range(n):
            E(i)
            if i + 2 < n:
                S(i + 2)
            V(i)
        return oacc, boacc

    def finalize(k2, g, br, oacc, boacc, ak):
        wc = wcs[br]; bwc = b_wc[br]
        P.op("vector", lambda e: e.tensor_scalar(out=wc[:], in0=oacc[:, :, 64], scalar1=1e-30, scalar2=None, op0=ALU.max), reads=[boacc], writes=[bwc])
        P.op("vector", lambda e: e.reciprocal(out=wc[:], in_=wc[:]), reads=[bwc], writes=[bwc])
        if debug:
            P.op("vector", lambda e: e.tensor_tensor(out=dbg_t[:, br, 4 * g:4 * g + 4, :], in0=oacc[:, :, 0:64], in1=wc[:].unsqueeze(2).to_broadcast([128, 4, 64]), op=ALU.mult), reads=[boacc, bwc], writes=[b_dbg])
        P.op("vector", lambda e: e.tensor_tensor(out=wc[:], in0=wc[:], in1=gts[k2][:, 4 * g:4 * g + 4, br], op=ALU.mult), reads=[bwc, b_sl[k2]], writes=[bwc])
        dst = acc[ak][:, 4 * g:4 * g + 4, :]
        wb = wc[:].unsqueeze(2).to_broadcast([128, 4, 64])
        if br == 0:
            P.op("vector", lambda e: e.tensor_tensor(out=dst, in0=oacc[:, :, 0:64], in1=wb, op=ALU.mult), reads=[boacc, bwc], writes=[b_acc[ak]])
        else:
            tmp = acc_tmp
            P.op("vector", lambda e: e.tensor_tensor(out=tmp[:], in0=oacc[:, :, 0:64], in1=wb, op=ALU.mult), reads=[boacc, bwc], writes=[b_acctmp])
            P.op("gpsimd", lambda e: e.tensor_tensor(out=dst, in0=dst, in1=tmp[:], op=ALU.add), reads=[b_acctmp, b_acc[ak]], writes=[b_acc[ak]])

    acc_tmp = P.sb("acc_tmp", [128, 4, 64], F32); b_acctmp = P.buf()
    dbg_t = P.sb("dbg_t", [128, 3, 8, 64], F32); b_dbg = P.buf()

    def do_slot(s):
        k2 = s % 2; j = s // 2
        KT = 16 * j + 8 if s % 2 == 0 else 16 * j + 16
        rag0 = KT - 8
        if s + 1 < nslots:
            load_slot(s + 1)
        for g in range(2):
            gp = slice(g * 64, (g + 1) * 64)
            def cmask(nt):
                if nt >= j - 1:
                    mi = nt - (j - 1)
                    return [("head", ident[:], msk[k2][:, mi, :], [b_c, b_sl[k2]])]
                return []

            def imp_mm(i, nt, p_ap, p_b, nn=j + 1):
                for h in range(4):
                    P.op("tensor", lambda e, h=h: e.matmul(imps[h // 2][:, h % 2, :], lhsT=p_ap[:, h * 128:(h + 1) * 128], rhs=ov[:, nt, :], start=(i == 0 and h % 2 == 0), stop=(i == nn - 1 and h % 2 == 1), skip_group_check=True), reads=[p_b, b_c], writes=[b_imp], accum=(i > 0 or h > 0))

            oacc, boacc = branch(k2, g, list(range(j + 1)), lambda nt: KcT[gp, nt * 128:(nt + 1) * 128], [b_KcT], lambda nt: Vc[:, nt, g, :], [b_Vc], cmask,
                                 lambda i: (eT[:, i, :], b_eT[i]), on_exp=imp_mm)
            P.op("vector", lambda e: e.tensor_scalar(out=rd[:, 0:2], in0=imps[0][:, :, 255], scalar1=1e-30, scalar2=None, op0=ALU.max), reads=[b_imp], writes=[b_rd])
            P.op("vector", lambda e: e.tensor_scalar(out=rd[:, 2:4], in0=imps[1][:, :, 255], scalar1=1e-30, scalar2=None, op0=ALU.max), reads=[b_imp], writes=[b_rd])
            P.op("vector", lambda e: e.reciprocal(out=rd[:], in_=rd[:]), reads=[b_rd], writes=[b_rd])
            P.op("vector", lambda e: e.scalar_tensor_tensor(out=score[:], in0=imps[0][:, 0, :], scalar=rd[:, 0:1], in1=sbias[k2][:], op0=ALU.mult, op1=ALU.add), reads=[b_imp, b_rd, b_sl[k2]], writes=[b_score])
            for h in range(1, 4):
                P.op("vector", lambda e, h=h: e.scalar_tensor_tensor(out=score[:], in0=imps[h // 2][:, h % 2, :], scalar=rd[:, h:h + 1], in1=score[:], op0=ALU.mult, op1=ALU.add), reads=[b_imp, b_rd, b_score], writes=[b_score])
            P.op("vector", lambda e: e.max(out=m8[:, 0:8], in_=score[:]), reads=[b_score], writes=[b_m8])
            P.op("vector", lambda e: e.match_replace(out=sc2[:], in_to_replace=m8[:, 0:8], in_values=score[:], imm_value=-3e38), reads=[b_score, b_m8], writes=[b_sc2])
            P.op("vector", lambda e: e.max(out=m8[:, 8:16], in_=sc2[:]), reads=[b_sc2], writes=[b_m8])
            P.op("vector", lambda e: e.tensor_scalar(out=negsel[:], in0=score[:], scalar1=m8[:, 15:16], scalar2=NEGM, op0=ALU.is_lt, op1=ALU.mult), reads=[b_score, b_m8], writes=[b_negsel])
            for hf in range(2):
                P.op("tensor", lambda e, hf=hf: e.transpose(out=tp[:, hf, :], in_=negsel[:, hf * 128:(hf + 1) * 128], identity=ident[:]), reads=[b_negsel, b_c], writes=[b_tp], accum=(hf == 1))
            P.op("vector", lambda e: e.tensor_copy(out=nsT4[:], in_=tp[:].unsqueeze(2).to_broadcast([128, 2, 4, 128])), reads=[b_tp], writes=[b_ns])
            finalize(k2, g, 0, oacc, boacc, k2)
            oacc, boacc = branch(k2, g, list(range(5)), lambda w: KwT[k2][gp, w * 128:(w + 1) * 128], [], lambda w: Vw[k2][:, w, g, :], [b_sl[k2]],
                                 lambda w: [("head", ident[:], msk[k2][:, 10 + w, :], [b_c, b_sl[k2]])],
                                 lambda i: (pTs[cnt_p(i)][:], b_pT[cnt_p(i)]))
            finalize(k2, g, 2, oacc, boacc, k2)
            def smask(kt):
                ex = [("full", IndAll[:, kt % 64, :], nsT4[:, kt // 64].rearrange("p a b -> p (a b)"), [b_c, b_ns])]
                if kt >= rag0:
                    ex.append(("head", ident[:], msk[k2][:, 2 + kt - rag0, :], [b_c, b_sl[k2]]))
                return ex
            oacc, boacc = branch(k2, g, list(range(KT)), lambda kt: KsT[gp, kt * 128:(kt + 1) * 128], [b_KsT], lambda kt: Vs[:, kt, g, :], [b_Vs], smask,
                                 lambda i: (pTs[cnt_p(i)][:], b_pT[cnt_p(i)]))
            finalize(k2, g, 1, oacc, boacc, k2)
        P.dma("sync", lambda e: e.dma_start(out=attn_o[s * 128:(s + 1) * 128, :], in_=acc[k2][:].rearrange("p a b -> p (a b)")), f"ao{k2}", reads=[b_acc[k2]])
        if debug:
            P.dma("sync", lambda e: e.dma_start(out=dbg_o[s * 128:(s + 1) * 128, :], in_=dbg_t[:].rearrange("p a b c -> p (a b c)")), "dbg", reads=[b_dbg])

    def cnt_p(i):
        return i % 4

    load_slot(0)
    for s in range(nslots):
        do_slot(s)
    P.emit()
    return nc


D = 1024; TPC = 2048; DFF = 2816
EPS = 1e-6
CH = 512
NCH = TPC // CH
TPCH = CH // 128

def build_C(halo=True, nchunks=NCH, stages=(1, 2, 3), v=0):
    nc = bass.Bass("TRN2", target_bir_lowering=False)
    din = lambda n, sh, dt=F32: nc.dram_tensor(n, sh, dt, kind="ExternalInput").ap()
    dout = lambda n, sh, dt=F32: nc.dram_tensor(n, sh, dt, kind="ExternalOutput").ap()
    h_d = din("h", [TPC + 128, D])
    attn_d = din("attn", [TPC + 128, 512])
    mlpn_d = din("mlpn", [TPC + 128, 512], BF16)
    p_d = din("p", [TPC, 256])
    gattn_d = din("gattn", [128, 512]); gpost_d = din("gpost", [128, D]); gpre_d = din("gpre", [128, D])
    gpffn_d = din("gpffn", [128, D]); gple_d = din("gple", [128, D])
    conv_d = din("conv", [128, 44 * 4])
    wo_d = din("wo", [128, 8 * D], BF16)
    wup_d = din("wup", [22, 128, 2 * 8 * 128], BF16)
    wdn_d = din("wdn", [128, 22 * D], BF16)
    wg_d = din("wg", [128, 8 * D], BF16)
    wp_d = din("wp", [128, 2 * D], BF16)
    id_d = din("ident", [128, 128], BF16)
    out_d = dout("hout", [TPC, D])
    P = Prog(nc)
    wo = P.sb("wo", [128, 8, D], BF16); wdn = P.sb("wdn", [128, 22, D], BF16); wg = P.sb("wg", [128, 8, D], BF16); wp = P.sb("wp", [128, 2, D], BF16)
    b_w = P.buf()
    gattn = P.sb("gattn", [128, 512], F32); gpost = P.sb("gpost", [128, D], F32); gpre = P.sb("gpre", [128, D], F32)
    gpffn = P.sb("gpffn", [128, D], F32); gple = P.sb("gple", [128, D], F32)
    conv = P.sb("conv", [128, 44, 4], F32); ident = P.sb("ident", [128, 128], BF16)
    b_c = P.buf()
    for (t, d_, q) in [(wo, wo_d, "sync"), (wdn, wdn_d, "gpsimd"), (wg, wg_d, "sync"), (wp, wp_d, "gpsimd")]:
        P.dma(q, lambda e, t=t, d_=d_: e.dma_start(out=t[:].rearrange("p a b -> p (a b)"), in_=d_), "cw" + q, writes=[b_w])
    for (t, d_) in [(gattn, gattn_d), (gpost, gpost_d), (gpre, gpre_d), (gpffn, gpffn_d), (gple, gple_d), (ident, id_d)]:
        P.dma("sync", lambda e, t=t, d_=d_: e.dma_start(out=t[:], in_=d_), "cc", writes=[b_c])
    P.dma("sync", lambda e: e.dma_start(out=conv[:].rearrange("p a b -> p (a b)"), in_=conv_d), "cc", writes=[b_c])
    psT = P.ps("psT", [128, 8, 128], BF16); b_psT = P.buf()
    M = [P.ps(f"M{i}", [128, 512], F32) for i in range(2)]; b_M = P.buf()
    U = [P.ps(f"U{i}", [128, 512], F32) for i in range(4)]; b_U = [P.buf() for _ in range(4)]
    h1c = P.sb("h1c", [128, TPCH, D], F32); b_h1 = [P.buf() for _ in range(TPCH)]
    hh = P.sb("hh", [128, D], F32); b_hh = P.buf()
    hnT = P.sb("hnT", [128, 8, CH], BF16); b_hnT = [P.buf() for _ in range(TPCH)]
    hnTh = P.sb("hnTh", [128, 8, 2], BF16); b_hnTh = P.buf()
    actT = P.sb("actT", [128, 22, CH], BF16); b_actT = [P.buf() for _ in range(22)]
    carry = P.sb("carry", [128, 44, 2], F32); b_carry = [P.buf() for _ in range(22)]
    hup = [[P.sb(f"hup{s}{x}", [128, CH + 2], F32) for x in range(2)] for s in range(2)]; b_hup = [[P.buf() for x in range(2)] for s in range(2)]
    cgu = [[P.sb(f"cgu{s}{x}", [128, CH], F32) for x in range(2)] for s in range(2)]; b_cgu = [[P.buf() for x in range(2)] for s in range(2)]
    wub = [P.sb(f"wub{i}", [128, 2, 8, 128], BF16) for i in range(2)]; b_wub = [P.buf() for _ in range(2)]
    att = [P.sb(f"att{i}", [128, 512], F32) for i in range(2)]; mlb = [P.sb(f"mlb{i}", [128, 512], BF16) for i in range(2)]; b_in = [P.buf() for _ in range(2)]
    pin = [P.sb(f"pin{i}", [128, 256], F32) for i in range(2)]; b_pin = [P.buf() for _ in range(2)]
    xb = P.sb("xb", [128, D], BF16); b_xb = P.buf()
    xT = P.sb("xT", [128, 8, 128], BF16); b_xT = P.buf()
    pb = P.sb("pb", [128, 256], BF16); b_pb = P.buf()
    pT = P.sb("pT", [128, 2, 128], BF16); b_pT = P.buf()
    tmp = P.sb("tmp", [128, D], F32); b_tmp = P.buf()
    junk = P.sb("junk", [128, D], BF16); b_junk = P.buf()
    st = P.sb("st", [128, 16], F32); b_st = P.buf()
    cnt = {"w": 0, "in": 0, "p": 0}

    def rstd(src_ap, nparts, width, col, reads):
        P.op("scalar", lambda e: e.activation(out=junk[0:nparts, 0:width], in_=src_ap, func=AF.Square, accum_out=st[0:nparts, col:col + 1]), reads=reads, writes=[b_junk, b_st])
        P.op("scalar", lambda e: e.activation(out=st[0:nparts, col + 1:col + 2], in_=st[0:nparts, col:col + 1], func=AF.Sqrt, scale=1.0 / width, bias=EPS), reads=[b_st], writes=[b_st])
        P.op("vector", lambda e: e.reciprocal(out=st[0:nparts, col + 2:col + 3], in_=st[0:nparts, col + 1:col + 2]), reads=[b_st], writes=[b_st])
        return st[0:nparts, col + 2:col + 3]

    def rstd_psum(nparts, col, reads):
        P.op("scalar", lambda e: e.activation(out=junk[0:nparts, 0:512], in_=M[0][0:nparts, :], func=AF.Square, accum_out=st[0:nparts, col:col + 1]), reads=reads, writes=[b_junk, b_st])
        P.op("scalar", lambda e: e.activation(out=junk[0:nparts, 512:1024], in_=M[1][0:nparts, :], func=AF.Square, accum_out=st[0:nparts, col + 3:col + 4]), reads=reads, writes=[b_junk, b_st])
        P.op("vector", lambda e: e.tensor_tensor(out=st[0:nparts, col:col + 1], in0=st[0:nparts, col:col + 1], in1=st[0:nparts, col + 3:col + 4], op=ALU.add), reads=[b_st], writes=[b_st])
        P.op("scalar", lambda e: e.activation(out=st[0:nparts, col + 1:col + 2], in_=st[0:nparts, col:col + 1], func=AF.Sqrt, scale=1.0 / D, bias=EPS), reads=[b_st], writes=[b_st])
        P.op("vector", lambda e: e.reciprocal(out=st[0:nparts, col + 2:col + 3], in_=st[0:nparts, col + 1:col + 2]), reads=[b_st], writes=[b_st])
        return st[0:nparts, col + 2:col + 3]

    def transposes(src_bf, nparts, nk, dstT, b_dst, reads):
        for k in range(nk):
            P.op("tensor", lambda e, k=k: e.transpose(out=psT[:, k, 0:nparts], in_=src_bf[0:nparts, k * 128:(k + 1) * 128], identity=ident[0:nparts, 0:nparts]), reads=reads + [b_c], writes=[b_psT])
        P.op("vector", lambda e: e.tensor_copy(out=dstT, in_=psT[:, 0:nk, 0:nparts]), reads=[b_psT], writes=[b_dst])

    def mm1024(lhsT_fn, nk, w_t, nparts, reads):
        for nch in range(2):
            for k in range(nk):
                P.op("tensor", lambda e, k=k, nch=nch: e.matmul(M[nch][0:nparts, :], lhsT=lhsT_fn(k), rhs=w_t[:, k, nch * 512:(nch + 1) * 512], start=(k == 0), stop=(k == nk - 1)), reads=reads + [b_w], writes=[b_M])

    def load_in(row0, nparts):
        k = cnt["in"] % 2; cnt["in"] += 1
        P.dma("sync", lambda e: e.dma_start(out=att[k][0:nparts, :], in_=attn_d[row0:row0 + nparts, :]), f"in{k}", writes=[b_in[k]])
        P.dma("sync", lambda e: e.dma_start(out=mlb[k][0:nparts, :], in_=mlpn_d[row0:row0 + nparts, :]), f"in{k}", writes=[b_in[k]])
        return k

    def stage1(h_ap, b_h, row0, nparts, dstT, b_dst):
        k = load_in(row0, nparts)
        r = rstd(att[k][0:nparts, :], nparts, 512, 0, [b_in[k]])
        P.op("vector", lambda e: e.scalar_tensor_tensor(out=xb[0:nparts, 0:512], in0=att[k][0:nparts, :], scalar=r, in1=gattn[0:nparts, :], op0=ALU.mult, op1=ALU.mult), reads=[b_in[k], b_st, b_c], writes=[b_xb])
        P.op("gpsimd", lambda e: e.tensor_copy(out=xb[0:nparts, 512:1024], in_=mlb[k][0:nparts, :]), reads=[b_in[k]], writes=[b_xb])
        transposes(xb, nparts, 8, xT[:, :, 0:nparts], b_xT, [b_xb])
        mm1024(lambda kk: xT[:, kk, 0:nparts], 8, wo, nparts, [b_xT])
        r2 = rstd_psum(nparts, 4, [b_M])
        for nch in range(2):
            P.op("vector", lambda e, nch=nch: e.scalar_tensor_tensor(out=tmp[0:nparts, nch * 512:(nch + 1) * 512], in0=M[nch][0:nparts, :], scalar=r2, in1=gpost[0:nparts, nch * 512:(nch + 1) * 512], op0=ALU.mult, op1=ALU.mult), reads=[b_M, b_st, b_c], writes=[b_tmp])
        P.op("gpsimd", lambda e: e.tensor_tensor(out=h_ap, in0=h_ap, in1=tmp[0:nparts, :], op=ALU.add), reads=[b_tmp, b_h], writes=[b_h])
        r3 = rstd(h_ap, nparts, D, 8, [b_h])
        P.op("vector", lambda e: e.scalar_tensor_tensor(out=xb[0:nparts, :], in0=h_ap, scalar=r3, in1=gpre[0:nparts, :], op0=ALU.mult, op1=ALU.mult), reads=[b_h, b_st, b_c], writes=[b_xb])
        transposes(xb, nparts, 8, dstT, b_dst, [b_xb])

    def load_w(m):
        k = cnt["w"] % 2; cnt["w"] += 1
        P.dma("gpsimd" if k else "sync", lambda e: e.dma_start(out=wub[k][:].rearrange("p a b c -> p (a b c)"), in_=wup_d[m]), f"wu{k}", writes=[b_wub[k]])
        return k

    def stage2_halo():
        pend = load_w(0)
        for m in range(22):
            k = pend
            if m + 1 < 22:
                pend = load_w(m + 1)
            for x in range(2):
                for kt in range(8):
                    P.op("tensor", lambda e, x=x, kt=kt, m=m, k=k: e.matmul(U[0][:, (x * 22 + m) * 2:(x * 22 + m) * 2 + 2], lhsT=wub[k][:, x, kt, :], rhs=hnTh[:, kt, :], start=(kt == 0), stop=(kt == 7), skip_group_check=True), reads=[b_wub[k], b_hnTh], writes=[b_U[0]])
        P.op("vector", lambda e: e.tensor_copy(out=carry[:].rearrange("p a b -> p (a b)"), in_=U[0][:, 0:88]), reads=[b_U[0]], writes=b_carry)

    def stage2():
        pend = load_w(0)
        for m in range(22):
            k = pend
            if m + 1 < 22:
                pend = load_w(m + 1)
            s = m % 2
            for x in range(2):
                ub = U[s * 2 + x]; bub = b_U[s * 2 + x]
                for kt in range(8):
                    P.op("tensor", lambda e, x=x, kt=kt, ub=ub, k=k: e.matmul(ub[:], lhsT=wub[k][:, x, kt, :], rhs=hnT[:, kt, :], start=(kt == 0), stop=(kt == 7)), reads=[b_wub[k]] + b_hnT, writes=[bub])
            for x in range(2):
                ub = U[s * 2 + x]; bub = b_U[s * 2 + x]
                cg = cgu[s][x]; bcg = b_cgu[s][x]
                ci = x * 22 + m
                bca = b_carry[m]
                P.op("vector", lambda e, cg=cg, ub=ub, ci=ci: e.tensor_scalar(out=cg[:], in0=ub[:], scalar1=conv[:, ci, 2:3], scalar2=conv[:, ci, 3:4], op0=ALU.mult, op1=ALU.add), reads=[bub, b_c], writes=[bcg])
                P.op("vector", lambda e, cg=cg, ub=ub, ci=ci: e.scalar_tensor_tensor(out=cg[:, 1:CH], in0=ub[:, 0:CH - 1], scalar=conv[:, ci, 1:2], in1=cg[:, 1:CH], op0=ALU.mult, op1=ALU.add), reads=[bub, bcg, b_c], writes=[bcg])
                P.op("vector", lambda e, cg=cg, ub=ub, ci=ci: e.scalar_tensor_tensor(out=cg[:, 2:CH], in0=ub[:, 0:CH - 2], scalar=conv[:, ci, 0:1], in1=cg[:, 2:CH], op0=ALU.mult, op1=ALU.add), reads=[bub, bcg, b_c], writes=[bcg])
                P.op("vector", lambda e, cg=cg, ci=ci: e.scalar_tensor_tensor(out=cg[:, 0:2], in0=carry[:, ci, :], scalar=conv[:, ci, 0:1], in1=cg[:, 0:2], op0=ALU.mult, op1=ALU.add), reads=[bca, bcg, b_c], writes=[bcg])
                P.op("vector", lambda e, cg=cg, ci=ci: e.scalar_tensor_tensor(out=cg[:, 0:1], in0=carry[:, ci, 1:2], scalar=conv[:, ci, 1:2], in1=cg[:, 0:1], op0=ALU.mult, op1=ALU.add), reads=[bca, bcg, b_c], writes=[bcg])
                P.op("vector", lambda e, ub=ub, ci=ci: e.tensor_copy(out=carry[:, ci, :], in_=ub[:, CH - 2:CH]), reads=[bub, bcg], writes=[bca])
            cg = cgu[s][0]; cu = cgu[s][1]
            P.op("scalar", lambda e, cg=cg: e.activation(out=cg[:], in_=cg[:], func=AF.Silu), reads=[b_cgu[s][0]], writes=[b_cgu[s][0]])
            P.op("gpsimd", lambda e, cg=cg, cu=cu, m=m: e.tensor_tensor(out=actT[:, m, :], in0=cg[:], in1=cu[:], op=ALU.mult), reads=[b_cgu[s][0], b_cgu[s][1]], writes=[b_actT[m]])

    def load_h(ti, row0):
        P.dma("sync", lambda e: e.dma_start(out=h1c[:, ti, :], in_=h_d[row0:row0 + 128, :]), f"hl{ti%2}", writes=[b_h1[ti]])

    def stage3(ti, row0):
        h_ap = h1c[:, ti, :]; b_h = b_h1[ti]
        kp = cnt["p"] % 2; cnt["p"] += 1
        P.dma("sync", lambda e: e.dma_start(out=pin[kp][:], in_=p_d[row0:row0 + 128, :]), f"pl{kp}", writes=[b_pin[kp]])
        for nch in range(2):
            for m in range(22):
                P.op("tensor", lambda e, m=m, nch=nch: e.matmul(M[nch][:], lhsT=actT[:, m, ti * 128:(ti + 1) * 128], rhs=wdn[:, m, nch * 512:(nch + 1) * 512], start=(m == 0), stop=(m == 21)), reads=[b_actT[m], b_w], writes=[b_M])
        r = rstd_psum(128, 4, [b_M])
        for nch in range(2):
            P.op("vector", lambda e, nch=nch: e.scalar_tensor_tensor(out=tmp[:, nch * 512:(nch + 1) * 512], in0=M[nch][:], scalar=r, in1=gpffn[:, nch * 512:(nch + 1) * 512], op0=ALU.mult, op1=ALU.mult), reads=[b_M, b_st, b_c], writes=[b_tmp])
        P.op("gpsimd", lambda e: e.tensor_tensor(out=h_ap, in0=h_ap, in1=tmp[:], op=ALU.add), reads=[b_tmp, b_h], writes=[b_h])
        r3 = rstd(h_ap, 128, D, 8, [b_h])
        P.op("vector", lambda e: e.scalar_tensor_tensor(out=xb[:], in0=h_ap, scalar=r3, in1=gple[:], op0=ALU.mult, op1=ALU.mult), reads=[b_h, b_st, b_c], writes=[b_xb])
        transposes(xb, 128, 8, xT[:], b_xT, [b_xb])
        mm1024(lambda kk: xT[:, kk, :], 8, wg, 128, [b_xT])
        for nch in range(2):
            P.op("scalar", lambda e, nch=nch: e.activation(out=tmp[:, nch * 512:(nch + 1) * 512], in_=M[nch][:], func=AF.Sigmoid), reads=[b_M], writes=[b_tmp])
        P.op("gpsimd", lambda e: e.tensor_copy(out=pb[:], in_=pin[kp][:]), reads=[b_pin[kp]], writes=[b_pb])
        transposes(pb, 128, 2, pT[:], b_pT, [b_pb])
        for nch in range(2):
            for k in range(2):
                P.op("tensor", lambda e, k=k, nch=nch: e.matmul(U[nch][:], lhsT=pT[:, k, :], rhs=wp[:, k, nch * 512:(nch + 1) * 512], start=(k == 0), stop=(k == 1)), reads=[b_pT, b_w], writes=[b_U[nch]])
            P.op("vector", lambda e, nch=nch: e.tensor_tensor(out=tmp[:, nch * 512:(nch + 1) * 512], in0=tmp[:, nch * 512:(nch + 1) * 512], in1=U[nch][:], op=ALU.mult), reads=[b_tmp, b_U[nch]], writes=[b_tmp])
        P.op("gpsimd", lambda e: e.tensor_tensor(out=h_ap, in0=h_ap, in1=tmp[:], op=ALU.add), reads=[b_tmp, b_h], writes=[b_h])
        P.dma("sync", lambda e: e.dma_start(out=out_d[row0:row0 + 128, :], in_=h_ap), f"so{ti%2}", reads=[b_h])

    if halo:
        P.dma("sync", lambda e: e.dma_start(out=hh[0:2, :], in_=h_d[TPC:TPC + 2, :]), "hl0", writes=[b_hh])
        stage1(hh[0:2, :], b_hh, TPC, 2, hnTh[:], b_hnTh)
        stage2_halo()
    else:
        P.op("vector", lambda e: e.memset(carry[:], 0.0), writes=b_carry)
    for ci in range(nchunks):
        for ti in range(TPCH):
            row0 = ci * CH + ti * 128
            load_h(ti, row0)
            if 1 in stages:
                stage1(h1c[:, ti, :], b_h1[ti], row0, 128, hnT[:, :, ti * 128:(ti + 1) * 128], b_hnT[ti])
        if 2 in stages:
            stage2()
        for ti in range(TPCH):
            if 3 in stages:
                stage3(ti, ci * CH + ti * 128)
            else:
                P.dma("sync", lambda e, ti=ti, ci=ci: e.dma_start(out=out_d[ci * CH + ti * 128:ci * CH + ti * 128 + 128, :], in_=h1c[:, ti, :]), f"so{ti%2}", reads=[b_h1[ti]])
    P.emit()
    return nc

S = 16384
PERM = np.concatenate([np.arange(0, 512), np.arange(768, 896), np.arange(1024, 1152), np.arange(512, 640), np.arange(640, 768), np.arange(896, 1024), np.arange(1152, 1280), np.arange(1280, 2328)])

def ktile(w):
    K, N = w.shape
    return np.ascontiguousarray(w.reshape(K // 128, 128, N).transpose(1, 0, 2)).reshape(128, -1)

def rope_tables(pos):
    half = 8
    inv = (np.float32(500000.0) ** (-np.arange(half, dtype=np.float32) / np.float32(half))).astype(np.float32)
    ang = pos.astype(np.float32)[:, None] * inv[None, :]
    return np.cos(ang).astype(np.float32), np.sin(ang).astype(np.float32)

def bc(v, n=128):
    return np.ascontiguousarray(np.broadcast_to(v[None, :], (n, v.shape[0]))).astype(np.float32)

def A_inputs(h, i, inp, wbf):
    cos, sin = rope_tables(np.arange(S))
    tri = (np.arange(128)[:, None] <= np.arange(128)[None, :]).astype(BF)
    maps = []
    for c in range(8):
        sl = slice(c * 2048, (c + 1) * 2048)
        t = lambda a: np.ascontiguousarray(a[sl].reshape(16, 128, 8).transpose(1, 0, 2)).reshape(128, 128)
        maps.append(dict(h=np.ascontiguousarray(h[sl]), gpre=bc(inp["pre_mix_g"][i]), w=wbf["w_in"], cos=t(cos), sin=t(sin),
                         lng=bc(inp["gmlp_ln_g"][i]), lnb=bc(inp["gmlp_ln_b"][i]), gmlp=bc(inp["mlp_out_g"][i]),
                         bsT=np.ascontiguousarray(inp["gmlp_bs"][i].T).astype(np.float32), wsT=wbf["wsT"], tri=tri, ident=np.eye(128, dtype=BF)))
    return maps

S = 16384
NEGM = -30000.0

def slot_qi(c, s):
    j = s // 2
    return 16 * j + c if s % 2 == 0 else 16 * j + 15 - c

_CONST = {}
def B_consts():
    if _CONST:
        return _CONST
    n = np.arange(1024)
    jb = np.arange(256)
    ov = ((16 * n[:, None] + 31 >= 64 * jb[None, :]) & (16 * n[:, None] <= 64 * jb[None, :] + 63)).astype(np.float32)
    ov[1023, :] = 0
    ov[:, 255] = 1.0
    _CONST["ov"] = np.ascontiguousarray(ov.reshape(8, 128, 256).transpose(1, 0, 2)).reshape(128, -1).astype(BF)
    ind = np.zeros((128, 64, 128), np.float32)
    for jj in range(64):
        ind[2 * jj, jj, :64] = 1; ind[2 * jj + 1, jj, 64:] = 1
    _CONST["ind"] = ind.reshape(128, -1).astype(BF)
    _CONST["ident"] = np.eye(128, dtype=BF)
    cosc, sinc = rope_tables(16 * np.arange(1024) + 31)
    t = lambda a: np.ascontiguousarray(a.reshape(8, 128, 8).transpose(1, 0, 2)).reshape(128, 64)
    _CONST["cosc"] = t(cosc); _CONST["sinc"] = t(sinc)
    kq = np.arange(128)
    msks = []; sbs = []
    for c in range(8):
        m = np.zeros((16, 128, 15, 128), np.float32)
        sb = np.zeros((16, 128, 256), np.float32)
        for s in range(16):
            qi = slot_qi(c, s); j = s // 2
            tq = 128 * qi + kq
            for mi, nt in enumerate((j - 1, j)):
                if nt < 0: continue
                nn = 128 * nt + kq
                ok = (16 * nn[:, None] + 31 <= tq[None, :]) & (nn[:, None] <= 1022)
                m[s, :, mi, :] = np.where(ok, 0.0, NEGM)
            KT = 16 * j + 8 if s % 2 == 0 else 16 * j + 16
            for r in range(8):
                kt = KT - 8 + r
                tk = 128 * kt + kq
                ok = tk[:, None] <= tq[None, :]
                m[s, :, 2 + r, :] = np.where(ok, 0.0, NEGM)
            for w in range(5):
                tk = 128 * qi - 512 + 128 * w + kq
                ok = (tk[:, None] >= 0) & (tk[:, None] <= tq[None, :]) & (tk[:, None] > tq[None, :] - 512)
                m[s, :, 10 + w, :] = np.where(ok, 0.0, NEGM)
            cur = tq // 64
            valid = jb[None, :] <= cur[:, None]
            forced = valid & ((jb[None, :] == 0) | (jb[None, :] == cur[:, None]) | (jb[None, :] == cur[:, None] - 1))
            sb[s] = np.where(forced, 1000.0, np.where(valid, 0.0, -1e29))
        msks.append(m.reshape(16, 128, -1).astype(BF)); sbs.append(sb)
    _CONST["msk"] = msks; _CONST["sbias"] = sbs
    return _CONST

def B_inputs(qkv, gates, i, inp, wbf):
    C = B_consts()
    q = qkv[:, 0:512].reshape(S, 2, 4, 64)
    ks = qkv[:, 512:640].reshape(S, 2, 64); kw = qkv[:, 640:768].reshape(S, 2, 64)
    zkc = qkv[:, 768:896].reshape(S, 2, 64); zvc = qkv[:, 896:1024].reshape(S, 2, 64)
    vs = qkv[:, 1024:1152].reshape(S, 2, 64); vw = qkv[:, 1152:1280].reshape(S, 2, 64)
    KsT = np.ascontiguousarray(ks.transpose(1, 2, 0)).reshape(128, S)
    one = np.ones((S, 2, 1), BF)
    Vs1 = np.concatenate([vs, one], -1)
    Vs = np.ascontiguousarray(Vs1.reshape(128, 128, 2, 65).transpose(1, 0, 2, 3)).reshape(128, -1)
    kwp = np.concatenate([np.zeros((512, 2, 64), BF), kw], 0)
    vwp = np.concatenate([np.zeros((512, 2, 65), BF), np.concatenate([vw, one], -1)], 0)
    F = np.zeros((2, 2, 128, 16, 1024), BF)
    for X, zz in enumerate((zkc, zvc)):
        zp = np.concatenate([zz, np.zeros((32, 2, 64), BF)], 0)
        n = np.arange(1024); jp = np.arange(16); jj = np.arange(2)
        tidx = 16 * n[None, None, :] + 2 * jp[None, :, None] + jj[:, None, None]
        g_ = zp[tidx]
        g_[:, :, 1023] = 0
        F[X] = g_.transpose(3, 0, 4, 1, 2).reshape(2, 128, 16, 1024)
    Fq = np.ascontiguousarray(F.reshape(2, 2, 128, 16, 4, 256).transpose(0, 1, 4, 2, 3, 5)).reshape(2, 2, 4, 128, 16 * 256)
    b1T = np.stack([np.ascontiguousarray(inp["cmp_b1"][i][X].reshape(2, 128).T) for X in range(2)]).astype(np.float32)
    b2 = np.stack([np.broadcast_to(inp["cmp_b2"][i][X][None, :], (128, 64)) for X in range(2)]).astype(np.float32)
    maps = []
    for c in range(8):
        qis = [slot_qi(c, s) for s in range(16)]
        QT = np.stack([np.ascontiguousarray(q[128 * qi:128 * qi + 128].transpose(1, 3, 2, 0)).reshape(128, 512) for qi in qis])
        KwT = np.stack([np.ascontiguousarray(kwp[128 * qi:128 * qi + 640].transpose(1, 2, 0)).reshape(128, 640) for qi in qis])
        Vw = np.stack([np.ascontiguousarray(vwp[128 * qi:128 * qi + 640].reshape(5, 128, 2, 65).transpose(1, 0, 2, 3)).reshape(128, -1) for qi in qis])
        gt = np.stack([gates[128 * qi:128 * qi + 128] for qi in qis]).astype(np.float32)
        maps.append(dict(QT=QT, KsT=KsT, Vs=Vs, KwT=KwT, Vw=Vw, F=Fq, gates=gt, sbias=C["sbias"][c], msk=C["msk"][c],
                         w1=wbf["cmp_w1"], w2=wbf["cmp_w2"], b1T=b1T, peT=wbf["cmp_peT"], b2=np.ascontiguousarray(b2),
                         cosc=C["cosc"], sinc=C["sinc"], ov=C["ov"], ind=C["ind"], ident=C["ident"]))
    return maps

def B_gather(results):
    attn = np.zeros((S, 512), np.float32)
    for c in range(8):
        a = results[c]["attn"]
        for s in range(16):
            qi = slot_qi(c, s)
            attn[128 * qi:128 * qi + 128] = a[128 * s:128 * s + 128]
    return attn

def B_weights_f32(inp, i):
    w1 = np.stack([ktile(inp["cmp_w1"][i][X]) for X in range(2)])
    w2 = np.stack([ktile(inp["cmp_w2"][i][X]) for X in range(2)])
    pe = inp["cmp_pe"][i]
    peT = np.stack([np.ascontiguousarray(pe[X].reshape(16, 2, 64).transpose(1, 2, 0)).reshape(128, 16) for X in range(2)])
    return w1, w2, peT

S = 16384

def C_weights_f32(inp, i):
    wup = inp["w_up"][i]
    wup_l = np.ascontiguousarray(wup.reshape(8, 128, 2, 22, 128).transpose(3, 1, 2, 0, 4)).reshape(22, 128, -1)
    return dict(wo=ktile(inp["w_o"][i]), wup=wup_l, wdn=ktile(inp["w_down"][i]), wg=ktile(inp["w_ple_gate"][i]), wp=ktile(inp["w_ple_proj"][i]))

def C_inputs(h, attn, mlpn, i, inp, wbf):
    cw = inp["conv_w"][i]; cb = inp["conv_b"][i]
    conv = np.concatenate([cw, cb[None, :]], 0)
    conv = np.ascontiguousarray(conv.reshape(4, 44, 128).transpose(2, 1, 0)).reshape(128, -1).astype(np.float32)
    maps = []
    for c in range(8):
        sl = slice(2048 * c, 2048 * (c + 1))
        def ext(a):
            o = np.zeros((2048 + 128,) + a.shape[1:], a.dtype)
            o[:2048] = a[sl]
            if c > 0:
                o[2048:2050] = a[2048 * c - 2:2048 * c]
            return o
        maps.append(dict(h=ext(h), attn=ext(attn), mlpn=ext(mlpn), p=np.ascontiguousarray(inp["p"][i, 0][sl]),
                         gattn=bc(inp["attn_out_g"][i]), gpost=bc(inp["post_mix_g"][i]), gpre=bc(inp["pre_ffn_g"][i]),
                         gpffn=bc(inp["post_ffn_g"][i]), gple=bc(inp["ple_norm_g"][i]), conv=conv,
                         wo=wbf["wo"], wup=wbf["wup"], wdn=wbf["wdn"], wg=wbf["wg"], wp=wbf["wp"], ident=np.eye(128, dtype=BF)))
    return maps


_PROGS = {}

def _prog(name, fn):
    if name not in _PROGS:
        _PROGS[name] = fn()
    return _PROGS[name]

def _run(nc, maps):
    res = run_bass_kernel_spmd(nc, maps, core_ids=list(range(8)))
    return res.results

WCOLS = [("w_in", 8 * 2328), ("wsT", 1024), ("cmp_w1", 2 * 4096), ("cmp_w2", 2 * 128), ("cmp_peT", 2 * 16),
         ("wo", 8192), ("wup", 22 * 2048), ("wdn", 22 * 1024), ("wg", 8192), ("wp", 2048)]
WTOT = sum(c for _, c in WCOLS)

def _pack_weights(inp, i):
    cw = C_weights_f32(inp, i)
    w1, w2, peT = B_weights_f32(inp, i)
    parts = {
        "w_in": ktile(inp["w_in"][i][:, PERM]),
        "wsT": np.ascontiguousarray(inp["gmlp_ws"][i].transpose(2, 0, 1)).reshape(128, 1024),
        "cmp_w1": np.ascontiguousarray(w1.transpose(1, 0, 2)).reshape(128, -1),
        "cmp_w2": np.ascontiguousarray(w2.transpose(1, 0, 2)).reshape(128, -1),
        "cmp_peT": np.ascontiguousarray(peT.transpose(1, 0, 2)).reshape(128, -1),
        "wo": cw["wo"], "wup": np.ascontiguousarray(cw["wup"].transpose(1, 0, 2)).reshape(128, -1),
        "wdn": cw["wdn"], "wg": cw["wg"], "wp": cw["wp"],
    }
    return np.concatenate([parts[n].astype(np.float32) for n, _ in WCOLS], axis=1)

def _unpack_weights(wb):
    out = {}
    o = 0
    for n, c in WCOLS:
        out[n] = np.ascontiguousarray(wb[:, o:o + c]); o += c
    out["cmp_w1"] = np.ascontiguousarray(out["cmp_w1"].reshape(128, 2, 4096).transpose(1, 0, 2))
    out["cmp_w2"] = np.ascontiguousarray(out["cmp_w2"].reshape(128, 2, 128).transpose(1, 0, 2))
    out["cmp_peT"] = np.ascontiguousarray(out["cmp_peT"].reshape(128, 2, 16).transpose(1, 0, 2))
    out["wup"] = np.ascontiguousarray(out["wup"].reshape(128, 22, 2048).transpose(1, 0, 2))
    return out

def kernel(**inputs):
    inp = {k: np.asarray(v) for k, v in inputs.items()}
    L = 2
    big = np.concatenate([_pack_weights(inp, i) for i in range(L)], axis=1)
    per = big.shape[1] // 8
    assert per * 8 == big.shape[1]
    ncW = _prog("W", lambda: build_W(per))
    resW = _run(ncW, [{"win": np.ascontiguousarray(big[:, c * per:(c + 1) * per])} for c in range(8)])
    wb = np.concatenate([r["wout"] for r in resW], axis=1)
    wbf = [_unpack_weights(wb[:, i * WTOT:(i + 1) * WTOT]) for i in range(L)]
    h = np.ascontiguousarray(inp["x"][0]).astype(np.float32)
    for i in range(L):
        ncA = _prog("A", build_A)
        resA = _run(ncA, A_inputs(h, i, inp, wbf[i]))
        qkv = np.concatenate([r["qkv"] for r in resA], 0)
        gates = np.concatenate([r["gates"] for r in resA], 0)
        mlpn = np.concatenate([r["mlpn"] for r in resA], 0)
        ncB = _prog("B", build_B)
        resB = _run(ncB, B_inputs(qkv, gates, i, inp, wbf[i]))
        attn = B_gather(resB)
        ncC = _prog("C", build_C)
        resC = _run(ncC, C_inputs(h, attn, mlpn, i, inp, wbf[i]))
        h = np.concatenate([r["hout"] for r in resC], 0)
    return h[None].astype(np.float32)
```

```python
import numpy as np
import ml_dtypes
from contextlib import ExitStack
import concourse.bass as bass
import concourse.mybir as mybir
from concourse.bass_utils import run_bass_kernel_spmd

F32 = mybir.dt.float32
BF16 = mybir.dt.bfloat16
AF = mybir.ActivationFunctionType
ALU = mybir.AluOpType
AX = mybir.AxisListType
NPBF16 = ml_dtypes.bfloat16


class Buf:
    __slots__ = ("name", "last_w", "readers")

    def __init__(self, name=""):
        self.name = name
        self.last_w = None
        self.readers = []


class Op:
    __slots__ = ("eng", "fn", "deps", "is_dma", "stream", "needs_inc", "tok", "idx", "dma_upto")

    def __init__(self, eng, fn, is_dma=False, stream=None):
        self.eng = eng
        self.fn = fn
        self.deps = set()
        self.is_dma = is_dma
        self.stream = stream
        self.needs_inc = False
        self.tok = None
        self.idx = -1
        self.dma_upto = {}


class Prog:
    ENGS = ("sync", "scalar", "vector", "gpsimd", "tensor")
    SEM_ROLL = 6000

    def __init__(self, nc):
        self.nc = nc
        self.ops = []
        self.stack = ExitStack()
        self.nbuf = 0

    def buf(self, name=""):
        self.nbuf += 1
        return Buf(name or f"b{self.nbuf}")

    def sb(self, name, shape, dtype):
        return self.stack.enter_context(self.nc.sbuf_tensor("sb_" + name, list(shape), dtype))

    def ps(self, name, shape, dtype=F32):
        return self.stack.enter_context(self.nc.psum_tensor("ps_" + name, list(shape), dtype))

    def op(self, eng, fn, reads=(), writes=(), accum=False):
        o = Op(eng, fn)
        self._deps(o, reads, writes, accum)
        return o

    def dma(self, eng, fn, stream, reads=(), writes=()):
        o = Op(eng, fn, is_dma=True, stream=stream)
        self._deps(o, reads, writes, False)
        return o

    def _deps(self, o, reads, writes, accum):
        o.idx = len(self.ops)
        for b in reads:
            if b.last_w is not None:
                o.deps.add(b.last_w)
        for b in writes:
            if b.last_w is not None:
                lw = self.ops[b.last_w]
                if not (accum and lw.eng == o.eng and not lw.is_dma):
                    o.deps.add(b.last_w)
            for r in b.readers:
                o.deps.add(r)
        for b in reads:
            b.readers.append(o.idx)
        for b in writes:
            b.last_w = o.idx
            b.readers = []
        o.deps.discard(o.idx)
        if o.eng == "tensor" and not o.is_dma:
            o.deps = {d for d in o.deps if not (self.ops[d].eng == "tensor" and not self.ops[d].is_dma)}
        self.ops.append(o)

    def emit(self):
        nc = self.nc
        ops = self.ops
        for o in ops:
            for d in o.deps:
                ops[d].needs_inc = True
        sems = {}

        def new_sem(tag):
            return self.stack.enter_context(nc.semaphore(f"s_{tag}_{len(sems)}"))

        eng_sem = {}
        eng_cnt = {}
        stream_sem = {}
        stream_cnt = {}
        stream_hist = {}
        for o in ops:
            if o.is_dma:
                if o.stream not in stream_sem:
                    stream_sem[o.stream] = new_sem("d")
                    sems[len(sems)] = 1
                    stream_cnt[o.stream] = 0
                    stream_hist[o.stream] = []
                stream_cnt[o.stream] += 16
                o.tok = (stream_sem[o.stream], stream_cnt[o.stream])
                stream_hist[o.stream].append(o.idx)
            elif o.needs_inc:
                if o.eng not in eng_sem or eng_cnt[o.eng] >= self.SEM_ROLL:
                    eng_sem[o.eng] = new_sem(o.eng[0])
                    sems[len(sems)] = 1
                    eng_cnt[o.eng] = 0
                eng_cnt[o.eng] += 1
                o.tok = (eng_sem[o.eng], eng_cnt[o.eng])
        self.n_sems = len(sems)
        import bisect
        seen = {e: {} for e in self.ENGS}
        waits = [None] * len(ops)
        for o in ops:
            need = {}
            for d in o.deps:
                do = ops[d]
                sem, val = do.tok
                if do.is_dma:
                    h = stream_hist[do.stream]
                    k = bisect.bisect_left(h, o.idx)
                    val = 16 * k
                key = id(sem)
                if key not in need or need[key][1] < val:
                    need[key] = (sem, val)
            w = []
            s = seen[o.eng]
            for key, (sem, val) in need.items():
                if s.get(key, 0) < val:
                    s[key] = val
                    w.append((sem, val))
            waits[o.idx] = w
        with nc.Block() as block:
            def make(engname):
                def body(e):
                    for o in ops:
                        if o.eng != engname:
                            continue
                        for sem, val in waits[o.idx]:
                            e.wait_ge(sem, val)
                        ins = o.fn(e)
                        if o.is_dma:
                            ins.then_inc(o.tok[0], 16)
                        elif o.needs_inc:
                            ins.then_inc(o.tok[0], 1)
                    if engname == "sync":
                        for st, sem in stream_sem.items():
                            e.wait_ge(sem, stream_cnt[st])
                return body
            block.sync(make("sync"))
            block.scalar(make("scalar"))
            block.vector(make("vector"))
            block.gpsimd(make("gpsimd"))
            block.tensor(make("tensor"))
        self.stack.close()

BF = NPBF16

S = 16384; D = 1024; NCORE = 8; TPC = 2048; NTILE = 16
INW = 2328
EPS = 1e-6

def build_W(ncols, chunk=4096):
    nc = bass.Bass("TRN2", target_bir_lowering=False)
    win = nc.dram_tensor("win", [128, ncols], F32, kind="ExternalInput").ap()
    wout = nc.dram_tensor("wout", [128, ncols], BF16, kind="ExternalOutput").ap()
    P = Prog(nc)
    nb = 3
    st = [P.sb(f"st{i}", [128, chunk], F32) for i in range(nb)]
    sb = [P.sb(f"sb{i}", [128, chunk], BF16) for i in range(nb)]
    b_st = [P.buf() for _ in range(nb)]
    b_sb = [P.buf() for _ in range(nb)]
    engs = ["vector", "gpsimd", "vector"]
    i = 0
    for c0 in range(0, ncols, chunk):
        c1 = min(ncols, c0 + chunk); w = c1 - c0; k = i % nb
        P.dma("sync", lambda e, k=k, c0=c0, c1=c1, w=w: e.dma_start(out=st[k][:, 0:w], in_=win[:, c0:c1]), f"wl{k}", writes=[b_st[k]])
        en = engs[i % 3]
        if en == "scalar":
            P.op(en, lambda e, k=k, w=w: e.copy(out=sb[k][:, 0:w], in_=st[k][:, 0:w]), reads=[b_st[k]], writes=[b_sb[k]])
        else:
            P.op(en, lambda e, k=k, w=w: e.tensor_copy(out=sb[k][:, 0:w], in_=st[k][:, 0:w]), reads=[b_st[k]], writes=[b_sb[k]])
        P.dma("sync", lambda e, k=k, c0=c0, c1=c1, w=w: e.dma_start(out=wout[:, c0:c1], in_=sb[k][:, 0:w]), f"ws{k}", reads=[b_sb[k]])
        i += 1
    P.emit()
    return nc


def build_A():
    nc = bass.Bass("TRN2", target_bir_lowering=False)
    din = lambda n, sh, dt=F32: nc.dram_tensor(n, sh, dt, kind="ExternalInput").ap()
    dout = lambda n, sh, dt=F32: nc.dram_tensor(n, sh, dt, kind="ExternalOutput").ap()
    h_d = din("h", [TPC, D])
    gpre_d = din("gpre", [128, D])
    w_d = din("w", [128, 8 * INW], BF16)
    cos_d = din("cos", [128, NTILE * 8]); sin_d = din("sin", [128, NTILE * 8])
    lng_d = din("lng", [128, 512]); lnb_d = din("lnb", [128, 512]); gmlp_d = din("gmlp", [128, 512])
    bsT_d = din("bsT", [128, 8])
    wsT_d = din("wsT", [128, 8 * 128], BF16)
    tri_d = din("tri", [128, 128], BF16)
    id_d = din("ident", [128, 128], BF16)
    qkv_o = dout("qkv", [TPC, 1280], BF16)
    gates_o = dout("gates", [TPC, 24])
    mlpn_o = dout("mlpn", [TPC, 512], BF16)
    P = Prog(nc)
    W = P.sb("W", [128, 8, INW], BF16); b_W = P.buf()
    gpre = P.sb("gpre", [128, D], F32); b_c = P.buf()
    cos = P.sb("cos", [128, NTILE, 8], F32); sin = P.sb("sin", [128, NTILE, 8], F32)
    lng = P.sb("lng", [128, 512], F32); lnb = P.sb("lnb", [128, 512], F32); gmlp = P.sb("gmlpg", [128, 512], F32)
    bsT = P.sb("bsT", [128, 8], F32)
    wsT = P.sb("wsT", [128, 8, 128], BF16); b_ws = P.buf()
    tri = P.sb("tri", [128, 128], BF16)
    ident = P.sb("ident", [128, 128], BF16)
    for kt in range(8):
        P.dma("sync" if kt % 2 == 0 else "gpsimd", lambda e, kt=kt: e.dma_start(out=W[:, kt, :], in_=w_d[:, kt * INW:(kt + 1) * INW]), f"cw{kt%2}", writes=[b_W])
    for (t, d_) in [(gpre, gpre_d), (lng, lng_d), (lnb, lnb_d), (gmlp, gmlp_d), (bsT, bsT_d), (tri, tri_d), (ident, id_d)]:
        P.dma("sync", lambda e, t=t, d_=d_: e.dma_start(out=t[:], in_=d_), "cc", writes=[b_c])
    P.dma("sync", lambda e: e.dma_start(out=cos[:].rearrange("p a b -> p (a b)"), in_=cos_d), "cc", writes=[b_c])
    P.dma("sync", lambda e: e.dma_start(out=sin[:].rearrange("p a b -> p (a b)"), in_=sin_d), "cc", writes=[b_c])
    P.dma("sync", lambda e: e.dma_start(out=wsT[:].rearrange("p a b -> p (a b)"), in_=wsT_d), "cc", writes=[b_ws])
    P.op("vector", lambda e: e.tensor_tensor(out=wsT[:], in0=wsT[:], in1=tri[:].unsqueeze(1).to_broadcast([128, 8, 128]), op=ALU.mult), reads=[b_ws, b_c], writes=[b_ws])

    NB = 2
    hs = [P.sb(f"hs{i}", [128, D], F32) for i in range(NB)]; b_hs = [P.buf() for _ in range(NB)]
    junk = P.sb("junk", [128, D], BF16); b_junk = P.buf()
    st = [P.sb(f"stat{i}", [128, 8], F32) for i in range(NB)]; b_st = [P.buf() for _ in range(NB)]
    a_bf = P.sb("a_bf", [128, D], BF16); b_a = P.buf()
    aT = P.sb("aT", [128, 8, 128], BF16); b_aT = P.buf()
    pT = P.ps("pT", [128, 8, 128], BF16); b_pT = P.buf()
    zps = [P.ps(f"zps{i}", [128, 512], F32) for i in range(3)]; b_zps = [P.buf() for _ in range(3)]
    mixps = P.ps("mixps", [128, 512], F32); b_mix = P.buf()
    z = P.sb("z", [128, INW], F32); b_z = [P.buf() for _ in range(5)]
    qkv = [P.sb(f"qkv{i}", [128, 1280], BF16) for i in range(NB)]; b_qkv = [P.buf() for _ in range(NB)]
    rt = P.sb("rt", [128, 4, 12, 8], F32); b_rt = P.buf()
    gt = [P.sb(f"gt{i}", [128, 24], F32) for i in range(NB)]; b_gt = [P.buf() for _ in range(NB)]
    u = P.sb("u", [128, 512], F32); b_u = P.buf()
    v = P.sb("v", [128, 512], F32); b_v = P.buf()
    bnst = P.sb("bnst", [128, 6], F32); b_bn = P.buf()
    mv = P.sb("mv", [128, 4], F32); b_mv = P.buf()
    vn = P.sb("vn", [128, 512], BF16); b_vn = P.buf()
    m1 = P.sb("m1", [128, 512], F32); b_m1 = P.buf()
    mlpn = [P.sb(f"mlpn{i}", [128, 512], BF16) for i in range(NB)]; b_mlpn = [P.buf() for _ in range(NB)]
    chunks = [(0, 512), (512, 512), (1024, 512), (1536, 512), (2048, 280)]

    def load(ti):
        k = ti % NB
        P.dma("sync", lambda e: e.dma_start(out=hs[k][:], in_=h_d[ti * 128:(ti + 1) * 128, :]), f"hl{k}", writes=[b_hs[k]])

    def do_tile(ti):
        k = ti % NB
        if ti + 1 < NTILE:
            load(ti + 1)
        s_ = st[k]
        P.op("scalar", lambda e: e.activation(out=junk[:], in_=hs[k][:], func=AF.Square, accum_out=s_[:, 0:1]), reads=[b_hs[k]], writes=[b_junk, b_st[k]])
        P.op("scalar", lambda e: e.activation(out=s_[:, 1:2], in_=s_[:, 0:1], func=AF.Sqrt, scale=1.0 / D, bias=EPS), reads=[b_st[k]], writes=[b_st[k]])
        P.op("vector", lambda e: e.reciprocal(out=s_[:, 2:3], in_=s_[:, 1:2]), reads=[b_st[k]], writes=[b_st[k]])
        P.op("vector", lambda e: e.scalar_tensor_tensor(out=a_bf[:], in0=hs[k][:], scalar=s_[:, 2:3], in1=gpre[:], op0=ALU.mult, op1=ALU.mult), reads=[b_hs[k], b_st[k], b_c], writes=[b_a])
        for kt in range(8):
            P.op("tensor", lambda e, kt=kt: e.transpose(out=pT[:, kt, :], in_=a_bf[:, kt * 128:(kt + 1) * 128], identity=ident[:]), reads=[b_a, b_c], writes=[b_pT], accum=True)
        P.op("vector", lambda e: e.tensor_copy(out=aT[:], in_=pT[:]), reads=[b_pT], writes=[b_aT])
        for ci, (c0, cw) in enumerate(chunks):
            zp = zps[ci % 3]; bz = b_zps[ci % 3]
            for kt in range(8):
                P.op("tensor", lambda e, kt=kt, zp=zp, c0=c0, cw=cw: e.matmul(zp[:, 0:cw], lhsT=aT[:, kt, :], rhs=W[:, kt, c0:c0 + cw], start=(kt == 0), stop=(kt == 7)), reads=[b_aT, b_W], writes=[bz], accum=True)
            if ci % 2 == 0:
                P.op("scalar", lambda e, zp=zp, c0=c0, cw=cw: e.copy(out=z[:, c0:c0 + cw], in_=zp[:, 0:cw]), reads=[bz], writes=[b_z[ci]])
            else:
                P.op("vector", lambda e, zp=zp, c0=c0, cw=cw: e.tensor_copy(out=z[:, c0:c0 + cw], in_=zp[:, 0:cw]), reads=[bz], writes=[b_z[ci]])
        P.op("gpsimd", lambda e: e.tensor_copy(out=qkv[k][:], in_=z[:, 0:1280]), reads=[b_z[0], b_z[1], b_z[2]], writes=[b_qkv[k]])
        zr = z[:, 0:768].rearrange("p (h d) -> p h d", d=64)
        qr = qkv[k][:, 0:768].rearrange("p (h d) -> p h d", d=64)
        cb = cos[:, ti, :].unsqueeze(1).to_broadcast([128, 12, 8]); sb_ = sin[:, ti, :].unsqueeze(1).to_broadcast([128, 12, 8])
        x1 = zr[:, :, 0:8]; x2 = zr[:, :, 8:16]
        P.op("vector", lambda e: e.tensor_tensor(out=rt[:, 0], in0=x1, in1=cb, op=ALU.mult), reads=[b_z[0], b_z[1], b_c], writes=[b_rt])
        P.op("vector", lambda e: e.tensor_tensor(out=rt[:, 1], in0=x2, in1=sb_, op=ALU.mult), reads=[b_z[0], b_z[1], b_c], writes=[b_rt])
        P.op("vector", lambda e: e.tensor_tensor(out=rt[:, 2], in0=x2, in1=cb, op=ALU.mult), reads=[b_z[0], b_z[1], b_c], writes=[b_rt])
        P.op("vector", lambda e: e.tensor_tensor(out=rt[:, 3], in0=x1, in1=sb_, op=ALU.mult), reads=[b_z[0], b_z[1], b_c], writes=[b_rt])
        P.op("vector", lambda e: e.tensor_tensor(out=qr[:, :, 0:8], in0=rt[:, 0], in1=rt[:, 1], op=ALU.subtract), reads=[b_rt], writes=[b_qkv[k]])
        P.op("vector", lambda e: e.tensor_tensor(out=qr[:, :, 8:16], in0=rt[:, 2], in1=rt[:, 3], op=ALU.add), reads=[b_rt], writes=[b_qkv[k]])
        P.dma("sync", lambda e: e.dma_start(out=qkv_o[ti * 128:(ti + 1) * 128, :], in_=qkv[k][:]), f"so{k}", reads=[b_qkv[k]])
        P.op("scalar", lambda e: e.activation(out=gt[k][:], in_=z[:, 1280:1304], func=AF.Sigmoid), reads=[b_z[2]], writes=[b_gt[k]])
        P.dma("sync", lambda e: e.dma_start(out=gates_o[ti * 128:(ti + 1) * 128, :], in_=gt[k][:]), f"so{k}", reads=[b_gt[k]])
        P.op("scalar", lambda e: e.activation(out=u[:], in_=z[:, 1304:1816], func=AF.Gelu_apprx_tanh), reads=[b_z[2], b_z[3]], writes=[b_u])
        P.op("scalar", lambda e: e.activation(out=v[:], in_=z[:, 1816:2328], func=AF.Gelu_apprx_tanh), reads=[b_z[3], b_z[4]], writes=[b_v])
        P.op("vector", lambda e: e.bn_stats(out=bnst[:], in_=v[:]), reads=[b_v], writes=[b_bn])
        P.op("vector", lambda e: e.bn_aggr(out=mv[:, 0:2], in_=bnst[:]), reads=[b_bn], writes=[b_mv])
        P.op("scalar", lambda e: e.activation(out=mv[:, 2:3], in_=mv[:, 1:2], func=AF.Sqrt, scale=1.0, bias=EPS), reads=[b_mv], writes=[b_mv])
        P.op("vector", lambda e: e.reciprocal(out=mv[:, 3:4], in_=mv[:, 2:3]), reads=[b_mv], writes=[b_mv])
        P.op("vector", lambda e: e.tensor_scalar(out=v[:], in0=v[:], scalar1=mv[:, 0:1], scalar2=mv[:, 3:4], op0=ALU.subtract, op1=ALU.mult), reads=[b_v, b_mv], writes=[b_v])
        P.op("gpsimd", lambda e: e.tensor_tensor(out=v[:], in0=v[:], in1=lng[:], op=ALU.mult), reads=[b_v, b_c], writes=[b_v])
        P.op("gpsimd", lambda e: e.tensor_tensor(out=vn[:], in0=v[:], in1=lnb[:], op=ALU.add), reads=[b_v, b_c], writes=[b_vn])
        for g in range(8):
            P.op("tensor", lambda e, g=g: e.matmul(mixps[:, g * 64:(g + 1) * 64], lhsT=wsT[:, g, :], rhs=vn[:, g * 64:(g + 1) * 64], start=True, stop=True), reads=[b_ws, b_vn], writes=[b_mix], accum=True)
        P.op("vector", lambda e: e.tensor_tensor(out=m1[:].rearrange("p (g d) -> p g d", d=64), in0=mixps[:].rearrange("p (g d) -> p g d", d=64), in1=bsT[:].unsqueeze(2).to_broadcast([128, 8, 64]), op=ALU.add), reads=[b_mix, b_c], writes=[b_m1])
        P.op("vector", lambda e: e.tensor_tensor(out=m1[:], in0=m1[:], in1=u[:], op=ALU.mult), reads=[b_m1, b_u], writes=[b_m1])
        P.op("scalar", lambda e: e.activation(out=junk[:, 0:512], in_=m1[:], func=AF.Square, accum_out=s_[:, 4:5]), reads=[b_m1], writes=[b_junk, b_st[k]])
        P.op("scalar", lambda e: e.activation(out=s_[:, 5:6], in_=s_[:, 4:5], func=AF.Sqrt, scale=1.0 / 512, bias=EPS), reads=[b_st[k]], writes=[b_st[k]])
        P.op("vector", lambda e: e.reciprocal(out=s_[:, 6:7], in_=s_[:, 5:6]), reads=[b_st[k]], writes=[b_st[k]])
        P.op("vector", lambda e: e.scalar_tensor_tensor(out=mlpn[k][:], in0=m1[:], scalar=s_[:, 6:7], in1=gmlp[:], op0=ALU.mult, op1=ALU.mult), reads=[b_m1, b_st[k], b_c], writes=[b_mlpn[k]])
        P.dma("sync", lambda e: e.dma_start(out=mlpn_o[ti * 128:(ti + 1) * 128, :], in_=mlpn[k][:]), f"so{k}", reads=[b_mlpn[k]])
    load(0)
    for ti in range(NTILE):
        do_tile(ti)
    P.emit()
    return nc


S = 16384
NEGM = -30000.0
SCALE = 0.125

def slot_qi(c, s):
    j = s // 2
    return 16 * j + c if s % 2 == 0 else 16 * j + 15 - c

def build_B(nslots=16, debug=False):
    nc = bass.Bass("TRN2", target_bir_lowering=False)
    din = lambda n, sh, dt=F32: nc.dram_tensor(n, sh, dt, kind="ExternalInput").ap()
    dout = lambda n, sh, dt=F32: nc.dram_tensor(n, sh, dt, kind="ExternalOutput").ap()
    QT_d = din("QT", [nslots, 128, 2 * 512], BF16)
    KsT_d = din("KsT", [128, S], BF16)
    Vs_d = din("Vs", [128, 128 * 2 * 65], BF16)
    KwT_d = din("KwT", [nslots, 128, 640], BF16)
    Vw_d = din("Vw", [nslots, 128, 5 * 2 * 65], BF16)
    F_d = din("F", [2, 2, 4, 128, 16 * 256], BF16)
    gates_d = din("gates", [nslots, 128, 24])
    sbias_d = din("sbias", [nslots, 128, 256])
    msk_d = din("msk", [nslots, 128, 15 * 128], BF16)
    w1_d = din("w1", [2, 128, 16 * 256], BF16)
    w2_d = din("w2", [2, 128, 2 * 64], BF16)
    b1T_d = din("b1T", [2, 128, 2])
    peT_d = din("peT", [2, 128, 16], BF16)
    b2_d = din("b2", [2, 128, 64])
    cosc_d = din("cosc", [128, 64]); sinc_d = din("sinc", [128, 64])
    ov_d = din("ov", [128, 8 * 256], BF16)
    ind_d = din("ind", [128, 64 * 128], BF16)
    id_d = din("ident", [128, 128], BF16)
    attn_o = dout("attn", [nslots * 128, 512])
    dbg_o = dout("dbg", [nslots * 128, 3 * 512]) if debug else None
    P = Prog(nc)
    KsT = P.sb("KsT", [128, S], BF16); b_KsT = P.buf()
    Vs = P.sb("Vs", [128, 128, 2, 65], BF16); b_Vs = P.buf()
    IndAll = P.sb("IndAll", [128, 64, 128], BF16)
    ov = P.sb("ov", [128, 8, 256], BF16)
    ident = P.sb("ident", [128, 128], BF16)
    cosc = P.sb("cosc", [128, 8, 8], F32); sinc = P.sb("sinc", [128, 8, 8], F32)
    b_c = P.buf()
    w1 = P.sb("w1", [128, 2, 16, 256], BF16); w2 = P.sb("w2", [128, 2, 2, 64], BF16)
    b1T = P.sb("b1T", [128, 2, 2], F32); peT = P.sb("peT", [128, 2, 16], BF16); b2 = P.sb("b2", [128, 2, 64], F32)
    b_cw = P.buf()
    KcT = P.sb("KcT", [128, 1024], BF16); b_KcT = P.buf()
    Vc = P.sb("Vc", [128, 8, 2, 65], BF16); b_Vc = P.buf()
    for X in range(2):
        P.dma("sync", lambda e, X=X: e.dma_start(out=w1[:, X].rearrange("p a b -> p (a b)"), in_=w1_d[X]), "cc", writes=[b_cw])
        P.dma("sync", lambda e, X=X: e.dma_start(out=w2[:, X].rearrange("p a b -> p (a b)"), in_=w2_d[X]), "cc", writes=[b_cw])
        P.dma("sync", lambda e, X=X: e.dma_start(out=b1T[:, X], in_=b1T_d[X]), "cc", writes=[b_cw])
        P.dma("sync", lambda e, X=X: e.dma_start(out=peT[:, X], in_=peT_d[X]), "cc", writes=[b_cw])
        P.dma("sync", lambda e, X=X: e.dma_start(out=b2[:, X], in_=b2_d[X]), "cc", writes=[b_cw])
    P.dma("sync", lambda e: e.dma_start(out=ident[:], in_=id_d), "cc", writes=[b_c])
    P.dma("sync", lambda e: e.dma_start(out=cosc[:].rearrange("p a b -> p (a b)"), in_=cosc_d), "cc", writes=[b_c])
    P.dma("sync", lambda e: e.dma_start(out=sinc[:].rearrange("p a b -> p (a b)"), in_=sinc_d), "cc", writes=[b_c])
    P.dma("sync", lambda e: e.dma_start(out=ov[:].rearrange("p a b -> p (a b)"), in_=ov_d), "cc", writes=[b_c])
    P.dma("gpsimd", lambda e: e.dma_start(out=IndAll[:].rearrange("p a b -> p (a b)"), in_=ind_d), "cc2", writes=[b_c])
    for q4 in range(4):
        P.dma("gpsimd", lambda e, q4=q4: e.dma_start(out=KsT[:, q4 * 4096:(q4 + 1) * 4096], in_=KsT_d[:, q4 * 4096:(q4 + 1) * 4096]), "cc2", writes=[b_KsT])
        P.dma("gpsimd", lambda e, q4=q4: e.dma_start(out=Vs[:, q4 * 32:(q4 + 1) * 32].rearrange("p a b c -> p (a b c)"), in_=Vs_d[:, q4 * 32 * 130:(q4 + 1) * 32 * 130]), "cc2", writes=[b_Vs])
    sTs = [P.ps(f"sT{i}", [128, 512], F32) for i in range(3)]; b_sT = [P.buf() for _ in range(3)]
    oaccs = [P.ps(f"oacc{i}", [128, 4, 128], F32) for i in range(2)]; b_oacc = [P.buf() for _ in range(2)]
    imps = [P.ps(f"imp{i}", [128, 2, 256], F32) for i in range(2)]; b_imp = P.buf()
    tp = P.ps("tp", [128, 2, 128], BF16); b_tp = P.buf()
    cps = sTs[2][:, 0:64]; b_cps = b_sT[2]
    b1ps = sTs[2][:, 64:66]; b_b1ps = b_sT[2]
    Fb = [P.sb(f"Fb{i}", [128, 16, 256], BF16) for i in range(2)]; b_Fb = [P.buf() for _ in range(2)]
    hT = P.sb("hT", [128, 2, 256], BF16); b_hT = [P.buf() for _ in range(2)]
    bias1 = P.sb("bias1", [128, 2, 2], F32); b_bias1 = P.buf()
    kcf = P.sb("kcf", [128, 8, 2, 64], F32); b_kcf = P.buf()
    kcb = P.sb("kcb", [128, 8, 2, 64], BF16); b_kcb = P.buf()
    rt = P.sb("rt", [128, 4, 8, 2, 8], F32); b_rt = P.buf()
    P.op("vector", lambda e: e.memset(Vc[:], 1.0), writes=[b_Vc])
    fi = 0
    for X in range(2):
        for hc in range(2):
            for jp in range(16):
                P.op("tensor", lambda e, X=X, hc=hc, jp=jp: e.matmul(sTs[2][:, 64 + hc:65 + hc], lhsT=w1[:, X, jp, hc * 128:(hc + 1) * 128], rhs=peT[:, X, jp:jp + 1], start=(jp == 0), stop=(jp == 15)), reads=[b_cw], writes=[b_b1ps], accum=True)
        P.op("vector", lambda e, X=X: e.tensor_tensor(out=bias1[:, X], in0=b1ps, in1=b1T[:, X], op=ALU.add), reads=[b_b1ps, b_cw], writes=[b_bias1])
        for g in range(2):
            for nh in range(4):
                fb = Fb[fi % 2]; bfb = b_Fb[fi % 2]
                P.dma("sync", lambda e, fb=fb, X=X, g=g, nh=nh: e.dma_start(out=fb[:].rearrange("p a b -> p (a b)"), in_=F_d[X, g, nh]), f"F{fi%2}", writes=[bfb])
                fi += 1
                for hc in range(2):
                    sT = sTs[hc]
                    for jp in range(16):
                        P.op("tensor", lambda e, X=X, hc=hc, jp=jp, fb=fb, sT=sT: e.matmul(sT[:, 0:256], lhsT=w1[:, X, jp, hc * 128:(hc + 1) * 128], rhs=fb[:, jp, :], start=(jp == 0), stop=(jp == 15)), reads=[b_cw, bfb], writes=[b_sT[hc]], accum=True)
                    P.op("scalar", lambda e, X=X, hc=hc, sT=sT: e.activation(out=hT[:, hc, :], in_=sT[:, 0:256], func=AF.Gelu_apprx_tanh, bias=bias1[:, X, hc:hc + 1], scale=1.0), reads=[b_sT[hc], b_bias1], writes=[b_hT[hc]])
                for ntl in range(2):
                    nt = nh * 2 + ntl
                    for hc in range(2):
                        P.op("tensor", lambda e, X=X, hc=hc, ntl=ntl: e.matmul(cps, lhsT=hT[:, hc, ntl * 128:(ntl + 1) * 128], rhs=w2[:, X, hc, :], start=(hc == 0), stop=(hc == 1)), reads=[b_hT[hc], b_cw], writes=[b_cps], accum=True)
                    if X == 1:
                        P.op("vector", lambda e, nt=nt, g=g: e.tensor_tensor(out=Vc[:, nt, g, 0:64], in0=cps, in1=b2[:, 1], op=ALU.add), reads=[b_cps, b_cw], writes=[b_Vc])
                    else:
                        P.op("vector", lambda e, nt=nt, g=g: e.tensor_tensor(out=kcf[:, nt, g, :], in0=cps, in1=b2[:, 0], op=ALU.add), reads=[b_cps, b_cw], writes=[b_kcf])
        if X == 0:
            P.op("gpsimd", lambda e: e.tensor_copy(out=kcb[:], in_=kcf[:]), reads=[b_kcf], writes=[b_kcb])
            cb = cosc[:].unsqueeze(2).to_broadcast([128, 8, 2, 8]); sb_ = sinc[:].unsqueeze(2).to_broadcast([128, 8, 2, 8])
            x1 = kcf[:, :, :, 0:8]; x2 = kcf[:, :, :, 8:16]
            P.op("vector", lambda e: e.tensor_tensor(out=rt[:, 0], in0=x1, in1=cb, op=ALU.mult), reads=[b_kcf, b_c], writes=[b_rt])
            P.op("vector", lambda e: e.tensor_tensor(out=rt[:, 1], in0=x2, in1=sb_, op=ALU.mult), reads=[b_kcf, b_c], writes=[b_rt])
            P.op("vector", lambda e: e.tensor_tensor(out=rt[:, 2], in0=x2, in1=cb, op=ALU.mult), reads=[b_kcf, b_c], writes=[b_rt])
            P.op("vector", lambda e: e.tensor_tensor(out=rt[:, 3], in0=x1, in1=sb_, op=ALU.mult), reads=[b_kcf, b_c], writes=[b_rt])
            P.op("vector", lambda e: e.tensor_tensor(out=kcb[:, :, :, 0:8], in0=rt[:, 0], in1=rt[:, 1], op=ALU.subtract), reads=[b_rt], writes=[b_kcb])
            P.op("vector", lambda e: e.tensor_tensor(out=kcb[:, :, :, 8:16], in0=rt[:, 2], in1=rt[:, 3], op=ALU.add), reads=[b_rt], writes=[b_kcb])
            for nt in range(8):
                P.op("tensor", lambda e, nt=nt: e.transpose(out=tp[:, 0, :], in_=kcb[:, nt].rearrange("p a b -> p (a b)"), identity=ident[:]), reads=[b_kcb, b_c], writes=[b_tp])
                P.op("vector", lambda e, nt=nt: e.tensor_copy(out=KcT[:, nt * 128:(nt + 1) * 128], in_=tp[:, 0, :]), reads=[b_tp], writes=[b_KcT])
    QT = [P.sb(f"QT{i}", [128, 2, 512], BF16) for i in range(2)]
    gts = [P.sb(f"gts{i}", [128, 8, 3], F32) for i in range(2)]
    sbias = [P.sb(f"sbias{i}", [128, 256], F32) for i in range(2)]
    msk = [P.sb(f"msk{i}", [128, 15, 128], BF16) for i in range(2)]
    KwT = [P.sb(f"KwT{i}", [128, 640], BF16) for i in range(2)]
    Vw = [P.sb(f"Vw{i}", [128, 5, 2, 65], BF16) for i in range(2)]
    b_sl = [P.buf() for _ in range(2)]
    eT = P.sb("eT", [128, 8, 512], BF16); b_eT = [P.buf() for _ in range(8)]
    pTs = [P.sb(f"pT{i}", [128, 512], BF16) for i in range(4)]; b_pT = [P.buf() for _ in range(4)]
    nsT4 = P.sb("nsT4", [128, 2, 4, 128], BF16); b_ns = P.buf()
    score = P.sb("score", [128, 256], F32); b_score = P.buf()
    sc2 = P.sb("sc2", [128, 256], F32); b_sc2 = P.buf()
    m8 = P.sb("m8", [128, 16], F32); b_m8 = P.buf()
    rd = P.sb("rd", [128, 4], F32); b_rd = P.buf()
    negsel = P.sb("negsel", [128, 256], BF16); b_negsel = P.buf()
    wcs = [P.sb(f"wc{i}", [128, 4], F32) for i in range(3)]; b_wc = [P.buf() for _ in range(3)]
    acc = [P.sb(f"acc{i}", [128, 8, 64], F32) for i in range(2)]; b_acc = [P.buf() for _ in range(2)]
    cnt = {"sT": 0, "pT": 0, "oa": 0}

    def load_slot(s):
        k2 = s % 2
        w = [b_sl[k2]]
        st = f"sl{k2}"
        P.dma("sync", lambda e: e.dma_start(out=QT[k2][:].rearrange("p a b -> p (a b)"), in_=QT_d[s]), st, writes=w)
        P.dma("sync", lambda e: e.dma_start(out=gts[k2][:].rearrange("p a b -> p (a b)"), in_=gates_d[s]), st, writes=w)
        P.dma("sync", lambda e: e.dma_start(out=sbias[k2][:], in_=sbias_d[s]), st, writes=w)
        P.dma("sync", lambda e: e.dma_start(out=msk[k2][:].rearrange("p a b -> p (a b)"), in_=msk_d[s]), st, writes=w)
        P.dma("sync", lambda e: e.dma_start(out=KwT[k2][:], in_=KwT_d[s]), st, writes=w)
        P.dma("sync", lambda e: e.dma_start(out=Vw[k2][:].rearrange("p a b c -> p (a b c)"), in_=Vw_d[s]), st, writes=w)

    def branch(k2, g, tiles, lhs_fn, lhs_bufs, v_fn, v_bufs, mask_fn, pbufs, on_exp=None):
        gp = slice(g * 64, (g + 1) * 64)
        oi = cnt["oa"] % 2; cnt["oa"] += 1
        oacc = oaccs[oi]; boacc = b_oacc[oi]
        n = len(tiles)
        sbank = {}

        def S(i):
            t = tiles[i]
            bi = cnt["sT"] % 3; cnt["sT"] += 1
            sbank[i] = bi
            sT = sTs[bi]
            extra = mask_fn(t)
            l0 = lhs_fn(t); rq = QT[k2][:, g, :]
            P.op("tensor", lambda e: e.matmul(sT[:], lhsT=l0, rhs=rq, start=True, stop=(len(extra) == 0)), reads=[b_sl[k2]] + lhs_bufs, writes=[b_sT[bi]])
            for xi, (kind, l_ap, r_ap, rb) in enumerate(extra):
                last = xi == len(extra) - 1
                if kind == "full":
                    P.op("tensor", lambda e, l_ap=l_ap, r_ap=r_ap, last=last: e.matmul(sT[:], lhsT=l_ap, rhs=r_ap, start=False, stop=last), reads=rb, writes=[b_sT[bi]], accum=True)
                else:
                    for h in range(4):
                        P.op("tensor", lambda e, l_ap=l_ap, r_ap=r_ap, last=last, h=h: e.matmul(sT[:, h * 128:(h + 1) * 128], lhsT=l_ap, rhs=r_ap, start=False, stop=(last and h == 3)), reads=rb, writes=[b_sT[bi]], accum=True)

        def E(i):
            t = tiles[i]
            bi = sbank[i]
            p_ap, p_b = pbufs(i)
            P.op("scalar", lambda e: e.activation(out=p_ap, in_=sTs[bi][:], func=AF.Exp, scale=SCALE), reads=[b_sT[bi]], writes=[p_b])

        def V(i):
            t = tiles[i]
            p_ap, p_b = pbufs(i)
            v0 = v_fn(t)
            for h in range(4):
                P.op("tensor", lambda e, h=h: e.matmul(oacc[:, h, 0:65], lhsT=p_ap[:, h * 128:(h + 1) * 128], rhs=v0, start=(i == 0 and h == 0), stop=(i == n - 1 and h == 3), skip_group_check=True), reads=[p_b] + v_bufs, writes=[boacc], accum=(i > 0 or h > 0))
            if on_exp is not None:
                on_exp(i, t, p_ap, p_b)

        for i in range(min(2, n)):
            S(i)
        for i in range(n):
            E(i)
            if i + 2 < n:
                S(i + 2)
            V(i)
        return oacc, boacc

    def finalize(k2, g, br, oacc, boacc, ak):
        wc = wcs[br]; bwc = b_wc[br]
        P.op("vector", lambda e: e.tensor_scalar(out=wc[:], in0=oacc[:, :, 64], scalar1=1e-30, scalar2=None, op0=ALU.max), reads=[boacc], writes=[bwc])
        P.op("vector", lambda e: e.reciprocal(out=wc[:], in_=wc[:]), reads=[bwc], writes=[bwc])
        if debug:
            P.op("vector", lambda e: e.tensor_tensor(out=dbg_t[:, br, 4 * g:4 * g + 4, :], in0=oacc[:, :, 0:64], in1=wc[:].unsqueeze(2).to_broadcast([128, 4, 64]), op=ALU.mult), reads=[boacc, bwc], writes=[b_dbg])
        P.op("vector", lambda e: e.tensor_tensor(out=wc[:], in0=wc[:], in1=gts[k2][:, 4 * g:4 * g + 4, br], op=ALU.mult), reads=[bwc, b_sl[k2]], writes=[bwc])
        dst = acc[ak][:, 4 * g:4 * g + 4, :]
        wb = wc[:].unsqueeze(2).to_broadcast([128, 4, 64])
        if br == 0:
            P.op("vector", lambda e: e.tensor_tensor(out=dst, in0=oacc[:, :, 0:64], in1=wb, op=ALU.mult), reads=[boacc, bwc], writes=[b_acc[ak]])
        else:
            tmp = acc_tmp
            P.op("vector", lambda e: e.tensor_tensor(out=tmp[:], in0=oacc[:, :, 0:64], in1=wb, op=ALU.mult), reads=[boacc, bwc], writes=[b_acctmp])
            P.op("gpsimd", lambda e: e.tensor_tensor(out=dst, in0=dst, in1=tmp[:], op=ALU.add), reads=[b_acctmp, b_acc[ak]], writes=[b_acc[ak]])

    acc_tmp = P.sb("acc_tmp", [128, 4, 64], F32); b_acctmp = P.buf()
    dbg_t = P.sb("dbg_t", [128, 3, 8, 64], F32); b_dbg = P.buf()

    def do_slot(s):
        k2 = s % 2; j = s // 2
        KT = 16 * j + 8 if s % 2 == 0 else 16 * j + 16
        rag0 = KT - 8
        if s + 1 < nslots:
            load_slot(s + 1)
        for g in range(2):
            gp = slice(g * 64, (g + 1) * 64)
            def cmask(nt):
                if nt >= j - 1:
                    mi = nt - (j - 1)
                    return [("head", ident[:], msk[k2][:, mi, :], [b_c, b_sl[k2]])]
                return []

            def imp_mm(i, nt, p_ap, p_b, nn=j + 1):
                for h in range(4):
                    P.op("tensor", lambda e, h=h: e.matmul(imps[h // 2][:, h % 2, :], lhsT=p_ap[:, h * 128:(h + 1) * 128], rhs=ov[:, nt, :], start=(i == 0 and h % 2 == 0), stop=(i == nn - 1 and h % 2 == 1), skip_group_check=True), reads=[p_b, b_c], writes=[b_imp], accum=(i > 0 or h > 0))

            oacc, boacc = branch(k2, g, list(range(j + 1)), lambda nt: KcT[:, nt * 128:(nt + 1) * 128], [b_KcT], lambda nt: Vc[:, nt, g, :], [b_Vc], cmask,
                                 lambda i: (eT[:, i, :], b_eT[i]), on_exp=imp_mm)
            P.op("vector", lambda e: e.tensor_scalar(out=rd[:, 0:2], in0=imps[0][:, :, 255], scalar1=1e-30, scalar2=None, op0=ALU.max), reads=[b_imp], writes=[b_rd])
            P.op("vector", lambda e: e.tensor_scalar(out=rd[:, 2:4], in0=imps[1][:, :, 255], scalar1=1e-30, scalar2=None, op0=ALU.max), reads=[b_imp], writes=[b_rd])
            P.op("vector", lambda e: e.reciprocal(out=rd[:], in_=rd[:]), reads=[b_rd], writes=[b_rd])
            P.op("vector", lambda e: e.scalar_tensor_tensor(out=score[:], in0=imps[0][:, 0, :], scalar=rd[:, 0:1], in1=sbias[k2][:], op0=ALU.mult, op1=ALU.add), reads=[b_imp, b_rd, b_sl[k2]], writes=[b_score])
            for h in range(1, 4):
                P.op("vector", lambda e, h=h: e.scalar_tensor_tensor(out=score[:], in0=imps[h // 2][:, h % 2, :], scalar=rd[:, h:h + 1], in1=score[:], op0=ALU.mult, op1=ALU.add), reads=[b_imp, b_rd, b_score], writes=[b_score])
            P.op("vector", lambda e: e.max(out=m8[:, 0:8], in_=score[:]), reads=[b_score], writes=[b_m8])
            P.op("vector", lambda e: e.match_replace(out=sc2[:], in_to_replace=m8[:, 0:8], in_values=score[:], imm_value=-3e38), reads=[b_score, b_m8], writes=[b_sc2])
            P.op("vector", lambda e: e.max(out=m8[:, 8:16], in_=sc2[:]), reads=[b_sc2], writes=[b_m8])
            P.op("vector", lambda e: e.tensor_scalar(out=negsel[:], in0=score[:], scalar1=m8[:, 15:16], scalar2=NEGM, op0=ALU.is_lt, op1=ALU.mult), reads=[b_score, b_m8], writes=[b_negsel])
            for hf in range(2):
                P.op("tensor", lambda e, hf=hf: e.transpose(out=tp[:, hf, :], in_=negsel[:, hf * 128:(hf + 1) * 128], identity=ident[:]), reads=[b_negsel, b_c], writes=[b_tp], accum=(hf == 1))
            P.op("vector", lambda e: e.tensor_copy(out=nsT4[:], in_=tp[:].unsqueeze(2).to_broadcast([128, 2, 4, 128])), reads=[b_tp], writes=[b_ns])
            finalize(k2, g, 0, oacc, boacc, k2)
            oacc, boacc = branch(k2, g, list(range(5)), lambda w: KwT[k2][:, w * 128:(w + 1) * 128], [], lambda w: Vw[k2][:, w, g, :], [b_sl[k2]],
                                 lambda w: [("head", ident[:], msk[k2][:, 10 + w, :], [b_c, b_sl[k2]])],
                                 lambda i: (pTs[cnt_p(i)][:], b_pT[cnt_p(i)]))
            finalize(k2, g, 2, oacc, boacc, k2)
            def smask(kt):
                ex = [("full", IndAll[:, kt % 64, :], nsT4[:, kt // 64].rearrange("p a b -> p (a b)"), [b_c, b_ns])]
                if kt >= rag0:
                    ex.append(("head", ident[:], msk[k2][:, 2 + kt - rag0, :], [b_c, b_sl[k2]]))
                return ex
            oacc, boacc = branch(k2, g, list(range(KT)), lambda kt: KsT[:, kt * 128:(kt + 1) * 128], [b_KsT], lambda kt: Vs[:, kt, g, :], [b_Vs], smask,
                                 lambda i: (pTs[cnt_p(i)][:], b_pT[cnt_p(i)]))
            finalize(k2, g, 1, oacc, boacc, k2)
        P.dma("sync", lambda e: e.dma_start(out=attn_o[s * 128:(s + 1) * 128, :], in_=acc[k2][:].rearrange("p a b -> p (a b)")), f"ao{k2}", reads=[b_acc[k2]])
        if debug:
            P.dma("sync", lambda e: e.dma_start(out=dbg_o[s * 128:(s + 1) * 128, :], in_=dbg_t[:].rearrange("p a b c -> p (a b c)")), "dbg", reads=[b_dbg])

    def cnt_p(i):
        return i % 4

    load_slot(0)
    for s in range(nslots):
        do_slot(s)
    P.emit()
    return nc


D = 1024; TPC = 2048; DFF = 2816
EPS = 1e-6
CH = 512
NCH = TPC // CH
TPCH = CH // 128

def build_C(halo=True, nchunks=NCH, stages=(1, 2, 3), v=0):
    nc = bass.Bass("TRN2", target_bir_lowering=False)
    din = lambda n, sh, dt=F32: nc.dram_tensor(n, sh, dt, kind="ExternalInput").ap()
    dout = lambda n, sh, dt=F32: nc.dram_tensor(n, sh, dt, kind="ExternalOutput").ap()
    h_d = din("h", [TPC + 128, D])
    attn_d = din("attn", [TPC + 128, 512])
    mlpn_d = din("mlpn", [TPC + 128, 512], BF16)
    p_d = din("p", [TPC, 256])
    gattn_d = din("gattn", [128, 512]); gpost_d = din("gpost", [128, D]); gpre_d = din("gpre", [128, D])
    gpffn_d = din("gpffn", [128, D]); gple_d = din("gple", [128, D])
    conv_d = din("conv", [128, 44 * 4])
    wo_d = din("wo", [128, 8 * D], BF16)
    wup_d = din("wup", [22, 128, 2 * 8 * 128], BF16)
    wdn_d = din("wdn", [128, 22 * D], BF16)
    wg_d = din("wg", [128, 8 * D], BF16)
    wp_d = din("wp", [128, 2 * D], BF16)
    id_d = din("ident", [128, 128], BF16)
    out_d = dout("hout", [TPC, D])
    P = Prog(nc)
    wo = P.sb("wo", [128, 8, D], BF16); wdn = P.sb("wdn", [128, 22, D], BF16); wg = P.sb("wg", [128, 8, D], BF16); wp = P.sb("wp", [128, 2, D], BF16)
    b_w = P.buf()
    gattn = P.sb("gattn", [128, 512], F32); gpost = P.sb("gpost", [128, D], F32); gpre = P.sb("gpre", [128, D], F32)
    gpffn = P.sb("gpffn", [128, D], F32); gple = P.sb("gple", [128, D], F32)
    conv = P.sb("conv", [128, 44, 4], F32); ident = P.sb("ident", [128, 128], BF16)
    b_c = P.buf()
    for (t, d_, q) in [(wo, wo_d, "sync"), (wdn, wdn_d, "gpsimd"), (wg, wg_d, "sync"), (wp, wp_d, "gpsimd")]:
        P.dma(q, lambda e, t=t, d_=d_: e.dma_start(out=t[:].rearrange("p a b -> p (a b)"), in_=d_), "cw" + q, writes=[b_w])
    for (t, d_) in [(gattn, gattn_d), (gpost, gpost_d), (gpre, gpre_d), (gpffn, gpffn_d), (gple, gple_d), (ident, id_d)]:
        P.dma("sync", lambda e, t=t, d_=d_: e.dma_start(out=t[:], in_=d_), "cc", writes=[b_c])
    P.dma("sync", lambda e: e.dma_start(out=conv[:].rearrange("p a b -> p (a b)"), in_=conv_d), "cc", writes=[b_c])
    psT = P.ps("psT", [128, 8, 128], BF16); b_psT = P.buf()
    M = [P.ps(f"M{i}", [128, 512], F32) for i in range(2)]; b_M = P.buf()
    U = [P.ps(f"U{i}", [128, 512], F32) for i in range(4)]; b_U = [P.buf() for _ in range(4)]
    h1c = P.sb("h1c", [128, TPCH, D], F32); b_h1 = [P.buf() for _ in range(TPCH)]
    hh = P.sb("hh", [128, D], F32); b_hh = P.buf()
    hnT = P.sb("hnT", [128, 8, CH], BF16); b_hnT = [P.buf() for _ in range(TPCH)]
    hnTh = P.sb("hnTh", [128, 8, 2], BF16); b_hnTh = P.buf()
    actT = P.sb("actT", [128, 22, CH], BF16); b_actT = [P.buf() for _ in range(22)]
    carry = P.sb("carry", [128, 44, 2], F32); b_carry = [P.buf() for _ in range(22)]
    hup = [[P.sb(f"hup{s}{x}", [128, CH + 2], F32) for x in range(2)] for s in range(2)]; b_hup = [[P.buf() for x in range(2)] for s in range(2)]
    cgu = [[P.sb(f"cgu{s}{x}", [128, CH], F32) for x in range(2)] for s in range(2)]; b_cgu = [[P.buf() for x in range(2)] for s in range(2)]
    wub = [P.sb(f"wub{i}", [128, 2, 8, 128], BF16) for i in range(2)]; b_wub = [P.buf() for _ in range(2)]
    att = [P.sb(f"att{i}", [128, 512], F32) for i in range(2)]; mlb = [P.sb(f"mlb{i}", [128, 512], BF16) for i in range(2)]; b_in = [P.buf() for _ in range(2)]
    pin = [P.sb(f"pin{i}", [128, 256], F32) for i in range(2)]; b_pin = [P.buf() for _ in range(2)]
    xb = P.sb("xb", [128, D], BF16); b_xb = P.buf()
    xT = P.sb("xT", [128, 8, 128], BF16); b_xT = P.buf()
    pb = P.sb("pb", [128, 256], BF16); b_pb = P.buf()
    pT = P.sb("pT", [128, 2, 128], BF16); b_pT = P.buf()
    tmp = P.sb("tmp", [128, D], F32); b_tmp = P.buf()
    junk = P.sb("junk", [128, D], BF16); b_junk = P.buf()
    st = P.sb("st", [128, 16], F32); b_st = P.buf()
    cnt = {"w": 0, "in": 0, "p": 0}

    def rstd(src_ap, nparts, width, col, reads):
        P.op("scalar", lambda e: e.activation(out=junk[0:nparts, 0:width], in_=src_ap, func=AF.Square, accum_out=st[0:nparts, col:col + 1]), reads=reads, writes=[b_junk, b_st])
        P.op("scalar", lambda e: e.activation(out=st[0:nparts, col + 1:col + 2], in_=st[0:nparts, col:col + 1], func=AF.Sqrt, scale=1.0 / width, bias=EPS), reads=[b_st], writes=[b_st])
        P.op("vector", lambda e: e.reciprocal(out=st[0:nparts, col + 2:col + 3], in_=st[0:nparts, col + 1:col + 2]), reads=[b_st], writes=[b_st])
        return st[0:nparts, col + 2:col + 3]

    def rstd_psum(nparts, col, reads):
        P.op("scalar", lambda e: e.activation(out=junk[0:nparts, 0:512], in_=M[0][0:nparts, :], func=AF.Square, accum_out=st[0:nparts, col:col + 1]), reads=reads, writes=[b_junk, b_st])
        P.op("scalar", lambda e: e.activation(out=junk[0:nparts, 512:1024], in_=M[1][0:nparts, :], func=AF.Square, accum_out=st[0:nparts, col + 3:col + 4]), reads=reads, writes=[b_junk, b_st])
        P.op("vector", lambda e: e.tensor_tensor(out=st[0:nparts, col:col + 1], in0=st[0:nparts, col:col + 1], in1=st[0:nparts, col + 3:col + 4], op=ALU.add), reads=[b_st], writes=[b_st])
        P.op("scalar", lambda e: e.activation(out=st[0:nparts, col + 1:col + 2], in_=st[0:nparts, col:col + 1], func=AF.Sqrt, scale=1.0 / D, bias=EPS), reads=[b_st], writes=[b_st])
        P.op("vector", lambda e: e.reciprocal(out=st[0:nparts, col + 2:col + 3], in_=st[0:nparts, col + 1:col + 2]), reads=[b_st], writes=[b_st])
        return st[0:nparts, col + 2:col + 3]

    def transposes(src_bf, nparts, nk, dstT, b_dst, reads):
        for k in range(nk):
            P.op("tensor", lambda e, k=k: e.transpose(out=psT[:, k, 0:nparts], in_=src_bf[0:nparts, k * 128:(k + 1) * 128], identity=ident[0:nparts, 0:nparts]), reads=reads + [b_c], writes=[b_psT])
        P.op("vector", lambda e: e.tensor_copy(out=dstT, in_=psT[:, 0:nk, 0:nparts]), reads=[b_psT], writes=[b_dst])

    def mm1024(lhsT_fn, nk, w_t, nparts, reads):
        for nch in range(2):
            for k in range(nk):
                P.op("tensor", lambda e, k=k, nch=nch: e.matmul(M[nch][0:nparts, :], lhsT=lhsT_fn(k), rhs=w_t[:, k, nch * 512:(nch + 1) * 512], start=(k == 0), stop=(k == nk - 1)), reads=reads + [b_w], writes=[b_M])

    def load_in(row0, nparts):
        k = cnt["in"] % 2; cnt["in"] += 1
        P.dma("sync", lambda e: e.dma_start(out=att[k][0:nparts, :], in_=attn_d[row0:row0 + nparts, :]), f"in{k}", writes=[b_in[k]])
        P.dma("sync", lambda e: e.dma_start(out=mlb[k][0:nparts, :], in_=mlpn_d[row0:row0 + nparts, :]), f"in{k}", writes=[b_in[k]])
        return k

    def stage1(h_ap, b_h, row0, nparts, dstT, b_dst):
        k = load_in(row0, nparts)
        r = rstd(att[k][0:nparts, :], nparts, 512, 0, [b_in[k]])
        P.op("vector", lambda e: e.scalar_tensor_tensor(out=xb[0:nparts, 0:512], in0=att[k][0:nparts, :], scalar=r, in1=gattn[0:nparts, :], op0=ALU.mult, op1=ALU.mult), reads=[b_in[k], b_st, b_c], writes=[b_xb])
        P.op("gpsimd", lambda e: e.tensor_copy(out=xb[0:nparts, 512:1024], in_=mlb[k][0:nparts, :]), reads=[b_in[k]], writes=[b_xb])
        transposes(xb, nparts, 8, xT[:, :, 0:nparts], b_xT, [b_xb])
        mm1024(lambda kk: xT[:, kk, 0:nparts], 8, wo, nparts, [b_xT])
        r2 = rstd_psum(nparts, 4, [b_M])
        for nch in range(2):
            P.op("vector", lambda e, nch=nch: e.scalar_tensor_tensor(out=tmp[0:nparts, nch * 512:(nch + 1) * 512], in0=M[nch][0:nparts, :], scalar=r2, in1=gpost[0:nparts, nch * 512:(nch + 1) * 512], op0=ALU.mult, op1=ALU.mult), reads=[b_M, b_st, b_c], writes=[b_tmp])
        P.op("gpsimd", lambda e: e.tensor_tensor(out=h_ap, in0=h_ap, in1=tmp[0:nparts, :], op=ALU.add), reads=[b_tmp, b_h], writes=[b_h])
        r3 = rstd(h_ap, nparts, D, 8, [b_h])
        P.op("vector", lambda e: e.scalar_tensor_tensor(out=xb[0:nparts, :], in0=h_ap, scalar=r3, in1=gpre[0:nparts, :], op0=ALU.mult, op1=ALU.mult), reads=[b_h, b_st, b_c], writes=[b_xb])
        transposes(xb, nparts, 8, dstT, b_dst, [b_xb])

    def load_w(m):
        k = cnt["w"] % 2; cnt["w"] += 1
        P.dma("gpsimd" if k else "sync", lambda e: e.dma_start(out=wub[k][:].rearrange("p a b c -> p (a b c)"), in_=wup_d[m]), f"wu{k}", writes=[b_wub[k]])
        return k

    def stage2_halo():
        pend = load_w(0)
        for m in range(22):
            k = pend
            if m + 1 < 22:
                pend = load_w(m + 1)
            for x in range(2):
                for kt in range(8):
                    P.op("tensor", lambda e, x=x, kt=kt, m=m, k=k: e.matmul(U[0][:, (x * 22 + m) * 2:(x * 22 + m) * 2 + 2], lhsT=wub[k][:, x, kt, :], rhs=hnTh[:, kt, :], start=(kt == 0), stop=(kt == 7), skip_group_check=True), reads=[b_wub[k], b_hnTh], writes=[b_U[0]])
        P.op("vector", lambda e: e.tensor_copy(out=carry[:].rearrange("p a b -> p (a b)"), in_=U[0][:, 0:88]), reads=[b_U[0]], writes=b_carry)

    def stage2():
        pend = load_w(0)
        for m in range(22):
            k = pend
            if m + 1 < 22:
                pend = load_w(m + 1)
            s = m % 2
            for x in range(2):
                ub = U[s * 2 + x]; bub = b_U[s * 2 + x]
                for kt in range(8):
                    P.op("tensor", lambda e, x=x, kt=kt, ub=ub, k=k: e.matmul(ub[:], lhsT=wub[k][:, x, kt, :], rhs=hnT[:, kt, :], start=(kt == 0), stop=(kt == 7)), reads=[b_wub[k]] + b_hnT, writes=[bub])
            for x in range(2):
                ub = U[s * 2 + x]; bub = b_U[s * 2 + x]
                cg = cgu[s][x]; bcg = b_cgu[s][x]
                ci = x * 22 + m
                bca = b_carry[m]
                P.op("vector", lambda e, cg=cg, ub=ub, ci=ci: e.tensor_scalar(out=cg[:], in0=ub[:], scalar1=conv[:, ci, 2:3], scalar2=conv[:, ci, 3:4], op0=ALU.mult, op1=ALU.add), reads=[bub, b_c], writes=[bcg])
                P.op("vector", lambda e, cg=cg, ub=ub, ci=ci: e.scalar_tensor_tensor(out=cg[:, 1:CH], in0=ub[:, 0:CH - 1], scalar=conv[:, ci, 1:2], in1=cg[:, 1:CH], op0=ALU.mult, op1=ALU.add), reads=[bub, bcg, b_c], writes=[bcg])
                P.op("vector", lambda e, cg=cg, ub=ub, ci=ci: e.scalar_tensor_tensor(out=cg[:, 2:CH], in0=ub[:, 0:CH - 2], scalar=conv[:, ci, 0:1], in1=cg[:, 2:CH], op0=ALU.mult, op1=ALU.add), reads=[bub, bcg, b_c], writes=[bcg])
                P.op("vector", lambda e, cg=cg, ci=ci: e.scalar_tensor_tensor(out=cg[:, 0:2], in0=carry[:, ci, :], scalar=conv[:, ci, 0:1], in1=cg[:, 0:2], op0=ALU.mult, op1=ALU.add), reads=[bca, bcg, b_c], writes=[bcg])
                P.op("vector", lambda e, cg=cg, ci=ci: e.scalar_tensor_tensor(out=cg[:, 0:1], in0=carry[:, ci, 1:2], scalar=conv[:, ci, 1:2], in1=cg[:, 0:1], op0=ALU.mult, op1=ALU.add), reads=[bca, bcg, b_c], writes=[bcg])
                P.op("vector", lambda e, ub=ub, ci=ci: e.tensor_copy(out=carry[:, ci, :], in_=ub[:, CH - 2:CH]), reads=[bub, bcg], writes=[bca])
            cg = cgu[s][0]; cu = cgu[s][1]
            P.op("scalar", lambda e, cg=cg: e.activation(out=cg[:], in_=cg[:], func=AF.Silu), reads=[b_cgu[s][0]], writes=[b_cgu[s][0]])
            P.op("gpsimd", lambda e, cg=cg, cu=cu, m=m: e.tensor_tensor(out=actT[:, m, :], in0=cg[:], in1=cu[:], op=ALU.mult), reads=[b_cgu[s][0], b_cgu[s][1]], writes=[b_actT[m]])

    def load_h(ti, row0):
        P.dma("sync", lambda e: e.dma_start(out=h1c[:, ti, :], in_=h_d[row0:row0 + 128, :]), f"hl{ti%2}", writes=[b_h1[ti]])

    def stage3(ti, row0):
        h_ap = h1c[:, ti, :]; b_h = b_h1[ti]
        kp = cnt["p"] % 2; cnt["p"] += 1
        P.dma("sync", lambda e: e.dma_start(out=pin[kp][:], in_=p_d[row0:row0 + 128, :]), f"pl{kp}", writes=[b_pin[kp]])
        for nch in range(2):
            for m in range(22):
                P.op("tensor", lambda e, m=m, nch=nch: e.matmul(M[nch][:], lhsT=actT[:, m, ti * 128:(ti + 1) * 128], rhs=wdn[:, m, nch * 512:(nch + 1) * 512], start=(m == 0), stop=(m == 21)), reads=[b_actT[m], b_w], writes=[b_M])
        r = rstd_psum(128, 4, [b_M])
        for nch in range(2):
            P.op("vector", lambda e, nch=nch: e.scalar_tensor_tensor(out=tmp[:, nch * 512:(nch + 1) * 512], in0=M[nch][:], scalar=r, in1=gpffn[:, nch * 512:(nch + 1) * 512], op0=ALU.mult, op1=ALU.mult), reads=[b_M, b_st, b_c], writes=[b_tmp])
        P.op("gpsimd", lambda e: e.tensor_tensor(out=h_ap, in0=h_ap, in1=tmp[:], op=ALU.add), reads=[b_tmp, b_h], writes=[b_h])
        r3 = rstd(h_ap, 128, D, 8, [b_h])
        P.op("vector", lambda e: e.scalar_tensor_tensor(out=xb[:], in0=h_ap, scalar=r3, in1=gple[:], op0=ALU.mult, op1=ALU.mult), reads=[b_h, b_st, b_c], writes=[b_xb])
        transposes(xb, 128, 8, xT[:], b_xT, [b_xb])
        mm1024(lambda kk: xT[:, kk, :], 8, wg, 128, [b_xT])
        for nch in range(2):
            P.op("scalar", lambda e, nch=nch: e.activation(out=tmp[:, nch * 512:(nch + 1) * 512], in_=M[nch][:], func=AF.Sigmoid), reads=[b_M], writes=[b_tmp])
        P.op("gpsimd", lambda e: e.tensor_copy(out=pb[:], in_=pin[kp][:]), reads=[b_pin[kp]], writes=[b_pb])
        transposes(pb, 128, 2, pT[:], b_pT, [b_pb])
        for nch in range(2):
            for k in range(2):
                P.op("tensor", lambda e, k=k, nch=nch: e.matmul(U[nch][:], lhsT=pT[:, k, :], rhs=wp[:, k, nch * 512:(nch + 1) * 512], start=(k == 0), stop=(k == 1)), reads=[b_pT, b_w], writes=[b_U[nch]])
            P.op("vector", lambda e, nch=nch: e.tensor_tensor(out=tmp[:, nch * 512:(nch + 1) * 512], in0=tmp[:, nch * 512:(nch + 1) * 512], in1=U[nch][:], op=ALU.mult), reads=[b_tmp, b_U[nch]], writes=[b_tmp])
        P.op("gpsimd", lambda e: e.tensor_tensor(out=h_ap, in0=h_ap, in1=tmp[:], op=ALU.add), reads=[b_tmp, b_h], writes=[b_h])
        P.dma("sync", lambda e: e.dma_start(out=out_d[row0:row0 + 128, :], in_=h_ap), f"so{ti%2}", reads=[b_h])

    if halo:
        P.dma("sync", lambda e: e.dma_start(out=hh[0:2, :], in_=h_d[TPC:TPC + 2, :]), "hl0", writes=[b_hh])
        stage1(hh[0:2, :], b_hh, TPC, 2, hnTh[:], b_hnTh)
        stage2_halo()
    else:
        P.op("vector", lambda e: e.memset(carry[:], 0.0), writes=b_carry)
    for ci in range(nchunks):
        for ti in range(TPCH):
            row0 = ci * CH + ti * 128
            load_h(ti, row0)
            if 1 in stages:
                stage1(h1c[:, ti, :], b_h1[ti], row0, 128, hnT[:, :, ti * 128:(ti + 1) * 128], b_hnT[ti])
        if 2 in stages:
            stage2()
        for ti in range(TPCH):
            if 3 in stages:
                stage3(ti, ci * CH + ti * 128)
            else:
                P.dma("sync", lambda e, ti=ti, ci=ci: e.dma_start(out=out_d[ci * CH + ti * 128:ci * CH + ti * 128 + 128, :], in_=h1c[:, ti, :]), f"so{ti%2}", reads=[b_h1[ti]])
    P.emit()
    return nc

S = 16384
PERM = np.concatenate([np.arange(0, 512), np.arange(768, 896), np.arange(1024, 1152), np.arange(512, 640), np.arange(640, 768), np.arange(896, 1024), np.arange(1152, 1280), np.arange(1280, 2328)])

def ktile(w):
    K, N = w.shape
    return np.ascontiguousarray(w.reshape(K // 128, 128, N).transpose(1, 0, 2)).reshape(128, -1)

def rope_tables(pos):
    half = 8
    inv = (np.float32(500000.0) ** (-np.arange(half, dtype=np.float32) / np.float32(half))).astype(np.float32)
    ang = pos.astype(np.float32)[:, None] * inv[None, :]
    return np.cos(ang).astype(np.float32), np.sin(ang).astype(np.float32)

def bc(v, n=128):
    return np.ascontiguousarray(np.broadcast_to(v[None, :], (n, v.shape[0]))).astype(np.float32)

def A_inputs(h, i, inp, wbf):
    cos, sin = rope_tables(np.arange(S))
    tri = (np.arange(128)[:, None] <= np.arange(128)[None, :]).astype(BF)
    maps = []
    for c in range(8):
        sl = slice(c * 2048, (c + 1) * 2048)
        t = lambda a: np.ascontiguousarray(a[sl].reshape(16, 128, 8).transpose(1, 0, 2)).reshape(128, 128)
        maps.append(dict(h=np.ascontiguousarray(h[sl]), gpre=bc(inp["pre_mix_g"][i]), w=wbf["w_in"], cos=t(cos), sin=t(sin),
                         lng=bc(inp["gmlp_ln_g"][i]), lnb=bc(inp["gmlp_ln_b"][i]), gmlp=bc(inp["mlp_out_g"][i]),
                         bsT=np.ascontiguousarray(inp["gmlp_bs"][i].T).astype(np.float32), wsT=wbf["wsT"], tri=tri, ident=np.eye(128, dtype=BF)))
    return maps

S = 16384
NEGM = -30000.0

def slot_qi(c, s):
    j = s // 2
    return 16 * j + c if s % 2 == 0 else 16 * j + 15 - c

_CONST = {}
def B_consts():
    if _CONST:
        return _CONST
    n = np.arange(1024)
    jb = np.arange(256)
    ov = ((16 * n[:, None] + 31 >= 64 * jb[None, :]) & (16 * n[:, None] <= 64 * jb[None, :] + 63)).astype(np.float32)
    ov[1023, :] = 0
    ov[:, 255] = 1.0
    _CONST["ov"] = np.ascontiguousarray(ov.reshape(8, 128, 256).transpose(1, 0, 2)).reshape(128, -1).astype(BF)
    ind = np.zeros((128, 64, 128), np.float32)
    for jj in range(64):
        ind[2 * jj, jj, :64] = 1; ind[2 * jj + 1, jj, 64:] = 1
    _CONST["ind"] = ind.reshape(128, -1).astype(BF)
    _CONST["ident"] = np.eye(128, dtype=BF)
    cosc, sinc = rope_tables(16 * np.arange(1024) + 31)
    t = lambda a: np.ascontiguousarray(a.reshape(8, 128, 8).transpose(1, 0, 2)).reshape(128, 64)
    _CONST["cosc"] = t(cosc); _CONST["sinc"] = t(sinc)
    kq = np.arange(128)
    msks = []; sbs = []
    for c in range(8):
        m = np.zeros((16, 128, 15, 128), np.float32)
        sb = np.zeros((16, 128, 256), np.float32)
        for s in range(16):
            qi = slot_qi(c, s); j = s // 2
            tq = 128 * qi + kq
            for mi, nt in enumerate((j - 1, j)):
                if nt < 0: continue
                nn = 128 * nt + kq
                ok = (16 * nn[:, None] + 31 <= tq[None, :]) & (nn[:, None] <= 1022)
                m[s, :, mi, :] = np.where(ok, 0.0, NEGM)
            KT = 16 * j + 8 if s % 2 == 0 else 16 * j + 16
            for r in range(8):
                kt = KT - 8 + r
                tk = 128 * kt + kq
                ok = tk[:, None] <= tq[None, :]
                m[s, :, 2 + r, :] = np.where(ok, 0.0, NEGM)
            for w in range(5):
                tk = 128 * qi - 512 + 128 * w + kq
                ok = (tk[:, None] >= 0) & (tk[:, None] <= tq[None, :]) & (tk[:, None] > tq[None, :] - 512)
                m[s, :, 10 + w, :] = np.where(ok, 0.0, NEGM)
            cur = tq // 64
            valid = jb[None, :] <= cur[:, None]
            forced = valid & ((jb[None, :] == 0) | (jb[None, :] == cur[:, None]) | (jb[None, :] == cur[:, None] - 1))
            sb[s] = np.where(forced, 1000.0, np.where(valid, 0.0, -1e29))
        msks.append(m.reshape(16, 128, -1).astype(BF)); sbs.append(sb)
    _CONST["msk"] = msks; _CONST["sbias"] = sbs
    return _CONST

def B_inputs(qkv, gates, i, inp, wbf):
    C = B_consts()
    q = qkv[:, 0:512].reshape(S, 2, 4, 64)
    ks = qkv[:, 512:640].reshape(S, 2, 64); kw = qkv[:, 640:768].reshape(S, 2, 64)
    zkc = qkv[:, 768:896].reshape(S, 2, 64); zvc = qkv[:, 896:1024].reshape(S, 2, 64)
    vs = qkv[:, 1024:1152].reshape(S, 2, 64); vw = qkv[:, 1152:1280].reshape(S, 2, 64)
    KsT = np.ascontiguousarray(ks.transpose(1, 2, 0)).reshape(128, S)
    one = np.ones((S, 2, 1), BF)
    Vs1 = np.concatenate([vs, one], -1)
    Vs = np.ascontiguousarray(Vs1.reshape(128, 128, 2, 65).transpose(1, 0, 2, 3)).reshape(128, -1)
    kwp = np.concatenate([np.zeros((512, 2, 64), BF), kw], 0)
    vwp = np.concatenate([np.zeros((512, 2, 65), BF), np.concatenate([vw, one], -1)], 0)
    F = np.zeros((2, 2, 128, 16, 1024), BF)
    for X, zz in enumerate((zkc, zvc)):
        zp = np.concatenate([zz, np.zeros((32, 2, 64), BF)], 0)
        n = np.arange(1024); jp = np.arange(16); jj = np.arange(2)
        tidx = 16 * n[None, None, :] + 2 * jp[None, :, None] + jj[:, None, None]
        g_ = zp[tidx]
        g_[:, :, 1023] = 0
        F[X] = g_.transpose(3, 0, 4, 1, 2).reshape(2, 128, 16, 1024)
    Fq = np.ascontiguousarray(F.reshape(2, 2, 128, 16, 4, 256).transpose(0, 1, 4, 2, 3, 5)).reshape(2, 2, 4, 128, 16 * 256)
    b1T = np.stack([np.ascontiguousarray(inp["cmp_b1"][i][X].reshape(2, 128).T) for X in range(2)]).astype(np.float32)
    b2 = np.stack([np.broadcast_to(inp["cmp_b2"][i][X][None, :], (128, 64)) for X in range(2)]).astype(np.float32)
    maps = []
    for c in range(8):
        qis = [slot_qi(c, s) for s in range(16)]
        QT = np.zeros((16, 128, 2, 512), BF)
        for s_, qi in enumerate(qis):
            qq = np.ascontiguousarray(q[128 * qi:128 * qi + 128].transpose(1, 3, 2, 0)).reshape(2, 64, 512)
            QT[s_, 0:64, 0] = qq[0]; QT[s_, 64:128, 1] = qq[1]
        QT = QT.reshape(16, 128, 1024)
        KwT = np.stack([np.ascontiguousarray(kwp[128 * qi:128 * qi + 640].transpose(1, 2, 0)).reshape(128, 640) for qi in qis])
        Vw = np.stack([np.ascontiguousarray(vwp[128 * qi:128 * qi + 640].reshape(5, 128, 2, 65).transpose(1, 0, 2, 3)).reshape(128, -1) for qi in qis])
        gt = np.stack([gates[128 * qi:128 * qi + 128] for qi in qis]).astype(np.float32)
        maps.append(dict(QT=QT, KsT=KsT, Vs=Vs, KwT=KwT, Vw=Vw, F=Fq, gates=gt, sbias=C["sbias"][c], msk=C["msk"][c],
                         w1=wbf["cmp_w1"], w2=wbf["cmp_w2"], b1T=b1T, peT=wbf["cmp_peT"], b2=np.ascontiguousarray(b2),
                         cosc=C["cosc"], sinc=C["sinc"], ov=C["ov"], ind=C["ind"], ident=C["ident"]))
    return maps

def B_gather(results):
    attn = np.zeros((S, 512), np.float32)
    for c in range(8):
        a = results[c]["attn"]
        for s in range(16):
            qi = slot_qi(c, s)
            attn[128 * qi:128 * qi + 128] = a[128 * s:128 * s + 128]
    return attn

def B_weights_f32(inp, i):
    w1 = np.stack([ktile(inp["cmp_w1"][i][X]) for X in range(2)])
    w2 = np.stack([ktile(inp["cmp_w2"][i][X]) for X in range(2)])
    pe = inp["cmp_pe"][i]
    peT = np.stack([np.ascontiguousarray(pe[X].reshape(16, 2, 64).transpose(1, 2, 0)).reshape(128, 16) for X in range(2)])
    return w1, w2, peT

S = 16384

def C_weights_f32(inp, i):
    wup = inp["w_up"][i]
    wup_l = np.ascontiguousarray(wup.reshape(8, 128, 2, 22, 128).transpose(3, 1, 2, 0, 4)).reshape(22, 128, -1)
    return dict(wo=ktile(inp["w_o"][i]), wup=wup_l, wdn=ktile(inp["w_down"][i]), wg=ktile(inp["w_ple_gate"][i]), wp=ktile(inp["w_ple_proj"][i]))

def C_inputs(h, attn, mlpn, i, inp, wbf):
    cw = inp["conv_w"][i]; cb = inp["conv_b"][i]
    conv = np.concatenate([cw, cb[None, :]], 0)
    conv = np.ascontiguousarray(conv.reshape(4, 44, 128).transpose(2, 1, 0)).reshape(128, -1).astype(np.float32)
    maps = []
    for c in range(8):
        sl = slice(2048 * c, 2048 * (c + 1))
        def ext(a):
            o = np.zeros((2048 + 128,) + a.shape[1:], a.dtype)
            o[:2048] = a[sl]
            if c > 0:
                o[2048:2050] = a[2048 * c - 2:2048 * c]
            return o
        maps.append(dict(h=ext(h), attn=ext(attn), mlpn=ext(mlpn), p=np.ascontiguousarray(inp["p"][i, 0][sl]),
                         gattn=bc(inp["attn_out_g"][i]), gpost=bc(inp["post_mix_g"][i]), gpre=bc(inp["pre_ffn_g"][i]),
                         gpffn=bc(inp["post_ffn_g"][i]), gple=bc(inp["ple_norm_g"][i]), conv=conv,
                         wo=wbf["wo"], wup=wbf["wup"], wdn=wbf["wdn"], wg=wbf["wg"], wp=wbf["wp"], ident=np.eye(128, dtype=BF)))
    return maps


_PROGS = {}

def _prog(name, fn):
    if name not in _PROGS:
        _PROGS[name] = fn()
    return _PROGS[name]

def _run(nc, maps):
    res = run_bass_kernel_spmd(nc, maps, core_ids=list(range(8)))
    return res.results

WCOLS = [("w_in", 8 * 2328), ("wsT", 1024), ("cmp_w1", 2 * 4096), ("cmp_w2", 2 * 128), ("cmp_peT", 2 * 16),
         ("wo", 8192), ("wup", 22 * 2048), ("wdn", 22 * 1024), ("wg", 8192), ("wp", 2048)]
WTOT = sum(c for _, c in WCOLS)

def _pack_weights(inp, i):
    cw = C_weights_f32(inp, i)
    w1, w2, peT = B_weights_f32(inp, i)
    parts = {
        "w_in": ktile(inp["w_in"][i][:, PERM]),
        "wsT": np.ascontiguousarray(inp["gmlp_ws"][i].transpose(2, 0, 1)).reshape(128, 1024),
        "cmp_w1": np.ascontiguousarray(w1.transpose(1, 0, 2)).reshape(128, -1),
        "cmp_w2": np.ascontiguousarray(w2.transpose(1, 0, 2)).reshape(128, -1),
        "cmp_peT": np.ascontiguousarray(peT.transpose(1, 0, 2)).reshape(128, -1),
        "wo": cw["wo"], "wup": np.ascontiguousarray(cw["wup"].transpose(1, 0, 2)).reshape(128, -1),
        "wdn": cw["wdn"], "wg": cw["wg"], "wp": cw["wp"],
    }
    return np.concatenate([parts[n].astype(np.float32) for n, _ in WCOLS], axis=1)

def _unpack_weights(wb):
    out = {}
    o = 0
    for n, c in WCOLS:
        out[n] = np.ascontiguousarray(wb[:, o:o + c]); o += c
    out["cmp_w1"] = np.ascontiguousarray(out["cmp_w1"].reshape(128, 2, 4096).transpose(1, 0, 2))
    out["cmp_w2"] = np.ascontiguousarray(out["cmp_w2"].reshape(128, 2, 128).transpose(1, 0, 2))
    out["cmp_peT"] = np.ascontiguousarray(out["cmp_peT"].reshape(128, 2, 16).transpose(1, 0, 2))
    out["wup"] = np.ascontiguousarray(out["wup"].reshape(128, 22, 2048).transpose(1, 0, 2))
    return out

def kernel(**inputs):
    inp = {k: np.asarray(v) for k, v in inputs.items()}
    L = 2
    big = np.concatenate([_pack_weights(inp, i) for i in range(L)], axis=1)
    per = big.shape[1] // 8
    assert per * 8 == big.shape[1]
    ncW = _prog("W", lambda: build_W(per))
    resW = _run(ncW, [{"win": np.ascontiguousarray(big[:, c * per:(c + 1) * per])} for c in range(8)])
    wb = np.concatenate([r["wout"] for r in resW], axis=1)
    wbf = [_unpack_weights(wb[:, i * WTOT:(i + 1) * WTOT]) for i in range(L)]
    h = np.ascontiguousarray(inp["x"][0]).astype(np.float32)
    for i in range(L):
        ncA = _prog("A", build_A)
        resA = _run(ncA, A_inputs(h, i, inp, wbf[i]))
        qkv = np.concatenate([r["qkv"] for r in resA], 0)
        gates = np.concatenate([r["gates"] for r in resA], 0)
        mlpn = np.concatenate([r["mlpn"] for r in resA], 0)
        ncB = _prog("B", build_B)
        resB = _run(ncB, B_inputs(qkv, gates, i, inp, wbf[i]))
        attn = B_gather(resB)
        ncC = _prog("C", build_C)
        resC = _run(ncC, C_inputs(h, attn, mlpn, i, inp, wbf[i]))
        h = np.concatenate([r["hout"] for r in resC], 0)
    return h[None].astype(np.float32)
```

```python
import numpy as np
import ml_dtypes
from contextlib import ExitStack
import concourse.bass as bass
import concourse.mybir as mybir
from concourse.bass_utils import run_bass_kernel_spmd

F32 = mybir.dt.float32
BF16 = mybir.dt.bfloat16
AF = mybir.ActivationFunctionType
ALU = mybir.AluOpType
AX = mybir.AxisListType
NPBF16 = ml_dtypes.bfloat16


class Buf:
    __slots__ = ("name", "last_w", "readers")

    def __init__(self, name=""):
        self.name = name
        self.last_w = None
        self.readers = []


class Op:
    __slots__ = ("eng", "fn", "deps", "is_dma", "stream", "needs_inc", "tok", "idx", "dma_upto")

    def __init__(self, eng, fn, is_dma=False, stream=None):
        self.eng = eng
        self.fn = fn
        self.deps = set()
        self.is_dma = is_dma
        self.stream = stream
        self.needs_inc = False
        self.tok = None
        self.idx = -1
        self.dma_upto = {}


class Prog:
    ENGS = ("sync", "scalar", "vector", "gpsimd", "tensor")
    SEM_ROLL = 6000

    def __init__(self, nc):
        self.nc = nc
        self.ops = []
        self.stack = ExitStack()
        self.nbuf = 0

    def buf(self, name=""):
        self.nbuf += 1
        return Buf(name or f"b{self.nbuf}")

    def sb(self, name, shape, dtype):
        return self.stack.enter_context(self.nc.sbuf_tensor("sb_" + name, list(shape), dtype))

    def ps(self, name, shape, dtype=F32):
        return self.stack.enter_context(self.nc.psum_tensor("ps_" + name, list(shape), dtype))

    def op(self, eng, fn, reads=(), writes=(), accum=False):
        o = Op(eng, fn)
        self._deps(o, reads, writes, accum)
        return o

    def dma(self, eng, fn, stream, reads=(), writes=()):
        o = Op(eng, fn, is_dma=True, stream=stream)
        self._deps(o, reads, writes, False)
        return o

    def _deps(self, o, reads, writes, accum):
        o.idx = len(self.ops)
        for b in reads:
            if b.last_w is not None:
                o.deps.add(b.last_w)
        for b in writes:
            if b.last_w is not None:
                lw = self.ops[b.last_w]
                if not (accum and lw.eng == o.eng and not lw.is_dma):
                    o.deps.add(b.last_w)
            for r in b.readers:
                o.deps.add(r)
        for b in reads:
            b.readers.append(o.idx)
        for b in writes:
            b.last_w = o.idx
            b.readers = []
        o.deps.discard(o.idx)
        if o.eng == "tensor" and not o.is_dma:
            o.deps = {d for d in o.deps if not (self.ops[d].eng == "tensor" and not self.ops[d].is_dma)}
        self.ops.append(o)

    def emit(self):
        nc = self.nc
        ops = self.ops
        for o in ops:
            for d in o.deps:
                ops[d].needs_inc = True
        sems = {}

        def new_sem(tag):
            return self.stack.enter_context(nc.semaphore(f"s_{tag}_{len(sems)}"))

        eng_sem = {}
        eng_cnt = {}
        stream_sem = {}
        stream_cnt = {}
        stream_hist = {}
        for o in ops:
            if o.is_dma:
                if o.stream not in stream_sem:
                    stream_sem[o.stream] = new_sem("d")
                    sems[len(sems)] = 1
                    stream_cnt[o.stream] = 0
                    stream_hist[o.stream] = []
                stream_cnt[o.stream] += 16
                o.tok = (stream_sem[o.stream], stream_cnt[o.stream])
                stream_hist[o.stream].append(o.idx)
            elif o.needs_inc:
                if o.eng not in eng_sem or eng_cnt[o.eng] >= self.SEM_ROLL:
                    eng_sem[o.eng] = new_sem(o.eng[0])
                    sems[len(sems)] = 1
                    eng_cnt[o.eng] = 0
                eng_cnt[o.eng] += 1
                o.tok = (eng_sem[o.eng], eng_cnt[o.eng])
        self.n_sems = len(sems)
        import bisect
        seen = {e: {} for e in self.ENGS}
        waits = [None] * len(ops)
        for o in ops:
            need = {}
            for d in o.deps:
                do = ops[d]
                sem, val = do.tok
                if do.is_dma:
                    h = stream_hist[do.stream]
                    k = bisect.bisect_left(h, o.idx)
                    val = 16 * k
                key = id(sem)
                if key not in need or need[key][1] < val:
                    need[key] = (sem, val)
            w = []
            s = seen[o.eng]
            for key, (sem, val) in need.items():
                if s.get(key, 0) < val:
                    s[key] = val
                    w.append((sem, val))
            waits[o.idx] = w
        with nc.Block() as block:
            def make(engname):
                def body(e):
                    for o in ops:
                        if o.eng != engname:
                            continue
                        for sem, val in waits[o.idx]:
                            e.wait_ge(sem, val)
                        ins = o.fn(e)
                        if o.is_dma:
                            ins.then_inc(o.tok[0], 16)
                        elif o.needs_inc:
                            ins.then_inc(o.tok[0], 1)
                    if engname == "sync":
                        for st, sem in stream_sem.items():
                            e.wait_ge(sem, stream_cnt[st])
                return body
            block.sync(make("sync"))
            block.scalar(make("scalar"))
            block.vector(make("vector"))
            block.gpsimd(make("gpsimd"))
            block.tensor(make("tensor"))
        self.stack.close()

BF = NPBF16

S = 16384; D = 1024; NCORE = 8; TPC = 2048; NTILE = 16
INW = 2328
EPS = 1e-6

def build_W(ncols, chunk=4096):
    nc = bass.Bass("TRN2", target_bir_lowering=False)
    win = nc.dram_tensor("win", [128, ncols], F32, kind="ExternalInput").ap()
    wout = nc.dram_tensor("wout", [128, ncols], BF16, kind="ExternalOutput").ap()
    P = Prog(nc)
    nb = 3
    st = [P.sb(f"st{i}", [128, chunk], F32) for i in range(nb)]
    sb = [P.sb(f"sb{i}", [128, chunk], BF16) for i in range(nb)]
    b_st = [P.buf() for _ in range(nb)]
    b_sb = [P.buf() for _ in range(nb)]
    engs = ["vector", "gpsimd", "vector"]
    i = 0
    for c0 in range(0, ncols, chunk):
        c1 = min(ncols, c0 + chunk); w = c1 - c0; k = i % nb
        P.dma("sync", lambda e, k=k, c0=c0, c1=c1, w=w: e.dma_start(out=st[k][:, 0:w], in_=win[:, c0:c1]), f"wl{k}", writes=[b_st[k]])
        en = engs[i % 3]
        if en == "scalar":
            P.op(en, lambda e, k=k, w=w: e.copy(out=sb[k][:, 0:w], in_=st[k][:, 0:w]), reads=[b_st[k]], writes=[b_sb[k]])
        else:
            P.op(en, lambda e, k=k, w=w: e.tensor_copy(out=sb[k][:, 0:w], in_=st[k][:, 0:w]), reads=[b_st[k]], writes=[b_sb[k]])
        P.dma("sync", lambda e, k=k, c0=c0, c1=c1, w=w: e.dma_start(out=wout[:, c0:c1], in_=sb[k][:, 0:w]), f"ws{k}", reads=[b_sb[k]])
        i += 1
    P.emit()
    return nc


def build_A():
    nc = bass.Bass("TRN2", target_bir_lowering=False)
    din = lambda n, sh, dt=F32: nc.dram_tensor(n, sh, dt, kind="ExternalInput").ap()
    dout = lambda n, sh, dt=F32: nc.dram_tensor(n, sh, dt, kind="ExternalOutput").ap()
    h_d = din("h", [TPC, D])
    gpre_d = din("gpre", [128, D])
    w_d = din("w", [128, 8 * INW], BF16)
    cos_d = din("cos", [128, NTILE * 8]); sin_d = din("sin", [128, NTILE * 8])
    lng_d = din("lng", [128, 512]); lnb_d = din("lnb", [128, 512]); gmlp_d = din("gmlp", [128, 512])
    bsT_d = din("bsT", [128, 8])
    wsT_d = din("wsT", [128, 8 * 128], BF16)
    tri_d = din("tri", [128, 128], BF16)
    id_d = din("ident", [128, 128], BF16)
    qkv_o = dout("qkv", [TPC, 1280], BF16)
    gates_o = dout("gates", [TPC, 24])
    mlpn_o = dout("mlpn", [TPC, 512], BF16)
    P = Prog(nc)
    W = P.sb("W", [128, 8, INW], BF16); b_W = P.buf()
    gpre = P.sb("gpre", [128, D], F32); b_c = P.buf()
    cos = P.sb("cos", [128, NTILE, 8], F32); sin = P.sb("sin", [128, NTILE, 8], F32)
    lng = P.sb("lng", [128, 512], F32); lnb = P.sb("lnb", [128, 512], F32); gmlp = P.sb("gmlpg", [128, 512], F32)
    bsT = P.sb("bsT", [128, 8], F32)
    wsT = P.sb("wsT", [128, 8, 128], BF16); b_ws = P.buf()
    tri = P.sb("tri", [128, 128], BF16)
    ident = P.sb("ident", [128, 128], BF16)
    for kt in range(8):
        P.dma("sync" if kt % 2 == 0 else "gpsimd", lambda e, kt=kt: e.dma_start(out=W[:, kt, :], in_=w_d[:, kt * INW:(kt + 1) * INW]), f"cw{kt%2}", writes=[b_W])
    for (t, d_) in [(gpre, gpre_d), (lng, lng_d), (lnb, lnb_d), (gmlp, gmlp_d), (bsT, bsT_d), (tri, tri_d), (ident, id_d)]:
        P.dma("sync", lambda e, t=t, d_=d_: e.dma_start(out=t[:], in_=d_), "cc", writes=[b_c])
    P.dma("sync", lambda e: e.dma_start(out=cos[:].rearrange("p a b -> p (a b)"), in_=cos_d), "cc", writes=[b_c])
    P.dma("sync", lambda e: e.dma_start(out=sin[:].rearrange("p a b -> p (a b)"), in_=sin_d), "cc", writes=[b_c])
    P.dma("sync", lambda e: e.dma_start(out=wsT[:].rearrange("p a b -> p (a b)"), in_=wsT_d), "cc", writes=[b_ws])
    P.op("vector", lambda e: e.tensor_tensor(out=wsT[:], in0=wsT[:], in1=tri[:].unsqueeze(1).to_broadcast([128, 8, 128]), op=ALU.mult), reads=[b_ws, b_c], writes=[b_ws])

    NB = 4
    hs = [P.sb(f"hs{i}", [128, D], F32) for i in range(NB)]; b_hs = [P.buf() for _ in range(NB)]
    junk_l = [P.sb(f"junk{i}", [128, D], BF16) for i in range(2)]; b_junk_l = [P.buf() for _ in range(2)]
    st = [P.sb(f"stat{i}", [128, 8], F32) for i in range(NB)]; b_st = [P.buf() for _ in range(NB)]
    a_bf_l = [P.sb(f"a_bf{i}", [128, D], BF16) for i in range(2)]; b_a_l = [P.buf() for _ in range(2)]
    aT_l = [P.sb(f"aT{i}", [128, 8, 128], BF16) for i in range(2)]; b_aT_l = [P.buf() for _ in range(2)]
    pT_l = [P.ps(f"pT{i}", [128, 8, 128], BF16) for i in range(2)]; b_pT_l = [P.buf() for _ in range(2)]
    zps_l = [[P.ps(f"zps{j}_{i}", [128, 512], F32) for i in range(2)] for j in range(2)]; b_zps_l = [[P.buf() for _ in range(2)] for j in range(2)]
    mixps_l = [P.ps(f"mixps{i}", [128, 512], F32) for i in range(2)]; b_mix_l = [P.buf() for _ in range(2)]
    z_l = [P.sb(f"z{i}", [128, INW], F32) for i in range(2)]; b_z_l = [[P.buf() for _ in range(5)] for j in range(2)]
    qkv = [P.sb(f"qkv{i}", [128, 1280], BF16) for i in range(NB)]; b_qkv = [P.buf() for _ in range(NB)]
    rt_l = [P.sb(f"rt{i}", [128, 4, 12, 8], F32) for i in range(2)]; b_rt_l = [P.buf() for _ in range(2)]
    gt = [P.sb(f"gt{i}", [128, 24], F32) for i in range(NB)]; b_gt = [P.buf() for _ in range(NB)]
    u_l = [P.sb(f"u{i}", [128, 512], F32) for i in range(2)]; b_u_l = [P.buf() for _ in range(2)]
    v_l = [P.sb(f"v{i}", [128, 512], F32) for i in range(2)]; b_v_l = [P.buf() for _ in range(2)]
    bnst_l = [P.sb(f"bnst{i}", [128, 6], F32) for i in range(2)]; b_bn_l = [P.buf() for _ in range(2)]
    mv_l = [P.sb(f"mv{i}", [128, 4], F32) for i in range(2)]; b_mv_l = [P.buf() for _ in range(2)]
    vn_l = [P.sb(f"vn{i}", [128, 512], BF16) for i in range(2)]; b_vn_l = [P.buf() for _ in range(2)]
    m1_l = [P.sb(f"m1{i}", [128, 512], F32) for i in range(2)]; b_m1_l = [P.buf() for _ in range(2)]
    mlpn = [P.sb(f"mlpn{i}", [128, 512], BF16) for i in range(NB)]; b_mlpn = [P.buf() for _ in range(NB)]
    chunks = [(0, 512), (512, 512), (1024, 512), (1536, 512), (2048, 280)]

    def load(ti):
        k = ti % NB
        P.dma("sync", lambda e: e.dma_start(out=hs[k][:], in_=h_d[ti * 128:(ti + 1) * 128, :]), f"hl{k}", writes=[b_hs[k]])

    def do_tile(ti):
        k = ti % NB; par = ti % 2
        junk = junk_l[par]; b_junk = b_junk_l[par]; a_bf = a_bf_l[par]; b_a = b_a_l[par]; aT = aT_l[par]; b_aT = b_aT_l[par]
        pT = pT_l[par]; b_pT = b_pT_l[par]; zps = zps_l[par]; b_zps = b_zps_l[par]; mixps = mixps_l[par]; b_mix = b_mix_l[par]
        z = z_l[par]; b_z = b_z_l[par]; rt = rt_l[par]; b_rt = b_rt_l[par]; u = u_l[par]; b_u = b_u_l[par]; v = v_l[par]; b_v = b_v_l[par]
        bnst = bnst_l[par]; b_bn = b_bn_l[par]; mv = mv_l[par]; b_mv = b_mv_l[par]; vn = vn_l[par]; b_vn = b_vn_l[par]; m1 = m1_l[par]; b_m1 = b_m1_l[par]
        if ti + 2 < NTILE:
            load(ti + 2)
        s_ = st[k]
        P.op("scalar", lambda e: e.activation(out=junk[:], in_=hs[k][:], func=AF.Square, accum_out=s_[:, 0:1]), reads=[b_hs[k]], writes=[b_junk, b_st[k]])
        P.op("scalar", lambda e: e.activation(out=s_[:, 1:2], in_=s_[:, 0:1], func=AF.Sqrt, scale=1.0 / D, bias=EPS), reads=[b_st[k]], writes=[b_st[k]])
        P.op("vector", lambda e: e.reciprocal(out=s_[:, 2:3], in_=s_[:, 1:2]), reads=[b_st[k]], writes=[b_st[k]])
        P.op("vector", lambda e: e.scalar_tensor_tensor(out=a_bf[:], in0=hs[k][:], scalar=s_[:, 2:3], in1=gpre[:], op0=ALU.mult, op1=ALU.mult), reads=[b_hs[k], b_st[k], b_c], writes=[b_a])
        yield
        for kt in range(8):
            P.op("tensor", lambda e, kt=kt: e.transpose(out=pT[:, kt, :], in_=a_bf[:, kt * 128:(kt + 1) * 128], identity=ident[:]), reads=[b_a, b_c], writes=[b_pT], accum=True)
        P.op("vector", lambda e: e.tensor_copy(out=aT[:], in_=pT[:]), reads=[b_pT], writes=[b_aT])
        yield
        for ci, (c0, cw) in enumerate(chunks):
            zp = zps[ci % 2]; bz = b_zps[ci % 2]
            for kt in range(8):
                P.op("tensor", lambda e, kt=kt, zp=zp, c0=c0, cw=cw: e.matmul(zp[:, 0:cw], lhsT=aT[:, kt, :], rhs=W[:, kt, c0:c0 + cw], start=(kt == 0), stop=(kt == 7)), reads=[b_aT, b_W], writes=[bz], accum=True)
            if ci % 2 == 0:
                P.op("scalar", lambda e, zp=zp, c0=c0, cw=cw: e.copy(out=z[:, c0:c0 + cw], in_=zp[:, 0:cw]), reads=[bz], writes=[b_z[ci]])
            else:
                P.op("vector", lambda e, zp=zp, c0=c0, cw=cw: e.tensor_copy(out=z[:, c0:c0 + cw], in_=zp[:, 0:cw]), reads=[bz], writes=[b_z[ci]])
        yield
        P.op("gpsimd", lambda e: e.tensor_copy(out=qkv[k][:], in_=z[:, 0:1280]), reads=[b_z[0], b_z[1], b_z[2]], writes=[b_qkv[k]])
        zr = z[:, 0:768].rearrange("p (h d) -> p h d", d=64)
        qr = qkv[k][:, 0:768].rearrange("p (h d) -> p h d", d=64)
        cb = cos[:, ti, :].unsqueeze(1).to_broadcast([128, 12, 8]); sb_ = sin[:, ti, :].unsqueeze(1).to_broadcast([128, 12, 8])
        x1 = zr[:, :, 0:8]; x2 = zr[:, :, 8:16]
        P.op("vector", lambda e: e.tensor_tensor(out=rt[:, 0], in0=x1, in1=cb, op=ALU.mult), reads=[b_z[0], b_z[1], b_c], writes=[b_rt])
        P.op("vector", lambda e: e.tensor_tensor(out=rt[:, 1], in0=x2, in1=sb_, op=ALU.mult), reads=[b_z[0], b_z[1], b_c], writes=[b_rt])
        P.op("vector", lambda e: e.tensor_tensor(out=rt[:, 2], in0=x2, in1=cb, op=ALU.mult), reads=[b_z[0], b_z[1], b_c], writes=[b_rt])
        P.op("vector", lambda e: e.tensor_tensor(out=rt[:, 3], in0=x1, in1=sb_, op=ALU.mult), reads=[b_z[0], b_z[1], b_c], writes=[b_rt])
        P.op("vector", lambda e: e.tensor_tensor(out=qr[:, :, 0:8], in0=rt[:, 0], in1=rt[:, 1], op=ALU.subtract), reads=[b_rt], writes=[b_qkv[k]])
        P.op("vector", lambda e: e.tensor_tensor(out=qr[:, :, 8:16], in0=rt[:, 2], in1=rt[:, 3], op=ALU.add), reads=[b_rt], writes=[b_qkv[k]])
        P.dma("sync", lambda e: e.dma_start(out=qkv_o[ti * 128:(ti + 1) * 128, :], in_=qkv[k][:]), f"so{k}", reads=[b_qkv[k]])
        yield
        P.op("scalar", lambda e: e.activation(out=gt[k][:], in_=z[:, 1280:1304], func=AF.Sigmoid), reads=[b_z[2]], writes=[b_gt[k]])
        P.dma("sync", lambda e: e.dma_start(out=gates_o[ti * 128:(ti + 1) * 128, :], in_=gt[k][:]), f"so{k}", reads=[b_gt[k]])
        P.op("scalar", lambda e: e.activation(out=u[:], in_=z[:, 1304:1816], func=AF.Gelu_apprx_tanh), reads=[b_z[2], b_z[3]], writes=[b_u])
        P.op("scalar", lambda e: e.activation(out=v[:], in_=z[:, 1816:2328], func=AF.Gelu_apprx_tanh), reads=[b_z[3], b_z[4]], writes=[b_v])
        yield
        P.op("vector", lambda e: e.bn_stats(out=bnst[:], in_=v[:]), reads=[b_v], writes=[b_bn])
        P.op("vector", lambda e: e.bn_aggr(out=mv[:, 0:2], in_=bnst[:]), reads=[b_bn], writes=[b_mv])
        P.op("scalar", lambda e: e.activation(out=mv[:, 2:3], in_=mv[:, 1:2], func=AF.Sqrt, scale=1.0, bias=EPS), reads=[b_mv], writes=[b_mv])
        P.op("vector", lambda e: e.reciprocal(out=mv[:, 3:4], in_=mv[:, 2:3]), reads=[b_mv], writes=[b_mv])
        P.op("vector", lambda e: e.tensor_scalar(out=v[:], in0=v[:], scalar1=mv[:, 0:1], scalar2=mv[:, 3:4], op0=ALU.subtract, op1=ALU.mult), reads=[b_v, b_mv], writes=[b_v])
        P.op("gpsimd", lambda e: e.tensor_tensor(out=v[:], in0=v[:], in1=lng[:], op=ALU.mult), reads=[b_v, b_c], writes=[b_v])
        P.op("gpsimd", lambda e: e.tensor_tensor(out=vn[:], in0=v[:], in1=lnb[:], op=ALU.add), reads=[b_v, b_c], writes=[b_vn])
        yield
        for g in range(8):
            P.op("tensor", lambda e, g=g: e.matmul(mixps[:, g * 64:(g + 1) * 64], lhsT=wsT[:, g, :], rhs=vn[:, g * 64:(g + 1) * 64], start=True, stop=True), reads=[b_ws, b_vn], writes=[b_mix], accum=True)
        P.op("vector", lambda e: e.tensor_tensor(out=m1[:].rearrange("p (g d) -> p g d", d=64), in0=mixps[:].rearrange("p (g d) -> p g d", d=64), in1=bsT[:].unsqueeze(2).to_broadcast([128, 8, 64]), op=ALU.add), reads=[b_mix, b_c], writes=[b_m1])
        P.op("vector", lambda e: e.tensor_tensor(out=m1[:], in0=m1[:], in1=u[:], op=ALU.mult), reads=[b_m1, b_u], writes=[b_m1])
        yield
        P.op("scalar", lambda e: e.activation(out=junk[:, 0:512], in_=m1[:], func=AF.Square, accum_out=s_[:, 4:5]), reads=[b_m1], writes=[b_junk, b_st[k]])
        P.op("scalar", lambda e: e.activation(out=s_[:, 5:6], in_=s_[:, 4:5], func=AF.Sqrt, scale=1.0 / 512, bias=EPS), reads=[b_st[k]], writes=[b_st[k]])
        P.op("vector", lambda e: e.reciprocal(out=s_[:, 6:7], in_=s_[:, 5:6]), reads=[b_st[k]], writes=[b_st[k]])
        P.op("vector", lambda e: e.scalar_tensor_tensor(out=mlpn[k][:], in0=m1[:], scalar=s_[:, 6:7], in1=gmlp[:], op0=ALU.mult, op1=ALU.mult), reads=[b_m1, b_st[k], b_c], writes=[b_mlpn[k]])
        P.dma("sync", lambda e: e.dma_start(out=mlpn_o[ti * 128:(ti + 1) * 128, :], in_=mlpn[k][:]), f"so{k}", reads=[b_mlpn[k]])
    load(0); load(1)
    for t2 in range(0, NTILE, 2):
        gens = [do_tile(t2), do_tile(t2 + 1)]
        alive = [True, True]
        while any(alive):
            for gi in range(2):
                if alive[gi]:
                    try:
                        next(gens[gi])
                    except StopIteration:
                        alive[gi] = False
    P.emit()
    return nc


S = 16384
NEGM = -30000.0
SCALE = 0.125

def slot_qi(c, s):
    j = s // 2
    return 16 * j + c if s % 2 == 0 else 16 * j + 15 - c

def build_B(nslots=16, debug=False):
    nc = bass.Bass("TRN2", target_bir_lowering=False)
    din = lambda n, sh, dt=F32: nc.dram_tensor(n, sh, dt, kind="ExternalInput").ap()
    dout = lambda n, sh, dt=F32: nc.dram_tensor(n, sh, dt, kind="ExternalOutput").ap()
    QT_d = din("QT", [nslots, 128, 2 * 512], BF16)
    KsT_d = din("KsT", [128, S], BF16)
    Vs_d = din("Vs", [128, 128 * 2 * 65], BF16)
    KwT_d = din("KwT", [nslots, 128, 640], BF16)
    Vw_d = din("Vw", [nslots, 128, 5 * 2 * 65], BF16)
    F_d = din("F", [2, 2, 4, 128, 16 * 256], BF16)
    gates_d = din("gates", [nslots, 128, 24])
    sbias_d = din("sbias", [nslots, 128, 256])
    msk_d = din("msk", [nslots, 128, 15 * 128], BF16)
    w1_d = din("w1", [2, 128, 16 * 256], BF16)
    w2_d = din("w2", [2, 128, 2 * 64], BF16)
    b1T_d = din("b1T", [2, 128, 2])
    peT_d = din("peT", [2, 128, 16], BF16)
    b2_d = din("b2", [2, 128, 64])
    cosc_d = din("cosc", [128, 64]); sinc_d = din("sinc", [128, 64])
    ov_d = din("ov", [128, 8 * 256], BF16)
    ind_d = din("ind", [128, 64 * 128], BF16)
    id_d = din("ident", [128, 128], BF16)
    attn_o = dout("attn", [nslots * 128, 512])
    dbg_o = dout("dbg", [nslots * 128, 3 * 512]) if debug else None
    P = Prog(nc)
    KsT = P.sb("KsT", [128, S], BF16); b_KsT = P.buf()
    Vs = P.sb("Vs", [128, 128, 2, 65], BF16); b_Vs = P.buf()
    IndAll = P.sb("IndAll", [128, 64, 128], BF16)
    ov = P.sb("ov", [128, 8, 256], BF16)
    ident = P.sb("ident", [128, 128], BF16)
    cosc = P.sb("cosc", [128, 8, 8], F32); sinc = P.sb("sinc", [128, 8, 8], F32)
    b_c = P.buf()
    w1 = P.sb("w1", [128, 2, 16, 256], BF16); w2 = P.sb("w2", [128, 2, 2, 64], BF16)
    b1T = P.sb("b1T", [128, 2, 2], F32); peT = P.sb("peT", [128, 2, 16], BF16); b2 = P.sb("b2", [128, 2, 64], F32)
    b_cw = P.buf()
    KcT = P.sb("KcT", [128, 1024], BF16); b_KcT = P.buf()
    Vc = P.sb("Vc", [128, 8, 2, 65], BF16); b_Vc = P.buf()
    for X in range(2):
        P.dma("sync", lambda e, X=X: e.dma_start(out=w1[:, X].rearrange("p a b -> p (a b)"), in_=w1_d[X]), "cc", writes=[b_cw])
        P.dma("sync", lambda e, X=X: e.dma_start(out=w2[:, X].rearrange("p a b -> p (a b)"), in_=w2_d[X]), "cc", writes=[b_cw])
        P.dma("sync", lambda e, X=X: e.dma_start(out=b1T[:, X], in_=b1T_d[X]), "cc", writes=[b_cw])
        P.dma("sync", lambda e, X=X: e.dma_start(out=peT[:, X], in_=peT_d[X]), "cc", writes=[b_cw])
        P.dma("sync", lambda e, X=X: e.dma_start(out=b2[:, X], in_=b2_d[X]), "cc", writes=[b_cw])
    P.dma("sync", lambda e: e.dma_start(out=ident[:], in_=id_d), "cc", writes=[b_c])
    P.dma("sync", lambda e: e.dma_start(out=cosc[:].rearrange("p a b -> p (a b)"), in_=cosc_d), "cc", writes=[b_c])
    P.dma("sync", lambda e: e.dma_start(out=sinc[:].rearrange("p a b -> p (a b)"), in_=sinc_d), "cc", writes=[b_c])
    P.dma("sync", lambda e: e.dma_start(out=ov[:].rearrange("p a b -> p (a b)"), in_=ov_d), "cc", writes=[b_c])
    P.dma("gpsimd", lambda e: e.dma_start(out=IndAll[:].rearrange("p a b -> p (a b)"), in_=ind_d), "cc2", writes=[b_c])
    for q4 in range(4):
        P.dma("gpsimd", lambda e, q4=q4: e.dma_start(out=KsT[:, q4 * 4096:(q4 + 1) * 4096], in_=KsT_d[:, q4 * 4096:(q4 + 1) * 4096]), "cc2", writes=[b_KsT])
        P.dma("gpsimd", lambda e, q4=q4: e.dma_start(out=Vs[:, q4 * 32:(q4 + 1) * 32].rearrange("p a b c -> p (a b c)"), in_=Vs_d[:, q4 * 32 * 130:(q4 + 1) * 32 * 130]), "cc2", writes=[b_Vs])
    sTs = [P.ps(f"sT{i}", [128, 512], F32) for i in range(3)]; b_sT = [P.buf() for _ in range(3)]
    oaccs = [P.ps(f"oacc{i}", [128, 4, 128], F32) for i in range(2)]; b_oacc = [P.buf() for _ in range(2)]
    imps = [P.ps(f"imp{i}", [128, 2, 256], F32) for i in range(2)]; b_imp = P.buf()
    tp = P.ps("tp", [128, 2, 128], BF16); b_tp = P.buf()
    cps = sTs[2][:, 0:64]; b_cps = b_sT[2]
    b1ps = sTs[2][:, 64:66]; b_b1ps = b_sT[2]
    Fb = [P.sb(f"Fb{i}", [128, 16, 256], BF16) for i in range(2)]; b_Fb = [P.buf() for _ in range(2)]
    hT = P.sb("hT", [128, 2, 256], BF16); b_hT = [P.buf() for _ in range(2)]
    bias1 = P.sb("bias1", [128, 2, 2], F32); b_bias1 = P.buf()
    kcf = P.sb("kcf", [128, 8, 2, 64], F32); b_kcf = P.buf()
    kcb = P.sb("kcb", [128, 8, 2, 64], BF16); b_kcb = P.buf()
    rt = P.sb("rt", [128, 4, 8, 2, 8], F32); b_rt = P.buf()
    P.op("vector", lambda e: e.memset(Vc[:], 1.0), writes=[b_Vc])
    fi = 0
    for X in range(2):
        for hc in range(2):
            for jp in range(16):
                P.op("tensor", lambda e, X=X, hc=hc, jp=jp: e.matmul(sTs[2][:, 64 + hc:65 + hc], lhsT=w1[:, X, jp, hc * 128:(hc + 1) * 128], rhs=peT[:, X, jp:jp + 1], start=(jp == 0), stop=(jp == 15)), reads=[b_cw], writes=[b_b1ps], accum=True)
        P.op("vector", lambda e, X=X: e.tensor_tensor(out=bias1[:, X], in0=b1ps, in1=b1T[:, X], op=ALU.add), reads=[b_b1ps, b_cw], writes=[b_bias1])
        for g in range(2):
            for nh in range(4):
                fb = Fb[fi % 2]; bfb = b_Fb[fi % 2]
                P.dma("sync", lambda e, fb=fb, X=X, g=g, nh=nh: e.dma_start(out=fb[:].rearrange("p a b -> p (a b)"), in_=F_d[X, g, nh]), f"F{fi%2}", writes=[bfb])
                fi += 1
                for hc in range(2):
                    sT = sTs[hc]
                    for jp in range(16):
                        P.op("tensor", lambda e, X=X, hc=hc, jp=jp, fb=fb, sT=sT: e.matmul(sT[:, 0:256], lhsT=w1[:, X, jp, hc * 128:(hc + 1) * 128], rhs=fb[:, jp, :], start=(jp == 0), stop=(jp == 15)), reads=[b_cw, bfb], writes=[b_sT[hc]], accum=True)
                    P.op("scalar", lambda e, X=X, hc=hc, sT=sT: e.activation(out=hT[:, hc, :], in_=sT[:, 0:256], func=AF.Gelu_apprx_tanh, bias=bias1[:, X, hc:hc + 1], scale=1.0), reads=[b_sT[hc], b_bias1], writes=[b_hT[hc]])
                for ntl in range(2):
                    nt = nh * 2 + ntl
                    for hc in range(2):
                        P.op("tensor", lambda e, X=X, hc=hc, ntl=ntl: e.matmul(cps, lhsT=hT[:, hc, ntl * 128:(ntl + 1) * 128], rhs=w2[:, X, hc, :], start=(hc == 0), stop=(hc == 1)), reads=[b_hT[hc], b_cw], writes=[b_cps], accum=True)
                    if X == 1:
                        P.op("vector", lambda e, nt=nt, g=g: e.tensor_tensor(out=Vc[:, nt, g, 0:64], in0=cps, in1=b2[:, 1], op=ALU.add), reads=[b_cps, b_cw], writes=[b_Vc])
                    else:
                        P.op("vector", lambda e, nt=nt, g=g: e.tensor_tensor(out=kcf[:, nt, g, :], in0=cps, in1=b2[:, 0], op=ALU.add), reads=[b_cps, b_cw], writes=[b_kcf])
        if X == 0:
            P.op("gpsimd", lambda e: e.tensor_copy(out=kcb[:], in_=kcf[:]), reads=[b_kcf], writes=[b_kcb])
            cb = cosc[:].unsqueeze(2).to_broadcast([128, 8, 2, 8]); sb_ = sinc[:].unsqueeze(2).to_broadcast([128, 8, 2, 8])
            x1 = kcf[:, :, :, 0:8]; x2 = kcf[:, :, :, 8:16]
            P.op("vector", lambda e: e.tensor_tensor(out=rt[:, 0], in0=x1, in1=cb, op=ALU.mult), reads=[b_kcf, b_c], writes=[b_rt])
            P.op("vector", lambda e: e.tensor_tensor(out=rt[:, 1], in0=x2, in1=sb_, op=ALU.mult), reads=[b_kcf, b_c], writes=[b_rt])
            P.op("vector", lambda e: e.tensor_tensor(out=rt[:, 2], in0=x2, in1=cb, op=ALU.mult), reads=[b_kcf, b_c], writes=[b_rt])
            P.op("vector", lambda e: e.tensor_tensor(out=rt[:, 3], in0=x1, in1=sb_, op=ALU.mult), reads=[b_kcf, b_c], writes=[b_rt])
            P.op("vector", lambda e: e.tensor_tensor(out=kcb[:, :, :, 0:8], in0=rt[:, 0], in1=rt[:, 1], op=ALU.subtract), reads=[b_rt], writes=[b_kcb])
            P.op("vector", lambda e: e.tensor_tensor(out=kcb[:, :, :, 8:16], in0=rt[:, 2], in1=rt[:, 3], op=ALU.add), reads=[b_rt], writes=[b_kcb])
            for nt in range(8):
                P.op("tensor", lambda e, nt=nt: e.transpose(out=tp[:, 0, :], in_=kcb[:, nt].rearrange("p a b -> p (a b)"), identity=ident[:]), reads=[b_kcb, b_c], writes=[b_tp])
                P.op("vector", lambda e, nt=nt: e.tensor_copy(out=KcT[:, nt * 128:(nt + 1) * 128], in_=tp[:, 0, :]), reads=[b_tp], writes=[b_KcT])
    QT = [P.sb(f"QT{i}", [128, 2, 512], BF16) for i in range(2)]
    gts = [P.sb(f"gts{i}", [128, 8, 3], F32) for i in range(2)]
    sbias = [P.sb(f"sbias{i}", [128, 256], F32) for i in range(2)]
    msk = [P.sb(f"msk{i}", [128, 15, 128], BF16) for i in range(2)]
    KwT = [P.sb(f"KwT{i}", [128, 640], BF16) for i in range(2)]
    Vw = [P.sb(f"Vw{i}", [128, 5, 2, 65], BF16) for i in range(2)]
    b_sl = [P.buf() for _ in range(2)]
    eT = P.sb("eT", [128, 8, 512], BF16); b_eT = [P.buf() for _ in range(8)]
    pTs = [P.sb(f"pT{i}", [128, 512], BF16) for i in range(4)]; b_pT = [P.buf() for _ in range(4)]
    nsT4 = P.sb("nsT4", [128, 2, 4, 128], BF16); b_ns = P.buf()
    score = P.sb("score", [128, 256], F32); b_score = P.buf()
    sc2 = P.sb("sc2", [128, 256], F32); b_sc2 = P.buf()
    m8 = P.sb("m8", [128, 16], F32); b_m8 = P.buf()
    rd = P.sb("rd", [128, 4], F32); b_rd = P.buf()
    negsel = P.sb("negsel", [128, 256], BF16); b_negsel = P.buf()
    wcs = [P.sb(f"wc{i}", [128, 4], F32) for i in range(3)]; b_wc = [P.buf() for _ in range(3)]
    acc = [P.sb(f"acc{i}", [128, 8, 64], F32) for i in range(2)]; b_acc = [P.buf() for _ in range(2)]
    cnt = {"sT": 0, "pT": 0, "oa": 0}

    def load_slot(s):
        k2 = s % 2
        w = [b_sl[k2]]
        st = f"sl{k2}"
        P.dma("sync", lambda e: e.dma_start(out=QT[k2][:].rearrange("p a b -> p (a b)"), in_=QT_d[s]), st, writes=w)
        P.dma("sync", lambda e: e.dma_start(out=gts[k2][:].rearrange("p a b -> p (a b)"), in_=gates_d[s]), st, writes=w)
        P.dma("sync", lambda e: e.dma_start(out=sbias[k2][:], in_=sbias_d[s]), st, writes=w)
        P.dma("sync", lambda e: e.dma_start(out=msk[k2][:].rearrange("p a b -> p (a b)"), in_=msk_d[s]), st, writes=w)
        P.dma("sync", lambda e: e.dma_start(out=KwT[k2][:], in_=KwT_d[s]), st, writes=w)
        P.dma("sync", lambda e: e.dma_start(out=Vw[k2][:].rearrange("p a b c -> p (a b c)"), in_=Vw_d[s]), st, writes=w)

    def branch(k2, g, tiles, lhs_fn, lhs_bufs, v_fn, v_bufs, mask_fn, pbufs, on_exp=None):
        gp = slice(g * 64, (g + 1) * 64)
        oi = cnt["oa"] % 2; cnt["oa"] += 1
        oacc = oaccs[oi]; boacc = b_oacc[oi]
        n = len(tiles)
        sbank = {}

        def S(i):
            t = tiles[i]
            bi = cnt["sT"] % 3; cnt["sT"] += 1
            sbank[i] = bi
            sT = sTs[bi]
            extra = mask_fn(t)
            l0 = lhs_fn(t); rq = QT[k2][:, g, :]
            P.op("tensor", lambda e: e.matmul(sT[:], lhsT=l0, rhs=rq, start=True, stop=(len(extra) == 0)), reads=[b_sl[k2]] + lhs_bufs, writes=[b_sT[bi]])
            for xi, (kind, l_ap, r_ap, rb) in enumerate(extra):
                last = xi == len(extra) - 1
                if kind == "full":
                    P.op("tensor", lambda e, l_ap=l_ap, r_ap=r_ap, last=last: e.matmul(sT[:], lhsT=l_ap, rhs=r_ap, start=False, stop=last), reads=rb, writes=[b_sT[bi]], accum=True)
                else:
                    for h in range(4):
                        P.op("tensor", lambda e, l_ap=l_ap, r_ap=r_ap, last=last, h=h: e.matmul(sT[:, h * 128:(h + 1) * 128], lhsT=l_ap, rhs=r_ap, start=False, stop=(last and h == 3)), reads=rb, writes=[b_sT[bi]], accum=True)

        def E(i):
            t = tiles[i]
            bi = sbank[i]
            p_ap, p_b = pbufs(i)
            P.op("scalar", lambda e: e.activation(out=p_ap, in_=sTs[bi][:], func=AF.Exp, scale=SCALE), reads=[b_sT[bi]], writes=[p_b])

        def V(i):
            t = tiles[i]
            p_ap, p_b = pbufs(i)
            v0 = v_fn(t)
            for h in range(4):
                P.op("tensor", lambda e, h=h: e.matmul(oacc[:, h, 0:65], lhsT=p_ap[:, h * 128:(h + 1) * 128], rhs=v0, start=(i == 0 and h == 0), stop=(i == n - 1 and h == 3), skip_group_check=True), reads=[p_b] + v_bufs, writes=[boacc], accum=(i > 0 or h > 0))
            if on_exp is not None:
                on_exp(i, t, p_ap, p_b)

        for i in range(min(2, n)):
            S(i)
        for i in range(n):
            E(i)
            if i + 2 < n:
                S(i + 2)
            V(i)
        return oacc, boacc

    def finalize(k2, g, br, oacc, boacc, ak):
        wc = wcs[br]; bwc = b_wc[br]
        P.op("vector", lambda e: e.tensor_scalar(out=wc[:], in0=oacc[:, :, 64], scalar1=1e-30, scalar2=None, op0=ALU.max), reads=[boacc], writes=[bwc])
        P.op("vector", lambda e: e.reciprocal(out=wc[:], in_=wc[:]), reads=[bwc], writes=[bwc])
        if debug:
            P.op("vector", lambda e: e.tensor_tensor(out=dbg_t[:, br, 4 * g:4 * g + 4, :], in0=oacc[:, :, 0:64], in1=wc[:].unsqueeze(2).to_broadcast([128, 4, 64]), op=ALU.mult), reads=[boacc, bwc], writes=[b_dbg])
        P.op("vector", lambda e: e.tensor_tensor(out=wc[:], in0=wc[:], in1=gts[k2][:, 4 * g:4 * g + 4, br], op=ALU.mult), reads=[bwc, b_sl[k2]], writes=[bwc])
        dst = acc[ak][:, 4 * g:4 * g + 4, :]
        wb = wc[:].unsqueeze(2).to_broadcast([128, 4, 64])
        if br == 0:
            P.op("vector", lambda e: e.tensor_tensor(out=dst, in0=oacc[:, :, 0:64], in1=wb, op=ALU.mult), reads=[boacc, bwc], writes=[b_acc[ak]])
        else:
            tmp = acc_tmp
            P.op("vector", lambda e: e.tensor_tensor(out=tmp[:], in0=oacc[:, :, 0:64], in1=wb, op=ALU.mult), reads=[boacc, bwc], writes=[b_acctmp])
            P.op("gpsimd", lambda e: e.tensor_tensor(out=dst, in0=dst, in1=tmp[:], op=ALU.add), reads=[b_acctmp, b_acc[ak]], writes=[b_acc[ak]])

    acc_tmp = P.sb("acc_tmp", [128, 4, 64], F32); b_acctmp = P.buf()
    dbg_t = P.sb("dbg_t", [128, 3, 8, 64], F32); b_dbg = P.buf()

    def do_slot(s):
        k2 = s % 2; j = s // 2
        KT = 16 * j + 8 if s % 2 == 0 else 16 * j + 16
        rag0 = KT - 8
        if s + 1 < nslots:
            load_slot(s + 1)
        for g in range(2):
            gp = slice(g * 64, (g + 1) * 64)
            def cmask(nt):
                if nt >= j - 1:
                    mi = nt - (j - 1)
                    return [("head", ident[:], msk[k2][:, mi, :], [b_c, b_sl[k2]])]
                return []

            def imp_mm(i, nt, p_ap, p_b, nn=j + 1):
                for h in range(4):
                    P.op("tensor", lambda e, h=h: e.matmul(imps[h // 2][:, h % 2, :], lhsT=p_ap[:, h * 128:(h + 1) * 128], rhs=ov[:, nt, :], start=(i == 0 and h % 2 == 0), stop=(i == nn - 1 and h % 2 == 1), skip_group_check=True), reads=[p_b, b_c], writes=[b_imp], accum=(i > 0 or h > 0))

            oacc, boacc = branch(k2, g, list(range(j + 1)), lambda nt: KcT[:, nt * 128:(nt + 1) * 128], [b_KcT], lambda nt: Vc[:, nt, g, :], [b_Vc], cmask,
                                 lambda i: (eT[:, i, :], b_eT[i]), on_exp=imp_mm)
            P.op("vector", lambda e: e.tensor_scalar(out=rd[:, 0:2], in0=imps[0][:, :, 255], scalar1=1e-30, scalar2=None, op0=ALU.max), reads=[b_imp], writes=[b_rd])
            P.op("vector", lambda e: e.tensor_scalar(out=rd[:, 2:4], in0=imps[1][:, :, 255], scalar1=1e-30, scalar2=None, op0=ALU.max), reads=[b_imp], writes=[b_rd])
            P.op("vector", lambda e: e.reciprocal(out=rd[:], in_=rd[:]), reads=[b_rd], writes=[b_rd])
            P.op("vector", lambda e: e.scalar_tensor_tensor(out=score[:], in0=imps[0][:, 0, :], scalar=rd[:, 0:1], in1=sbias[k2][:], op0=ALU.mult, op1=ALU.add), reads=[b_imp, b_rd, b_sl[k2]], writes=[b_score])
            for h in range(1, 4):
                P.op("vector", lambda e, h=h: e.scalar_tensor_tensor(out=score[:], in0=imps[h // 2][:, h % 2, :], scalar=rd[:, h:h + 1], in1=score[:], op0=ALU.mult, op1=ALU.add), reads=[b_imp, b_rd, b_score], writes=[b_score])
            P.op("vector", lambda e: e.max(out=m8[:, 0:8], in_=score[:]), reads=[b_score], writes=[b_m8])
            P.op("vector", lambda e: e.match_replace(out=sc2[:], in_to_replace=m8[:, 0:8], in_values=score[:], imm_value=-3e38), reads=[b_score, b_m8], writes=[b_sc2])
            P.op("vector", lambda e: e.max(out=m8[:, 8:16], in_=sc2[:]), reads=[b_sc2], writes=[b_m8])
            P.op("vector", lambda e: e.tensor_scalar(out=negsel[:], in0=score[:], scalar1=m8[:, 15:16], scalar2=NEGM, op0=ALU.is_lt, op1=ALU.mult), reads=[b_score, b_m8], writes=[b_negsel])
            for hf in range(2):
                P.op("tensor", lambda e, hf=hf: e.transpose(out=tp[:, hf, :], in_=negsel[:, hf * 128:(hf + 1) * 128], identity=ident[:]), reads=[b_negsel, b_c], writes=[b_tp], accum=(hf == 1))
            P.op("vector", lambda e: e.tensor_copy(out=nsT4[:], in_=tp[:].unsqueeze(2).to_broadcast([128, 2, 4, 128])), reads=[b_tp], writes=[b_ns])
            finalize(k2, g, 0, oacc, boacc, k2)
            oacc, boacc = branch(k2, g, list(range(5)), lambda w: KwT[k2][:, w * 128:(w + 1) * 128], [], lambda w: Vw[k2][:, w, g, :], [b_sl[k2]],
                                 lambda w: [("head", ident[:], msk[k2][:, 10 + w, :], [b_c, b_sl[k2]])],
                                 lambda i: (pTs[cnt_p(i)][:], b_pT[cnt_p(i)]))
            finalize(k2, g, 2, oacc, boacc, k2)
            def smask(kt):
                ex = [("full", IndAll[:, kt % 64, :], nsT4[:, kt // 64].rearrange("p a b -> p (a b)"), [b_c, b_ns])]
                if kt >= rag0:
                    ex.append(("head", ident[:], msk[k2][:, 2 + kt - rag0, :], [b_c, b_sl[k2]]))
                return ex
            oacc, boacc = branch(k2, g, list(range(KT)), lambda kt: KsT[:, kt * 128:(kt + 1) * 128], [b_KsT], lambda kt: Vs[:, kt, g, :], [b_Vs], smask,
                                 lambda i: (pTs[cnt_p(i)][:], b_pT[cnt_p(i)]))
            finalize(k2, g, 1, oacc, boacc, k2)
        P.dma("sync", lambda e: e.dma_start(out=attn_o[s * 128:(s + 1) * 128, :], in_=acc[k2][:].rearrange("p a b -> p (a b)")), f"ao{k2}", reads=[b_acc[k2]])
        if debug:
            P.dma("sync", lambda e: e.dma_start(out=dbg_o[s * 128:(s + 1) * 128, :], in_=dbg_t[:].rearrange("p a b c -> p (a b c)")), "dbg", reads=[b_dbg])

    def cnt_p(i):
        return i % 4

    load_slot(0)
    for s in range(nslots):
        do_slot(s)
    P.emit()
    return nc


D = 1024; TPC = 2048; DFF = 2816
EPS = 1e-6
CH = 512
NCH = TPC // CH
TPCH = CH // 128

def build_C(halo=True, nchunks=NCH, stages=(1, 2, 3), v=0):
    nc = bass.Bass("TRN2", target_bir_lowering=False)
    din = lambda n, sh, dt=F32: nc.dram_tensor(n, sh, dt, kind="ExternalInput").ap()
    dout = lambda n, sh, dt=F32: nc.dram_tensor(n, sh, dt, kind="ExternalOutput").ap()
    h_d = din("h", [TPC + 128, D])
    attn_d = din("attn", [TPC + 128, 512])
    mlpn_d = din("mlpn", [TPC + 128, 512], BF16)
    p_d = din("p", [TPC, 256])
    gattn_d = din("gattn", [128, 512]); gpost_d = din("gpost", [128, D]); gpre_d = din("gpre", [128, D])
    gpffn_d = din("gpffn", [128, D]); gple_d = din("gple", [128, D])
    conv_d = din("conv", [128, 44 * 4])
    wo_d = din("wo", [128, 8 * D], BF16)
    wup_d = din("wup", [22, 128, 2 * 8 * 128], BF16)
    wdn_d = din("wdn", [128, 22 * D], BF16)
    wg_d = din("wg", [128, 8 * D], BF16)
    wp_d = din("wp", [128, 2 * D], BF16)
    id_d = din("ident", [128, 128], BF16)
    out_d = dout("hout", [TPC, D])
    P = Prog(nc)
    wo = P.sb("wo", [128, 8, D], BF16); wdn = P.sb("wdn", [128, 22, D], BF16); wg = P.sb("wg", [128, 8, D], BF16); wp = P.sb("wp", [128, 2, D], BF16)
    b_w = P.buf()
    gattn = P.sb("gattn", [128, 512], F32); gpost = P.sb("gpost", [128, D], F32); gpre = P.sb("gpre", [128, D], F32)
    gpffn = P.sb("gpffn", [128, D], F32); gple = P.sb("gple", [128, D], F32)
    conv = P.sb("conv", [128, 44, 4], F32); ident = P.sb("ident", [128, 128], BF16)
    b_c = P.buf()
    for (t, d_, q) in [(wo, wo_d, "sync"), (wdn, wdn_d, "gpsimd"), (wg, wg_d, "gpsimd"), (wp, wp_d, "gpsimd")]:
        P.dma(q, lambda e, t=t, d_=d_: e.dma_start(out=t[:].rearrange("p a b -> p (a b)"), in_=d_), "cw" + q, writes=[b_w])
    for (t, d_) in [(gattn, gattn_d), (gpost, gpost_d), (gpre, gpre_d), (gpffn, gpffn_d), (gple, gple_d), (ident, id_d)]:
        P.dma("sync", lambda e, t=t, d_=d_: e.dma_start(out=t[:], in_=d_), "cc", writes=[b_c])
    P.dma("sync", lambda e: e.dma_start(out=conv[:].rearrange("p a b -> p (a b)"), in_=conv_d), "cc", writes=[b_c])
    psT = P.ps("psT", [128, 8, 128], BF16); b_psT = P.buf()
    M = [P.ps(f"M{i}", [128, 512], F32) for i in range(2)]; b_M = P.buf()
    U = [P.ps(f"U{i}", [128, 512], F32) for i in range(4)]; b_U = [P.buf() for _ in range(4)]
    h1c = P.sb("h1c", [128, TPCH, D], F32); b_h1 = [P.buf() for _ in range(TPCH)]
    hh = P.sb("hh", [128, D], F32); b_hh = P.buf()
    hnT = P.sb("hnT", [128, 8, CH], BF16); b_hnT = [P.buf() for _ in range(TPCH)]
    hnTh = P.sb("hnTh", [128, 8, 2], BF16); b_hnTh = P.buf()
    actT = P.sb("actT", [128, 22, CH], BF16); b_actT = [P.buf() for _ in range(22)]
    carry = P.sb("carry", [128, 44, 2], F32); b_carry = [P.buf() for _ in range(22)]
    hup = [[P.sb(f"hup{s}{x}", [128, CH + 2], F32) for x in range(2)] for s in range(2)]; b_hup = [[P.buf() for x in range(2)] for s in range(2)]
    cgu = [[P.sb(f"cgu{s}{x}", [128, CH], F32) for x in range(2)] for s in range(2)]; b_cgu = [[P.buf() for x in range(2)] for s in range(2)]
    wub = [P.sb(f"wub{i}", [128, 2, 8, 128], BF16) for i in range(2)]; b_wub = [P.buf() for _ in range(2)]
    att = [P.sb(f"att{i}", [128, 512], F32) for i in range(2)]; mlb = [P.sb(f"mlb{i}", [128, 512], BF16) for i in range(2)]; b_in = [P.buf() for _ in range(2)]
    pin = [P.sb(f"pin{i}", [128, 256], F32) for i in range(2)]; b_pin = [P.buf() for _ in range(2)]
    NP_ = 2
    xb_l = [P.sb(f"xb{i}", [128, D], BF16) for i in range(NP_)]; b_xb_l = [P.buf() for _ in range(NP_)]
    xT_l = [P.sb(f"xT{i}", [128, 8, 128], BF16) for i in range(NP_)]; b_xT_l = [P.buf() for _ in range(NP_)]
    pb_l = [P.sb(f"pb{i}", [128, 256], BF16) for i in range(NP_)]; b_pb_l = [P.buf() for _ in range(NP_)]
    pT_l = [P.sb(f"pT{i}", [128, 2, 128], BF16) for i in range(NP_)]; b_pT_l = [P.buf() for _ in range(NP_)]
    tmp_l = [P.sb(f"tmp{i}", [128, D], F32) for i in range(NP_)]; b_tmp_l = [P.buf() for _ in range(NP_)]
    junk_l = [P.sb(f"junk{i}", [128, D], BF16) for i in range(NP_)]; b_junk_l = [P.buf() for _ in range(NP_)]
    st_l = [P.sb(f"st{i}", [128, 16], F32) for i in range(NP_)]; b_st_l = [P.buf() for _ in range(NP_)]
    cnt = {"w": 0, "in": 0, "p": 0, "t": 0}

    def rstd(par, src_ap, nparts, width, col, reads):
        junk = junk_l[par]; b_junk = b_junk_l[par]; st = st_l[par]; b_st = b_st_l[par]
        P.op("scalar", lambda e: e.activation(out=junk[0:nparts, 0:width], in_=src_ap, func=AF.Square, accum_out=st[0:nparts, col:col + 1]), reads=reads, writes=[b_junk, b_st])
        P.op("scalar", lambda e: e.activation(out=st[0:nparts, col + 1:col + 2], in_=st[0:nparts, col:col + 1], func=AF.Sqrt, scale=1.0 / width, bias=EPS), reads=[b_st], writes=[b_st])
        P.op("vector", lambda e: e.reciprocal(out=st[0:nparts, col + 2:col + 3], in_=st[0:nparts, col + 1:col + 2]), reads=[b_st], writes=[b_st])
        return st[0:nparts, col + 2:col + 3]

    def rstd_psum(par, nparts, col, reads):
        junk = junk_l[par]; b_junk = b_junk_l[par]; st = st_l[par]; b_st = b_st_l[par]
        P.op("scalar", lambda e: e.activation(out=junk[0:nparts, 0:512], in_=M[0][0:nparts, :], func=AF.Square, accum_out=st[0:nparts, col:col + 1]), reads=reads, writes=[b_junk, b_st])
        P.op("scalar", lambda e: e.activation(out=junk[0:nparts, 512:1024], in_=M[1][0:nparts, :], func=AF.Square, accum_out=st[0:nparts, col + 3:col + 4]), reads=reads, writes=[b_junk, b_st])
        P.op("vector", lambda e: e.tensor_tensor(out=st[0:nparts, col:col + 1], in0=st[0:nparts, col:col + 1], in1=st[0:nparts, col + 3:col + 4], op=ALU.add), reads=[b_st], writes=[b_st])
        P.op("scalar", lambda e: e.activation(out=st[0:nparts, col + 1:col + 2], in_=st[0:nparts, col:col + 1], func=AF.Sqrt, scale=1.0 / D, bias=EPS), reads=[b_st], writes=[b_st])
        P.op("vector", lambda e: e.reciprocal(out=st[0:nparts, col + 2:col + 3], in_=st[0:nparts, col + 1:col + 2]), reads=[b_st], writes=[b_st])
        return st[0:nparts, col + 2:col + 3]

    def transposes(src_bf, nparts, nk, dstT, b_dst, reads):
        for k in range(nk):
            P.op("tensor", lambda e, k=k: e.transpose(out=psT[:, k, 0:nparts], in_=src_bf[0:nparts, k * 128:(k + 1) * 128], identity=ident[0:nparts, 0:nparts]), reads=reads + [b_c], writes=[b_psT])
        P.op("vector", lambda e: e.tensor_copy(out=dstT, in_=psT[:, 0:nk, 0:nparts]), reads=[b_psT], writes=[b_dst])

    def mm1024(lhsT_fn, nk, w_t, nparts, reads, banks=None, bbufs=None):
        banks = banks or M
        for nch in range(2):
            for k in range(nk):
                P.op("tensor", lambda e, k=k, nch=nch: e.matmul(banks[nch][0:nparts, :], lhsT=lhsT_fn(k), rhs=w_t[:, k, nch * 512:(nch + 1) * 512], start=(k == 0), stop=(k == nk - 1)), reads=reads + [b_w], writes=[bbufs[nch] if bbufs else b_M])

    def load_in(row0, nparts):
        k = cnt["in"] % 2; cnt["in"] += 1
        P.dma("sync", lambda e: e.dma_start(out=att[k][0:nparts, :], in_=attn_d[row0:row0 + nparts, :]), f"in{k}", writes=[b_in[k]])
        P.dma("sync", lambda e: e.dma_start(out=mlb[k][0:nparts, :], in_=mlpn_d[row0:row0 + nparts, :]), f"in{k}", writes=[b_in[k]])
        return k

    def stage1(h_ap, b_h, row0, nparts, dstT, b_dst):
        par = cnt["t"] % NP_; cnt["t"] += 1
        xb = xb_l[par]; b_xb = b_xb_l[par]; xT = xT_l[par]; b_xT = b_xT_l[par]; tmp = tmp_l[par]; b_tmp = b_tmp_l[par]; b_st = b_st_l[par]
        k = load_in(row0, nparts)
        r = rstd(par, att[k][0:nparts, :], nparts, 512, 0, [b_in[k]])
        P.op("vector", lambda e: e.scalar_tensor_tensor(out=xb[0:nparts, 0:512], in0=att[k][0:nparts, :], scalar=r, in1=gattn[0:nparts, :], op0=ALU.mult, op1=ALU.mult), reads=[b_in[k], b_st, b_c], writes=[b_xb])
        P.op("gpsimd", lambda e: e.tensor_copy(out=xb[0:nparts, 512:1024], in_=mlb[k][0:nparts, :]), reads=[b_in[k]], writes=[b_xb])
        transposes(xb, nparts, 8, xT[:, :, 0:nparts], b_xT, [b_xb])
        mm1024(lambda kk: xT[:, kk, 0:nparts], 8, wo, nparts, [b_xT])
        yield
        r2 = rstd_psum(par, nparts, 4, [b_M])
        for nch in range(2):
            P.op("vector", lambda e, nch=nch: e.scalar_tensor_tensor(out=tmp[0:nparts, nch * 512:(nch + 1) * 512], in0=M[nch][0:nparts, :], scalar=r2, in1=gpost[0:nparts, nch * 512:(nch + 1) * 512], op0=ALU.mult, op1=ALU.mult), reads=[b_M, b_st, b_c], writes=[b_tmp])
        P.op("gpsimd", lambda e: e.tensor_tensor(out=h_ap, in0=h_ap, in1=tmp[0:nparts, :], op=ALU.add), reads=[b_tmp, b_h], writes=[b_h])
        r3 = rstd(par, h_ap, nparts, D, 8, [b_h])
        P.op("vector", lambda e: e.scalar_tensor_tensor(out=xb[0:nparts, :], in0=h_ap, scalar=r3, in1=gpre[0:nparts, :], op0=ALU.mult, op1=ALU.mult), reads=[b_h, b_st, b_c], writes=[b_xb])
        transposes(xb, nparts, 8, dstT, b_dst, [b_xb])

    def load_w(m):
        k = cnt["w"] % 2; cnt["w"] += 1
        P.dma("sync", lambda e: e.dma_start(out=wub[k][:].rearrange("p a b c -> p (a b c)"), in_=wup_d[m]), f"wu{k}", writes=[b_wub[k]])
        return k

    def stage2_halo():
        pend = load_w(0)
        for m in range(22):
            k = pend
            if m + 1 < 22:
                pend = load_w(m + 1)
            for x in range(2):
                for kt in range(8):
                    P.op("tensor", lambda e, x=x, kt=kt, m=m, k=k: e.matmul(U[0][:, (x * 22 + m) * 2:(x * 22 + m) * 2 + 2], lhsT=wub[k][:, x, kt, :], rhs=hnTh[:, kt, :], start=(kt == 0), stop=(kt == 7), skip_group_check=True), reads=[b_wub[k], b_hnTh], writes=[b_U[0]])
        P.op("vector", lambda e: e.tensor_copy(out=carry[:].rearrange("p a b -> p (a b)"), in_=U[0][:, 0:88]), reads=[b_U[0]], writes=b_carry)

    def stage2():
        pend = load_w(0)
        for m in range(22):
            k = pend
            if m + 1 < 22:
                pend = load_w(m + 1)
            s = m % 2
            for x in range(2):
                ub = U[s * 2 + x]; bub = b_U[s * 2 + x]
                for kt in range(8):
                    P.op("tensor", lambda e, x=x, kt=kt, ub=ub, k=k: e.matmul(ub[:], lhsT=wub[k][:, x, kt, :], rhs=hnT[:, kt, :], start=(kt == 0), stop=(kt == 7)), reads=[b_wub[k]] + b_hnT, writes=[bub])
            for x in range(2):
                ub = U[s * 2 + x]; bub = b_U[s * 2 + x]
                cg = cgu[s][x]; bcg = b_cgu[s][x]
                ci = x * 22 + m
                bca = b_carry[m]
                P.op("vector", lambda e, cg=cg, ub=ub, ci=ci: e.tensor_scalar(out=cg[:], in0=ub[:], scalar1=conv[:, ci, 2:3], scalar2=conv[:, ci, 3:4], op0=ALU.mult, op1=ALU.add), reads=[bub, b_c], writes=[bcg])
                P.op("vector", lambda e, cg=cg, ub=ub, ci=ci: e.scalar_tensor_tensor(out=cg[:, 1:CH], in0=ub[:, 0:CH - 1], scalar=conv[:, ci, 1:2], in1=cg[:, 1:CH], op0=ALU.mult, op1=ALU.add), reads=[bub, bcg, b_c], writes=[bcg])
                P.op("vector", lambda e, cg=cg, ub=ub, ci=ci: e.scalar_tensor_tensor(out=cg[:, 2:CH], in0=ub[:, 0:CH - 2], scalar=conv[:, ci, 0:1], in1=cg[:, 2:CH], op0=ALU.mult, op1=ALU.add), reads=[bub, bcg, b_c], writes=[bcg])
                P.op("vector", lambda e, cg=cg, ci=ci: e.scalar_tensor_tensor(out=cg[:, 0:2], in0=carry[:, ci, :], scalar=conv[:, ci, 0:1], in1=cg[:, 0:2], op0=ALU.mult, op1=ALU.add), reads=[bca, bcg, b_c], writes=[bcg])
                P.op("vector", lambda e, cg=cg, ci=ci: e.scalar_tensor_tensor(out=cg[:, 0:1], in0=carry[:, ci, 1:2], scalar=conv[:, ci, 1:2], in1=cg[:, 0:1], op0=ALU.mult, op1=ALU.add), reads=[bca, bcg, b_c], writes=[bcg])
                P.op("vector", lambda e, ub=ub, ci=ci: e.tensor_copy(out=carry[:, ci, :], in_=ub[:, CH - 2:CH]), reads=[bub, bcg], writes=[bca])
            cg = cgu[s][0]; cu = cgu[s][1]
            P.op("scalar", lambda e, cg=cg: e.activation(out=cg[:], in_=cg[:], func=AF.Silu), reads=[b_cgu[s][0]], writes=[b_cgu[s][0]])
            P.op("gpsimd", lambda e, cg=cg, cu=cu, m=m: e.tensor_tensor(out=actT[:, m, :], in0=cg[:], in1=cu[:], op=ALU.mult), reads=[b_cgu[s][0], b_cgu[s][1]], writes=[b_actT[m]])

    def load_h(ti, row0):
        P.dma("sync", lambda e: e.dma_start(out=h1c[:, ti, :], in_=h_d[row0:row0 + 128, :]), f"hl{ti%2}", writes=[b_h1[ti]])

    def stage3(ti, row0):
        h_ap = h1c[:, ti, :]; b_h = b_h1[ti]
        kp = cnt["p"] % 2; cnt["p"] += 1
        par = cnt["t"] % NP_; cnt["t"] += 1
        xb = xb_l[par]; b_xb = b_xb_l[par]; xT = xT_l[par]; b_xT = b_xT_l[par]; tmp = tmp_l[par]; b_tmp = b_tmp_l[par]; b_st = b_st_l[par]
        pb = pb_l[par]; b_pb = b_pb_l[par]; pT = pT_l[par]; b_pT = b_pT_l[par]
        P.dma("sync", lambda e: e.dma_start(out=pin[kp][:], in_=p_d[row0:row0 + 128, :]), f"pl{kp}", writes=[b_pin[kp]])
        for nch in range(2):
            for m in range(22):
                P.op("tensor", lambda e, m=m, nch=nch: e.matmul(M[nch][:], lhsT=actT[:, m, ti * 128:(ti + 1) * 128], rhs=wdn[:, m, nch * 512:(nch + 1) * 512], start=(m == 0), stop=(m == 21)), reads=[b_actT[m], b_w], writes=[b_M])
        yield
        r = rstd_psum(par, 128, 4, [b_M])
        for nch in range(2):
            P.op("vector", lambda e, nch=nch: e.scalar_tensor_tensor(out=tmp[:, nch * 512:(nch + 1) * 512], in0=M[nch][:], scalar=r, in1=gpffn[:, nch * 512:(nch + 1) * 512], op0=ALU.mult, op1=ALU.mult), reads=[b_M, b_st, b_c], writes=[b_tmp])
        P.op("gpsimd", lambda e: e.tensor_tensor(out=h_ap, in0=h_ap, in1=tmp[:], op=ALU.add), reads=[b_tmp, b_h], writes=[b_h])
        r3 = rstd(par, h_ap, 128, D, 8, [b_h])
        P.op("vector", lambda e: e.scalar_tensor_tensor(out=xb[:], in0=h_ap, scalar=r3, in1=gple[:], op0=ALU.mult, op1=ALU.mult), reads=[b_h, b_st, b_c], writes=[b_xb])
        yield
        transposes(xb, 128, 8, xT[:], b_xT, [b_xb])
        mm1024(lambda kk: xT[:, kk, :], 8, wg, 128, [b_xT], banks=[U[2], U[3]], bbufs=[b_U[2], b_U[3]])
        for nch in range(2):
            P.op("scalar", lambda e, nch=nch: e.activation(out=tmp[:, nch * 512:(nch + 1) * 512], in_=U[2 + nch][:], func=AF.Sigmoid), reads=[b_U[2 + nch]], writes=[b_tmp])
        P.op("gpsimd", lambda e: e.tensor_copy(out=pb[:], in_=pin[kp][:]), reads=[b_pin[kp]], writes=[b_pb])
        transposes(pb, 128, 2, pT[:], b_pT, [b_pb])
        for nch in range(2):
            for k in range(2):
                P.op("tensor", lambda e, k=k, nch=nch: e.matmul(U[nch][:], lhsT=pT[:, k, :], rhs=wp[:, k, nch * 512:(nch + 1) * 512], start=(k == 0), stop=(k == 1)), reads=[b_pT, b_w], writes=[b_U[nch]])
            P.op("vector", lambda e, nch=nch: e.tensor_tensor(out=tmp[:, nch * 512:(nch + 1) * 512], in0=tmp[:, nch * 512:(nch + 1) * 512], in1=U[nch][:], op=ALU.mult), reads=[b_tmp, b_U[nch]], writes=[b_tmp])
        P.op("gpsimd", lambda e: e.tensor_tensor(out=h_ap, in0=h_ap, in1=tmp[:], op=ALU.add), reads=[b_tmp, b_h], writes=[b_h])
        P.dma("sync", lambda e: e.dma_start(out=out_d[row0:row0 + 128, :], in_=h_ap), f"so{ti%2}", reads=[b_h])

    if halo:
        P.dma("sync", lambda e: e.dma_start(out=hh[0:2, :], in_=h_d[TPC:TPC + 2, :]), "hl0", writes=[b_hh])
        for _ in stage1(hh[0:2, :], b_hh, TPC, 2, hnTh[:], b_hnTh):
            pass
        stage2_halo()
    else:
        P.op("vector", lambda e: e.memset(carry[:], 0.0), writes=b_carry)
    def pipeline(gens, lag):
        n = len(gens); done = [False] * n; step = 0
        while not all(done):
            for i in range(n):
                if done[i] or step < i * lag:
                    continue
                try:
                    next(gens[i])
                except StopIteration:
                    done[i] = True
            step += 1

    for ci in range(nchunks):
        for ti in range(TPCH):
            load_h(ti, ci * CH + ti * 128)
        if 1 in stages:
            pipeline([stage1(h1c[:, ti, :], b_h1[ti], ci * CH + ti * 128, 128, hnT[:, :, ti * 128:(ti + 1) * 128], b_hnT[ti]) for ti in range(TPCH)], 1)
        if 2 in stages:
            stage2()
        if 3 in stages:
            pipeline([stage3(ti, ci * CH + ti * 128) for ti in range(TPCH)], 1)
        for ti in range(TPCH):
            if 3 in stages:
                pass
            else:
                P.dma("sync", lambda e, ti=ti, ci=ci: e.dma_start(out=out_d[ci * CH + ti * 128:ci * CH + ti * 128 + 128, :], in_=h1c[:, ti, :]), f"so{ti%2}", reads=[b_h1[ti]])
    P.emit()
    return nc

S = 16384
PERM = np.concatenate([np.arange(0, 512), np.arange(768, 896), np.arange(1024, 1152), np.arange(512, 640), np.arange(640, 768), np.arange(896, 1024), np.arange(1152, 1280), np.arange(1280, 2328)])

def ktile(w):
    K, N = w.shape
    return np.ascontiguousarray(w.reshape(K // 128, 128, N).transpose(1, 0, 2)).reshape(128, -1)

def rope_tables(pos):
    half = 8
    inv = (np.float32(500000.0) ** (-np.arange(half, dtype=np.float32) / np.float32(half))).astype(np.float32)
    ang = pos.astype(np.float32)[:, None] * inv[None, :]
    return np.cos(ang).astype(np.float32), np.sin(ang).astype(np.float32)

def bc(v, n=128):
    return np.ascontiguousarray(np.broadcast_to(v[None, :], (n, v.shape[0]))).astype(np.float32)

def A_inputs(h, i, inp, wbf):
    cos, sin = rope_tables(np.arange(S))
    tri = (np.arange(128)[:, None] <= np.arange(128)[None, :]).astype(BF)
    maps = []
    for c in range(8):
        sl = slice(c * 2048, (c + 1) * 2048)
        t = lambda a: np.ascontiguousarray(a[sl].reshape(16, 128, 8).transpose(1, 0, 2)).reshape(128, 128)
        maps.append(dict(h=np.ascontiguousarray(h[sl]), gpre=bc(inp["pre_mix_g"][i]), w=wbf["w_in"], cos=t(cos), sin=t(sin),
                         lng=bc(inp["gmlp_ln_g"][i]), lnb=bc(inp["gmlp_ln_b"][i]), gmlp=bc(inp["mlp_out_g"][i]),
                         bsT=np.ascontiguousarray(inp["gmlp_bs"][i].T).astype(np.float32), wsT=wbf["wsT"], tri=tri, ident=np.eye(128, dtype=BF)))
    return maps

S = 16384
NEGM = -30000.0

def slot_qi(c, s):
    j = s // 2
    return 16 * j + c if s % 2 == 0 else 16 * j + 15 - c

_CONST = {}
def B_consts():
    if _CONST:
        return _CONST
    n = np.arange(1024)
    jb = np.arange(256)
    ov = ((16 * n[:, None] + 31 >= 64 * jb[None, :]) & (16 * n[:, None] <= 64 * jb[None, :] + 63)).astype(np.float32)
    ov[1023, :] = 0
    ov[:, 255] = 1.0
    _CONST["ov"] = np.ascontiguousarray(ov.reshape(8, 128, 256).transpose(1, 0, 2)).reshape(128, -1).astype(BF)
    ind = np.zeros((128, 64, 128), np.float32)
    for jj in range(64):
        ind[2 * jj, jj, :64] = 1; ind[2 * jj + 1, jj, 64:] = 1
    _CONST["ind"] = ind.reshape(128, -1).astype(BF)
    _CONST["ident"] = np.eye(128, dtype=BF)
    cosc, sinc = rope_tables(16 * np.arange(1024) + 31)
    t = lambda a: np.ascontiguousarray(a.reshape(8, 128, 8).transpose(1, 0, 2)).reshape(128, 64)
    _CONST["cosc"] = t(cosc); _CONST["sinc"] = t(sinc)
    kq = np.arange(128)
    msks = []; sbs = []
    for c in range(8):
        m = np.zeros((16, 128, 15, 128), np.float32)
        sb = np.zeros((16, 128, 256), np.float32)
        for s in range(16):
            qi = slot_qi(c, s); j = s // 2
            tq = 128 * qi + kq
            for mi, nt in enumerate((j - 1, j)):
                if nt < 0: continue
                nn = 128 * nt + kq
                ok = (16 * nn[:, None] + 31 <= tq[None, :]) & (nn[:, None] <= 1022)
                m[s, :, mi, :] = np.where(ok, 0.0, NEGM)
            KT = 16 * j + 8 if s % 2 == 0 else 16 * j + 16
            for r in range(8):
                kt = KT - 8 + r
                tk = 128 * kt + kq
                ok = tk[:, None] <= tq[None, :]
                m[s, :, 2 + r, :] = np.where(ok, 0.0, NEGM)
            for w in range(5):
                tk = 128 * qi - 512 + 128 * w + kq
                ok = (tk[:, None] >= 0) & (tk[:, None] <= tq[None, :]) & (tk[:, None] > tq[None, :] - 512)
                m[s, :, 10 + w, :] = np.where(ok, 0.0, NEGM)
            cur = tq // 64
            valid = jb[None, :] <= cur[:, None]
            forced = valid & ((jb[None, :] == 0) | (jb[None, :] == cur[:, None]) | (jb[None, :] == cur[:, None] - 1))
            sb[s] = np.where(forced, 1000.0, np.where(valid, 0.0, -1e29))
        msks.append(m.reshape(16, 128, -1).astype(BF)); sbs.append(sb)
    _CONST["msk"] = msks; _CONST["sbias"] = sbs
    return _CONST

def B_inputs(qkv, gates, i, inp, wbf):
    C = B_consts()
    q = qkv[:, 0:512].reshape(S, 2, 4, 64)
    ks = qkv[:, 512:640].reshape(S, 2, 64); kw = qkv[:, 640:768].reshape(S, 2, 64)
    zkc = qkv[:, 768:896].reshape(S, 2, 64); zvc = qkv[:, 896:1024].reshape(S, 2, 64)
    vs = qkv[:, 1024:1152].reshape(S, 2, 64); vw = qkv[:, 1152:1280].reshape(S, 2, 64)
    KsT = np.ascontiguousarray(ks.transpose(1, 2, 0)).reshape(128, S)
    one = np.ones((S, 2, 1), BF)
    Vs1 = np.concatenate([vs, one], -1)
    Vs = np.ascontiguousarray(Vs1.reshape(128, 128, 2, 65).transpose(1, 0, 2, 3)).reshape(128, -1)
    kwp = np.concatenate([np.zeros((512, 2, 64), BF), kw], 0)
    vwp = np.concatenate([np.zeros((512, 2, 65), BF), np.concatenate([vw, one], -1)], 0)
    F = np.zeros((2, 2, 128, 16, 1024), BF)
    for X, zz in enumerate((zkc, zvc)):
        zp = np.concatenate([zz, np.zeros((32, 2, 64), BF)], 0)
        n = np.arange(1024); jp = np.arange(16); jj = np.arange(2)
        tidx = 16 * n[None, None, :] + 2 * jp[None, :, None] + jj[:, None, None]
        g_ = zp[tidx]
        g_[:, :, 1023] = 0
        F[X] = g_.transpose(3, 0, 4, 1, 2).reshape(2, 128, 16, 1024)
    Fq = np.ascontiguousarray(F.reshape(2, 2, 128, 16, 4, 256).transpose(0, 1, 4, 2, 3, 5)).reshape(2, 2, 4, 128, 16 * 256)
    b1T = np.stack([np.ascontiguousarray(inp["cmp_b1"][i][X].reshape(2, 128).T) for X in range(2)]).astype(np.float32)
    b2 = np.stack([np.broadcast_to(inp["cmp_b2"][i][X][None, :], (128, 64)) for X in range(2)]).astype(np.float32)
    maps = []
    for c in range(8):
        qis = [slot_qi(c, s) for s in range(16)]
        QT = np.zeros((16, 128, 2, 512), BF)
        for s_, qi in enumerate(qis):
            qq = np.ascontiguousarray(q[128 * qi:128 * qi + 128].transpose(1, 3, 2, 0)).reshape(2, 64, 512)
            QT[s_, 0:64, 0] = qq[0]; QT[s_, 64:128, 1] = qq[1]
        QT = QT.reshape(16, 128, 1024)
        KwT = np.stack([np.ascontiguousarray(kwp[128 * qi:128 * qi + 640].transpose(1, 2, 0)).reshape(128, 640) for qi in qis])
        Vw = np.stack([np.ascontiguousarray(vwp[128 * qi:128 * qi + 640].reshape(5, 128, 2, 65).transpose(1, 0, 2, 3)).reshape(128, -1) for qi in qis])
        gt = np.stack([gates[128 * qi:128 * qi + 128] for qi in qis]).astype(np.float32)
        maps.append(dict(QT=QT, KsT=KsT, Vs=Vs, KwT=KwT, Vw=Vw, F=Fq, gates=gt, sbias=C["sbias"][c], msk=C["msk"][c],
                         w1=wbf["cmp_w1"], w2=wbf["cmp_w2"], b1T=b1T, peT=wbf["cmp_peT"], b2=np.ascontiguousarray(b2),
                         cosc=C["cosc"], sinc=C["sinc"], ov=C["ov"], ind=C["ind"], ident=C["ident"]))
    return maps

def B_gather(results):
    attn = np.zeros((S, 512), np.float32)
    for c in range(8):
        a = results[c]["attn"]
        for s in range(16):
            qi = slot_qi(c, s)
            attn[128 * qi:128 * qi + 128] = a[128 * s:128 * s + 128]
    return attn

def B_weights_f32(inp, i):
    w1 = np.stack([ktile(inp["cmp_w1"][i][X]) for X in range(2)])
    w2 = np.stack([ktile(inp["cmp_w2"][i][X]) for X in range(2)])
    pe = inp["cmp_pe"][i]
    peT = np.stack([np.ascontiguousarray(pe[X].reshape(16, 2, 64).transpose(1, 2, 0)).reshape(128, 16) for X in range(2)])
    return w1, w2, peT

S = 16384

def C_weights_f32(inp, i):
    wup = inp["w_up"][i]
    wup_l = np.ascontiguousarray(wup.reshape(8, 128, 2, 22, 128).transpose(3, 1, 2, 0, 4)).reshape(22, 128, -1)
    return dict(wo=ktile(inp["w_o"][i]), wup=wup_l, wdn=ktile(inp["w_down"][i]), wg=ktile(inp["w_ple_gate"][i]), wp=ktile(inp["w_ple_proj"][i]))

def C_inputs(h, attn, mlpn, i, inp, wbf):
    cw = inp["conv_w"][i]; cb = inp["conv_b"][i]
    conv = np.concatenate([cw, cb[None, :]], 0)
    conv = np.ascontiguousarray(conv.reshape(4, 44, 128).transpose(2, 1, 0)).reshape(128, -1).astype(np.float32)
    maps = []
    for c in range(8):
        sl = slice(2048 * c, 2048 * (c + 1))
        def ext(a):
            o = np.zeros((2048 + 128,) + a.shape[1:], a.dtype)
            o[:2048] = a[sl]
            if c > 0:
                o[2048:2050] = a[2048 * c - 2:2048 * c]
            return o
        maps.append(dict(h=ext(h), attn=ext(attn), mlpn=ext(mlpn), p=np.ascontiguousarray(inp["p"][i, 0][sl]),
                         gattn=bc(inp["attn_out_g"][i]), gpost=bc(inp["post_mix_g"][i]), gpre=bc(inp["pre_ffn_g"][i]),
                         gpffn=bc(inp["post_ffn_g"][i]), gple=bc(inp["ple_norm_g"][i]), conv=conv,
                         wo=wbf["wo"], wup=wbf["wup"], wdn=wbf["wdn"], wg=wbf["wg"], wp=wbf["wp"], ident=np.eye(128, dtype=BF)))
    return maps


_PROGS = {}

def _prog(name, fn):
    if name not in _PROGS:
        _PROGS[name] = fn()
    return _PROGS[name]

def _run(nc, maps):
    res = run_bass_kernel_spmd(nc, maps, core_ids=list(range(8)))
    return res.results

WCOLS = [("w_in", 8 * 2328), ("wsT", 1024), ("cmp_w1", 2 * 4096), ("cmp_w2", 2 * 128), ("cmp_peT", 2 * 16),
         ("wo", 8192), ("wup", 22 * 2048), ("wdn", 22 * 1024), ("wg", 8192), ("wp", 2048)]
WTOT = sum(c for _, c in WCOLS)

def _pack_weights(inp, i):
    cw = C_weights_f32(inp, i)
    w1, w2, peT = B_weights_f32(inp, i)
    parts = {
        "w_in": ktile(inp["w_in"][i][:, PERM]),
        "wsT": np.ascontiguousarray(inp["gmlp_ws"][i].transpose(2, 0, 1)).reshape(128, 1024),
        "cmp_w1": np.ascontiguousarray(w1.transpose(1, 0, 2)).reshape(128, -1),
        "cmp_w2": np.ascontiguousarray(w2.transpose(1, 0, 2)).reshape(128, -1),
        "cmp_peT": np.ascontiguousarray(peT.transpose(1, 0, 2)).reshape(128, -1),
        "wo": cw["wo"], "wup": np.ascontiguousarray(cw["wup"].transpose(1, 0, 2)).reshape(128, -1),
        "wdn": cw["wdn"], "wg": cw["wg"], "wp": cw["wp"],
    }
    return np.concatenate([parts[n].astype(np.float32) for n, _ in WCOLS], axis=1)

def _unpack_weights(wb):
    out = {}
    o = 0
    for n, c in WCOLS:
        out[n] = np.ascontiguousarray(wb[:, o:o + c]); o += c
    out["cmp_w1"] = np.ascontiguousarray(out["cmp_w1"].reshape(128, 2, 4096).transpose(1, 0, 2))
    out["cmp_w2"] = np.ascontiguousarray(out["cmp_w2"].reshape(128, 2, 128).transpose(1, 0, 2))
    out["cmp_peT"] = np.ascontiguousarray(out["cmp_peT"].reshape(128, 2, 16).transpose(1, 0, 2))
    out["wup"] = np.ascontiguousarray(out["wup"].reshape(128, 22, 2048).transpose(1, 0, 2))
    return out

def kernel(**inputs):
    inp = {k: np.asarray(v) for k, v in inputs.items()}
    L = 2
    big = np.concatenate([_pack_weights(inp, i) for i in range(L)], axis=1)
    per = big.shape[1] // 8
    assert per * 8 == big.shape[1]
    ncW = _prog("W", lambda: build_W(per))
    resW = _run(ncW, [{"win": np.ascontiguousarray(big[:, c * per:(c + 1) * per])} for c in range(8)])
    wb = np.concatenate([r["wout"] for r in resW], axis=1)
    wbf = [_unpack_weights(wb[:, i * WTOT:(i + 1) * WTOT]) for i in range(L)]
    h = np.ascontiguousarray(inp["x"][0]).astype(np.float32)
    for i in range(L):
        ncA = _prog("A", build_A)
        resA = _run(ncA, A_inputs(h, i, inp, wbf[i]))
        qkv = np.concatenate([r["qkv"] for r in resA], 0)
        gates = np.concatenate([r["gates"] for r in resA], 0)
        mlpn = np.concatenate([r["mlpn"] for r in resA], 0)
        ncB = _prog("B", build_B)
        resB = _run(ncB, B_inputs(qkv, gates, i, inp, wbf[i]))
        attn = B_gather(resB)
        ncC = _prog("C", build_C)
        resC = _run(ncC, C_inputs(h, attn, mlpn, i, inp, wbf[i]))
        h = np.concatenate([r["hout"] for r in resC], 0)
    return h[None].astype(np.float32)
```
